# Optimizing a Trainium2 kernel written in Bass

```python
import math
import jax
import jax.numpy as jnp
from jax import lax
import numpy as np

D_MODEL = 1024
BATCH = 32
SEQ = 256
DEPTH = 4
DEC_BATCH = 8
DEC_SEQ = 2048
PAST_LEN = 256

GRID_W = 64
N_MIXERS = 2
EPS = 1e-6
RET_HEADS = 4
RET_DK = 256
RET_DV = 512
RET_CHUNK = 128
ROPE_BASE = 10000.0
HY_ORDER = 2
HY_SHORT = 3
HY_BANDS = 16
HY_EMB = 1 + 2 * HY_BANDS
HY_FILTER_W = 64
HY_TARGET = 1e-2
HY_FAST_DECAY = 0.3
HY_SLOW_DECAY = 1.5
HY_MIN_DECAY = math.log(HY_TARGET) / HY_FAST_DECAY
HY_MAX_DECAY = math.log(HY_TARGET) / HY_SLOW_DECAY
D_FF = 2816
N_EXPERTS = 8
TOP_K = 2
D_FF_EXPERT = 3584
N_RET = (DEPTH + 1) // 2
N_HY = DEPTH // 2
N_DENSE = (DEPTH + 1) // 2
N_MOE = DEPTH // 2
STATE_SCALE = 0.1

kernel_name = 'hybrid_retention_hyena_prefix_dit_step'


def rms_norm(x, g):
    xf = x.astype(jnp.float32)
    y = xf * lax.rsqrt(jnp.mean(xf * xf, axis=-1, keepdims=True) + EPS)
    return (y * g.astype(jnp.float32)).astype(x.dtype)


def adaln_params(cond, w_mod, b_mod, dtype):
    m = jax.nn.silu(cond.astype(jnp.float32)) @ w_mod.astype(jnp.float32) + b_mod.astype(jnp.float32)
    m = m.astype(dtype)[:, None, :]
    return jnp.split(m, 6, axis=-1)


def axial_rope(n_tokens):
    rows = n_tokens // GRID_W
    r = jnp.repeat(jnp.arange(rows, dtype=jnp.float32), GRID_W)
    col = jnp.tile(jnp.arange(GRID_W, dtype=jnp.float32), rows)
    quarter = RET_DK // 4
    inv = ROPE_BASE ** (-jnp.arange(quarter, dtype=jnp.float32) / quarter)
    ang = jnp.concatenate([r[:, None] * inv, col[:, None] * inv], axis=-1)
    return jnp.cos(ang), jnp.sin(ang)


def apply_rope(x, cos, sin):
    half = x.shape[-1] // 2
    x1, x2 = x[..., :half], x[..., half:]
    return jnp.concatenate([x1 * cos - x2 * sin, x1 * sin + x2 * cos], axis=-1)


def retention_scan(q, k, v, log_gamma, s0):
    b, h, L, _ = q.shape
    C = RET_CHUNK
    n = L // C
    lg = log_gamma.astype(jnp.float32)[:, None, None]
    pos = jnp.arange(C, dtype=jnp.float32)
    diff = pos[:, None] - pos[None, :]
    dmask = jnp.exp(jnp.where(diff >= 0, lg * diff, -jnp.inf))
    xi = jnp.exp(lg * (pos[:, None] + 1.0))
    kdec = jnp.exp(lg * (C - 1.0 - pos[:, None]))
    cdec = jnp.exp(lg * C)
    qc = q.reshape(b, h, n, C, RET_DK)
    kc = k.reshape(b, h, n, C, RET_DK)
    vc = v.reshape(b, h, n, C, RET_DV)
    scores = jnp.einsum('bhncd,bhnmd->bhncm', qc, kc) * dmask[:, None]
    inner = jnp.einsum('bhncm,bhnmv->bhncv', scores, vc)

    def step(s, blk):
        qn, kn, vn = blk
        cross = jnp.einsum('bhcd,bhdv->bhcv', qn * xi, s)
        s = s * cdec + jnp.einsum('bhcd,bhcv->bhdv', kn * kdec, vn)
        return s, cross

    s_final, cross = lax.scan(step, s0.astype(jnp.float32),
                              (jnp.moveaxis(qc, 2, 0), jnp.moveaxis(kc, 2, 0), jnp.moveaxis(vc, 2, 0)))
    out = inner + jnp.moveaxis(cross, 0, 2)
    return out.reshape(b, h, L, RET_DV), s_final


def retention_mixer(h, w_in, w_out, ln_g, lg_fwd, lg_bwd, s_fwd, s_bwd, rope):
    b, L, _ = h.shape
    qd = RET_HEADS * RET_DK
    vd = RET_HEADS * RET_DV
    proj = (h @ w_in).astype(jnp.float32)
    q, k, v, g = jnp.split(proj, [qd, 2 * qd, 2 * qd + vd], axis=-1)
    q = q.reshape(b, L, RET_HEADS, RET_DK).transpose(0, 2, 1, 3)
    k = k.reshape(b, L, RET_HEADS, RET_DK).transpose(0, 2, 1, 3) * RET_DK ** -0.5
    v = v.reshape(b, L, RET_HEADS, RET_DV).transpose(0, 2, 1, 3)
    if rope is not None:
        q = apply_rope(q, rope[0], rope[1])
        k = apply_rope(k, rope[0], rope[1])
    o_f, s_f = retention_scan(q, k, v, lg_fwd, s_fwd)
    o_b, s_b = retention_scan(q[:, :, ::-1], k[:, :, ::-1], v[:, :, ::-1], lg_bwd, s_bwd)
    o = o_f + o_b[:, :, ::-1]
    mu = jnp.mean(o, axis=-1, keepdims=True)
    var = jnp.mean(jnp.square(o - mu), axis=-1, keepdims=True)
    o = (o - mu) * lax.rsqrt(var + EPS)
    o = o.transpose(0, 2, 1, 3).reshape(b, L, vd) * ln_g.astype(jnp.float32)
    out = (jax.nn.silu(g) * o).astype(h.dtype) @ w_out
    return out, s_f, s_b


def hyena_filters(L, w1, b1, w2, b2, w3, freq):
    f32 = jnp.float32
    t = jnp.arange(L, dtype=f32) / L
    bands = jnp.linspace(1e-4, HY_BANDS - 1, HY_BANDS, dtype=f32)
    ang = 2.0 * math.pi * t[:, None] * bands
    z = jnp.concatenate([t[:, None], jnp.cos(ang), -jnp.sin(ang)], axis=-1)
    freq = freq.astype(f32)
    hd = jnp.sin(freq[0] * (z @ w1.astype(f32) + b1.astype(f32)))
    hd = jnp.sin(freq[1] * (hd @ w2.astype(f32) + b2.astype(f32)))
    filt = (hd @ w3.astype(f32)).reshape(L, 2, HY_ORDER, D_MODEL)
    deltas = jnp.abs(jnp.linspace(HY_MIN_DECAY, HY_MAX_DECAY, D_MODEL, dtype=f32))
    window = jnp.exp(-t[:, None] * deltas)
    return filt * window[:, None, None, :]


def two_sided_kernel(h_fwd, h_bwd):
    kern = jnp.concatenate([h_fwd, jnp.zeros_like(h_fwd[:1]), h_bwd[:0:-1]], axis=0)
    return kern / (jnp.sum(jnp.abs(kern), axis=0, keepdims=True) + EPS)


def fft_long_conv(u, kern, bias):
    L = u.shape[1]
    uf = jnp.fft.rfft(u.astype(jnp.float32), n=2 * L, axis=1)
    kf = jnp.fft.rfft(kern, n=2 * L, axis=0)
    y = jnp.fft.irfft(uf * kf[None], n=2 * L, axis=1)[:, :L]
    return (y + u.astype(jnp.float32) * bias.astype(jnp.float32)).astype(u.dtype)


def short_conv(u, w, b):
    L = u.shape[1]
    pad = HY_SHORT // 2
    up = jnp.pad(u, ((0, 0), (pad, HY_SHORT - 1 - pad), (0, 0)))
    out = b
    for j in range(HY_SHORT):
        out = out + up[:, j:j + L] * w[j]
    return out


def hyena_mixer(h, w_in, b_in, conv_w, conv_b, f_w1, f_b1, f_w2, f_b2, f_w3, f_freq, f_bias, w_out, b_out):
    L = h.shape[1]
    u = short_conv(h @ w_in + b_in, conv_w, conv_b)
    parts = jnp.split(u, HY_ORDER + 1, axis=-1)
    filt = hyena_filters(L, f_w1, f_b1, f_w2, f_b2, f_w3, f_freq)
    z = parts[0]
    for o in range(HY_ORDER):
        kern = two_sided_kernel(filt[:, 0, o], filt[:, 1, o])
        z = parts[o + 1] * fft_long_conv(z, kern, f_bias[o])
    return z @ w_out + b_out


def swiglu(h, wg, wu, wd):
    return (jax.nn.silu(h @ wg) * (h @ wu)) @ wd


def moe_swiglu(h, w_router, wg, wu, wd):
    b, L, d = h.shape
    t = h.reshape(b * L, d)
    logits = (t @ w_router).astype(jnp.float32)
    top_v, top_i = lax.top_k(logits, TOP_K)
    probs = jax.nn.softmax(top_v, axis=-1)
    gates = jnp.sum(jax.nn.one_hot(top_i, N_EXPERTS, dtype=jnp.float32) * probs[..., None], axis=1).astype(h.dtype)
    out = jnp.zeros_like(t)
    for e in range(N_EXPERTS):
        out = out + gates[:, e:e + 1] * swiglu(t, wg[e], wu[e], wd[e])
    return out.reshape(b, L, d)


def trunk(x, cond, s_fwd0, s_bwd0, rope, p):
    bsz = x.shape[0]
    new_f = []
    new_b = []
    for i in range(DEPTH):
        j = i // 2
        sh1, sc1, g1, sh2, sc2, g2 = adaln_params(cond, p['w_mod'][i], p['b_mod'][i], x.dtype)
        hn = rms_norm(x, p['norm1_g'][i]) * (1.0 + sc1) + sh1
        if i % N_MIXERS == 0:
            if s_fwd0 is None:
                zero = jnp.zeros((bsz, RET_HEADS, RET_DK, RET_DV), jnp.float32)
                sf0, sb0 = zero, zero
            else:
                sf0, sb0 = s_fwd0[:, j], s_bwd0[:, j]
            mix, sf, sb = retention_mixer(hn, p['ret_w_in'][j], p['ret_w_out'][j], p['ret_ln_g'][j],
                                          p['ret_log_gamma_fwd'][j], p['ret_log_gamma_bwd'][j], sf0, sb0, rope)
            new_f.append(sf)
            new_b.append(sb)
        else:
            mix = hyena_mixer(hn, p['hy_w_in'][j], p['hy_b_in'][j], p['hy_conv_w'][j], p['hy_conv_b'][j],
                              p['hy_f_w1'][j], p['hy_f_b1'][j], p['hy_f_w2'][j], p['hy_f_b2'][j],
                              p['hy_f_w3'][j], p['hy_f_freq'][j], p['hy_f_bias'][j],
                              p['hy_w_out'][j], p['hy_b_out'][j])
        x = x + g1 * mix
        hn = rms_norm(x, p['norm2_g'][i]) * (1.0 + sc2) + sh2
        if i % 2 == 0:
            ff = swiglu(hn, p['ffn_w_gate'][j], p['ffn_w_up'][j], p['ffn_w_down'][j])
        else:
            ff = moe_swiglu(hn, p['moe_w_router'][j], p['moe_w_gate'][j], p['moe_w_up'][j], p['moe_w_down'][j])
        x = x + g2 * ff
    x = rms_norm(x, p['final_g'])
    return x, jnp.stack(new_f, axis=1), jnp.stack(new_b, axis=1)


def setup_inputs(seed: int = 0) -> dict:
    key = jax.random.key(seed)
    ks = iter(jax.random.split(key, 48))

    def nrm(shape, scale):
        return scale * jax.random.normal(next(ks), shape, dtype=jnp.float32)

    D = D_MODEL
    qd = RET_HEADS * RET_DK
    vd = RET_HEADS * RET_DV
    base_lg = jnp.log(1.0 - 2.0 ** (-5.0 - jnp.arange(RET_HEADS, dtype=jnp.float32)))
    return {
        'x_prompt': nrm((BATCH, SEQ, D), 1.0),
        'x_sample': nrm((DEC_BATCH, DEC_SEQ, D), 1.0),
        'state_ret_fwd': nrm((DEC_BATCH, N_RET, RET_HEADS, RET_DK, RET_DV), STATE_SCALE),
        'state_ret_bwd': nrm((DEC_BATCH, N_RET, RET_HEADS, RET_DK, RET_DV), STATE_SCALE),
        'c': nrm((DEC_BATCH, D), 1.0),
        'c_ctx': nrm((D,), 1.0),
        'w_mod': nrm((DEPTH, D, 6 * D), 0.5 * D ** -0.5),
        'b_mod': nrm((DEPTH, 6 * D), 0.01),
        'norm1_g': 1.0 + nrm((DEPTH, D), 0.02),
        'norm2_g': 1.0 + nrm((DEPTH, D), 0.02),
        'final_g': 1.0 + nrm((D,), 0.02),
        'ret_w_in': nrm((N_RET, D, 2 * qd + 2 * vd), D ** -0.5),
        'ret_w_out': nrm((N_RET, vd, D), vd ** -0.5),
        'ret_ln_g': 1.0 + nrm((N_RET, vd), 0.02),
        'ret_log_gamma_fwd': base_lg * jnp.exp(nrm((N_RET, RET_HEADS), 0.1)),
        'ret_log_gamma_bwd': base_lg * jnp.exp(nrm((N_RET, RET_HEADS), 0.1)),
        'hy_w_in': nrm((N_HY, D, (HY_ORDER + 1) * D), D ** -0.5),
        'hy_b_in': nrm((N_HY, (HY_ORDER + 1) * D), 0.01),
        'hy_conv_w': nrm((N_HY, HY_SHORT, (HY_ORDER + 1) * D), HY_SHORT ** -0.5),
        'hy_conv_b': nrm((N_HY, (HY_ORDER + 1) * D), 0.01),
        'hy_f_w1': nrm((N_HY, HY_EMB, HY_FILTER_W), HY_EMB ** -0.5),
        'hy_f_b1': nrm((N_HY, HY_FILTER_W), 0.1),
        'hy_f_w2': nrm((N_HY, HY_FILTER_W, HY_FILTER_W), HY_FILTER_W ** -0.5),
        'hy_f_b2': nrm((N_HY, HY_FILTER_W), 0.1),
        'hy_f_w3': nrm((N_HY, HY_FILTER_W, 2 * HY_ORDER * D), HY_FILTER_W ** -0.5),
        'hy_f_freq': 1.0 + nrm((N_HY, 2, HY_FILTER_W), 0.01),
        'hy_f_bias': nrm((N_HY, HY_ORDER, D), 0.5),
        'hy_w_out': nrm((N_HY, D, D), D ** -0.5),
        'hy_b_out': nrm((N_HY, D), 0.01),
        'ffn_w_gate': nrm((N_DENSE, D, D_FF), D ** -0.5),
        'ffn_w_up': nrm((N_DENSE, D, D_FF), D ** -0.5),
        'ffn_w_down': nrm((N_DENSE, D_FF, D), D_FF ** -0.5),
        'moe_w_router': nrm((N_MOE, D, N_EXPERTS), D ** -0.5),
        'moe_w_gate': nrm((N_MOE, N_EXPERTS, D, D_FF_EXPERT), D ** -0.5),
        'moe_w_up': nrm((N_MOE, N_EXPERTS, D, D_FF_EXPERT), D ** -0.5),
        'moe_w_down': nrm((N_MOE, N_EXPERTS, D_FF_EXPERT, D), D_FF_EXPERT ** -0.5),
    }


def reference(x_prompt, x_sample, state_ret_fwd, state_ret_bwd, c, c_ctx,
              w_mod, b_mod, norm1_g, norm2_g, final_g,
              ret_w_in, ret_w_out, ret_ln_g, ret_log_gamma_fwd, ret_log_gamma_bwd,
              hy_w_in, hy_b_in, hy_conv_w, hy_conv_b, hy_f_w1, hy_f_b1, hy_f_w2, hy_f_b2,
              hy_f_w3, hy_f_freq, hy_f_bias, hy_w_out, hy_b_out,
              ffn_w_gate, ffn_w_up, ffn_w_down,
              moe_w_router, moe_w_gate, moe_w_up, moe_w_down):
    p = {
        'w_mod': w_mod, 'b_mod': b_mod, 'norm1_g': norm1_g, 'norm2_g': norm2_g, 'final_g': final_g,
        'ret_w_in': ret_w_in, 'ret_w_out': ret_w_out, 'ret_ln_g': ret_ln_g,
        'ret_log_gamma_fwd': ret_log_gamma_fwd, 'ret_log_gamma_bwd': ret_log_gamma_bwd,
        'hy_w_in': hy_w_in, 'hy_b_in': hy_b_in, 'hy_conv_w': hy_conv_w, 'hy_conv_b': hy_conv_b,
        'hy_f_w1': hy_f_w1, 'hy_f_b1': hy_f_b1, 'hy_f_w2': hy_f_w2, 'hy_f_b2': hy_f_b2,
        'hy_f_w3': hy_f_w3, 'hy_f_freq': hy_f_freq, 'hy_f_bias': hy_f_bias,
        'hy_w_out': hy_w_out, 'hy_b_out': hy_b_out,
        'ffn_w_gate': ffn_w_gate, 'ffn_w_up': ffn_w_up, 'ffn_w_down': ffn_w_down,
        'moe_w_router': moe_w_router, 'moe_w_gate': moe_w_gate, 'moe_w_up': moe_w_up, 'moe_w_down': moe_w_down,
    }
    y_prompt, new_state_ret_fwd, new_state_ret_bwd = trunk(x_prompt, c_ctx[None, :], None, None, None, p)
    rope = axial_rope(x_sample.shape[1])
    y_sample, _, _ = trunk(x_sample, c, state_ret_fwd, state_ret_bwd, rope, p)
    return (y_prompt, y_sample, new_state_ret_fwd, new_state_ret_bwd)
```

```python
import contextlib
import math
import numpy as np
import ml_dtypes
import concourse.bass as bass
import concourse.mybir as mybir
from concourse.bass_utils import run_bass_kernel_spmd

F32 = mybir.dt.float32
BF16 = mybir.dt.bfloat16
AF = mybir.ActivationFunctionType
ALU = mybir.AluOpType
AX = mybir.AxisListType

D = 1024
NCH = 8
DEPTH = 4
TP = 1024
TS = 2048
EPS = 1e-6
D_FF = 2816
NE = 8
D_FFE = 3584
RH, RDK, RDV = 4, 256, 512
EPOCH = 60000


class Buf:
    __slots__ = ("name", "w", "r")

    def __init__(self, name=""):
        self.name = name
        self.w = None
        self.r = {}


class KB:
    def __init__(self, nc):
        self.nc = nc
        self.E = {"pe": nc.tensor, "act": nc.scalar, "dve": nc.vector, "pool": nc.gpsimd, "sp": nc.sync}
        self.cnt = {e: 0 for e in ("pe", "act", "dve", "pool")}
        self.esem = {e: [] for e in self.cnt}
        self.waited = {e: {} for e in self.E}
        self.semobj = {}
        self.nsem = 0
        self.dq = {"sp": [], "pool": []}
        self.dq_i = {"sp": 0, "pool": 0}
        self.NDQ = 12
        self.last_tok = {}
        self.all_dma_toks = {}

    def _newsem(self, name):
        s = self.nc.alloc_semaphore(f"{name}_{self.nsem}")
        self.nsem += 1
        sid = self.nsem
        self.semobj[sid] = s
        return sid

    def wait(self, e, sid, val):
        if self.waited[e].get(sid, 0) >= val:
            return
        self.E[e].wait_ge(self.semobj[sid], val)
        self.waited[e][sid] = val

    def _deps(self, e, reads, writes):
        deps = {}

        def add(t):
            if t is None:
                return
            if deps.get(t[0], 0) < t[1]:
                deps[t[0]] = t[1]
        for b in reads:
            add(b.w)
        for b in writes:
            add(b.w)
            for s, v in b.r.items():
                add((s, v))
        own = set(self.esem[e]) if e in self.esem else set()
        for s, v in deps.items():
            if e == "pe" and s in own:
                continue
            self.wait(e, s, v)

    def _record(self, tok, reads, writes):
        for b in reads:
            if b.r.get(tok[0], 0) < tok[1]:
                b.r[tok[0]] = tok[1]
        for b in writes:
            b.w = tok
            b.r = {}

    def op(self, e, fn, reads=(), writes=()):
        self._deps(e, reads, writes)
        ins = fn(self.E[e])
        n = self.cnt[e]
        ep, val = n // EPOCH, n % EPOCH + 1
        while len(self.esem[e]) <= ep:
            self.esem[e].append(self._newsem(e))
        sid = self.esem[e][ep]
        ins.then_inc(self.semobj[sid], 1)
        self.cnt[e] = n + 1
        tok = (sid, val)
        self.last_tok[e] = tok
        self._record(tok, reads, writes)
        return tok

    def dma(self, q, out, in_, reads=(), writes=()):
        lst = self.dq[q]
        i = self.dq_i[q]
        self.dq_i[q] = i + 1
        k = i % self.NDQ
        if len(lst) <= k:
            lst.append([self._newsem("d" + q), 0])
        if lst[k][1] >= 4000:
            lst[k] = [self._newsem("d" + q), 0]
        slot = lst[k]
        if slot[1] > 0:
            self.wait(q, slot[0], 16 * slot[1])
        self._deps(q, reads, writes)
        ins = self.E[q].dma_start(out=out, in_=in_)
        slot[1] += 1
        ins.then_inc(self.semobj[slot[0]], 16)
        tok = (slot[0], 16 * slot[1])
        self.all_dma_toks[slot[0]] = tok[1]
        self._record(tok, reads, writes)
        return tok

    def barrier(self):
        toks = list(self.last_tok.values()) + [(s, v) for s, v in self.all_dma_toks.items()]
        for e in self.E:
            own = set(self.esem[e]) if e in self.esem else set()
            for s, v in toks:
                if s in own:
                    continue
                self.wait(e, s, v)


def fm(v):
    v = np.asarray(v, dtype=np.float32)
    n = v.shape[-1] // 128
    r = v.reshape(v.shape[:-1] + (n, 128))
    return np.ascontiguousarray(np.moveaxis(r, -1, 0))


def kmaj(w):
    K, Fd = w.shape
    return np.ascontiguousarray(w.reshape(K // 128, 128, Fd).transpose(1, 0, 2))


class PV:
    def __init__(self):
        self.cols = []
        self.off = {}
        self.n = 0

    def add(self, name, arr):
        arr = np.asarray(arr, dtype=np.float32).reshape(128, -1)
        self.off[name] = (self.n, arr.shape[1])
        self.cols.append(arr)
        self.n += arr.shape[1]

    def build(self):
        return np.ascontiguousarray(np.concatenate(self.cols, axis=1))


def pv_layout():
    off = {}
    n = 0
    for name, w in (("b_mod", DEPTH * 48), ("norm1_g", DEPTH * 8), ("norm2_g", DEPTH * 8), ("final_g", 8),
                    ("ret_ln_g", 32), ("lg", 16), ("hy_b_in", 48), ("hy_cw", 144), ("hy_cb", 48),
                    ("hy_fb", 32), ("hy_b_out", 16)):
        off[name] = (n, w)
        n += w
    return off, n


def host_prep(inp, core):
    m = {}
    xp = inp["x_prompt"][4 * core:4 * core + 4].reshape(TP, D)
    xs = inp["x_sample"][core].reshape(TS, D)
    m["xT_p"] = np.ascontiguousarray(xp.T.reshape(NCH, 128, TP).transpose(1, 0, 2))
    m["xT_s"] = np.ascontiguousarray(xs.T.reshape(NCH, 128, TS).transpose(1, 0, 2))
    for nm, key in (("srf", "state_ret_fwd"), ("srb", "state_ret_bwd")):
        m[nm] = np.ascontiguousarray(inp[key][core].reshape(2, RH, 2, 128, RDV).transpose(0, 1, 3, 2, 4))
    cond = np.stack([inp["c_ctx"], inp["c"][core]], axis=-1)
    m["condT"] = np.ascontiguousarray(cond.reshape(NCH, 128, 2).transpose(1, 0, 2))
    return m


def host_prep_shared(inp, with_ffn=True):
    m = {}
    pv = PV()
    pv.add("b_mod", fm(inp["b_mod"]))
    pv.add("norm1_g", fm(inp["norm1_g"]))
    pv.add("norm2_g", fm(inp["norm2_g"]))
    pv.add("final_g", fm(inp["final_g"]))
    pv.add("ret_ln_g", fm(inp["ret_ln_g"]))
    lg = np.stack([inp["ret_log_gamma_fwd"], inp["ret_log_gamma_bwd"]], axis=-1)
    pv.add("lg", np.broadcast_to(lg.reshape(1, 16), (128, 16)))
    pv.add("hy_b_in", fm(inp["hy_b_in"]))
    pv.add("hy_cw", fm(inp["hy_conv_w"]))
    pv.add("hy_cb", fm(inp["hy_conv_b"]))
    pv.add("hy_fb", fm(inp["hy_f_bias"]))
    pv.add("hy_b_out", fm(inp["hy_b_out"]))
    m["pvec"] = pv.build()
    m["hy_wi"] = np.ascontiguousarray(
        inp["hy_w_in"].reshape(2, NCH, 128, 3, 8, 128).transpose(0, 4, 3, 2, 1, 5))
    m["hy_wo"] = np.ascontiguousarray(inp["hy_w_out"].reshape(2, 8, 128, D))
    m["hy_w1"] = np.ascontiguousarray(inp["hy_f_w1"])
    m["hy_w2"] = np.ascontiguousarray(inp["hy_f_w2"])
    m["hy_w3"] = np.ascontiguousarray(inp["hy_f_w3"])
    m["hyf"] = np.ascontiguousarray(np.stack([inp["hy_f_b1"], inp["hy_f_b2"], inp["hy_f_freq"][:, 0], inp["hy_f_freq"][:, 1]], -1))
    for L, nm in ((256, "p"), (2048, "s")):
        t = np.arange(L, dtype=np.float32) / np.float32(L)
        bands = np.linspace(1e-4, 15.0, 16, dtype=np.float32)
        ang = (2.0 * math.pi) * t[:, None] * bands
        z = np.concatenate([t[:, None], np.cos(ang), -np.sin(ang)], -1)
        m["zf_" + nm] = np.ascontiguousarray(z.T.astype(np.float32))
        mn, mx = math.log(1e-2) / 0.3, math.log(1e-2) / 1.5
        deltas = np.abs(np.linspace(mn, mx, D, dtype=np.float32))
        wn = np.exp(-t[:, None] * deltas).astype(np.float32)
        m["win_" + nm] = np.ascontiguousarray(wn.reshape(L // 128, 128, NCH, 128).transpose(2, 1, 0, 3))
        N2 = 2 * L
        tt_ = np.arange(L, dtype=np.float64)
        om = 2.0 * math.pi * (np.arange(L, dtype=np.float64) + 0.5) / N2
        ph_ = tt_[:, None] * om[None, :]
        nq = L // 128
        def lay_f(a):
            return a.reshape(nq, 128, nq, 128).transpose(2, 1, 0, 3)
        m["dft_" + nm] = np.ascontiguousarray(np.stack([lay_f(np.cos(ph_)), lay_f(np.sin(ph_))]).astype(np.float32))
        def lay_g(a):
            return (a.T * (2.0 / N2)).reshape(nq, 128, L)
        m["idft_" + nm] = np.ascontiguousarray(np.stack([lay_g(np.cos(ph_)), lay_g(np.sin(ph_))]).astype(np.float32))
    qd, vd = RH * RDK, RH * RDV
    wi = inp["ret_w_in"]
    def units(w):
        n = w.shape[1] // 128
        return w.reshape(NCH, 128, n, 128).transpose(2, 1, 0, 3)
    wq = np.stack([np.stack([np.concatenate([
        units(wi[l][:, h * RDK:(h + 1) * RDK]),
        units(wi[l][:, qd + h * RDK:qd + (h + 1) * RDK]),
        units(wi[l][:, 2 * qd + vd + h * RDV:2 * qd + vd + (h + 1) * RDV])], axis=0)
        for h in range(RH)]) for l in range(2)])
    m["ret_wqkg"] = np.ascontiguousarray(wq)
    m["ret_wv"] = np.ascontiguousarray(np.stack([np.stack([
        wi[l][:, 2 * qd + h * RDV:2 * qd + (h + 1) * RDV].reshape(NCH, 128, RDV).transpose(1, 0, 2)
        for h in range(RH)]) for l in range(2)]))
    m["ret_wo"] = np.ascontiguousarray(inp["ret_w_out"].reshape(2, 16, 128, D))
    ar = np.arange(128, dtype=np.float32)
    diff = ar[None, :] - ar[:, None]
    rc = np.concatenate([diff, (diff >= 0).astype(np.float32), (diff <= 0).astype(np.float32),
                         np.broadcast_to(ar[None, :] + 1.0, (128, 128)), np.broadcast_to(128.0 - ar[None, :], (128, 128)),
                         ar[:, None], 127.0 - ar[:, None], np.full((128, 1), 128.0, np.float32)], axis=1)
    m["rc"] = np.ascontiguousarray(rc.astype(np.float32))
    rows = TS // 64
    r = np.repeat(np.arange(rows, dtype=np.float32), 64)
    col = np.tile(np.arange(64, dtype=np.float32), rows)
    inv = (10000.0 ** (-np.arange(64, dtype=np.float32) / 64)).astype(np.float32)
    ang = np.concatenate([r[:, None] * inv, col[:, None] * inv], axis=-1)
    m["rope_cos"] = np.ascontiguousarray(np.cos(ang).T.astype(np.float32))
    m["rope_sin"] = np.ascontiguousarray(np.sin(ang).T.astype(np.float32))
    m["w_mod"] = np.ascontiguousarray(
        inp["w_mod"].reshape(DEPTH, NCH, 128, 6, 1024).transpose(0, 3, 2, 1, 4))
    def gu(wg, wu, nj):
        a = wg.reshape(NCH, 128, nj, 128).transpose(2, 1, 0, 3)
        b = wu.reshape(NCH, 128, nj, 128).transpose(2, 1, 0, 3)
        return np.ascontiguousarray(np.stack([a, b], axis=2))
    if with_ffn:
        m["ffn_gu"] = np.stack([gu(inp["ffn_w_gate"][l], inp["ffn_w_up"][l], 22) for l in range(2)])
        m["ffn_d"] = np.ascontiguousarray(inp["ffn_w_down"].reshape(2, 22, 128, D))
        m["moe_gu"] = np.stack([np.stack([gu(inp["moe_w_gate"][l, e], inp["moe_w_up"][l, e], 28)
                                          for e in range(NE)]) for l in range(2)])
        m["moe_d"] = np.ascontiguousarray(inp["moe_w_down"].reshape(2, NE, 28, 128, D))
    m["moe_r"] = np.ascontiguousarray(
        inp["moe_w_router"].reshape(2, NCH, 128, NE).transpose(0, 2, 1, 3))
    m["ident"] = np.eye(128, dtype=np.float32)
    return m


def build_program(shapes, cfg):
    nc = bass.Bass("TRN2", target_bir_lowering=False)
    kb = KB(nc)
    dr = {}
    for name, (shp, kind) in shapes.items():
        dt_ = F32
        dr[name] = nc.dram_tensor(name, list(shp), dt_, kind=kind).ap()
    pvo, pvn = pv_layout()

    es = contextlib.ExitStack()
    with es:
        uid = [0]

        def sb(name, shape, dt, st=None):
            uid[0] += 1
            return (st or es).enter_context(nc.sbuf_tensor(f"{name}_{uid[0]}", list(shape), dt))

        Xh = [None]
        HN = sb("HN", [128, NCH, TS], BF16)
        PVEC = sb("PVEC", [128, pvn], F32)
        MOD = sb("MOD", [128, DEPTH, 48, 2], F32)
        AB = sb("AB", [128, 8, NCH], F32)
        IDF = sb("IDF", [128, 128], F32)
        IDB = sb("IDB", [128, 128], BF16)
        ONESB = sb("ONESB", [128, 128], BF16)
        EPSC = sb("EPSC", [128, 1], F32)
        PS = [es.enter_context(nc.psum_tensor(f"ps{i}", [128, 512], F32)) for i in range(8)]
        bPS = [Buf(f"ps{i}") for i in range(8)]
        bX = [[Buf() for _ in range(4)] for _ in range(NCH)]
        bHN = [Buf() for _ in range(4)]
        bC = Buf("consts")
        bMOD = Buf("mod")
        bAB = Buf("ab")
        ps_rr = [0]

        ps_n = [8]

        def next_ps():
            i = ps_rr[0] % ps_n[0]
            ps_rr[0] += 1
            return i

        kb.dma("sp", PVEC[:], dr["pvec"], writes=[bC])
        kb.dma("sp", IDF[:], dr["ident"], writes=[bC])
        kb.op("dve", lambda e: e.tensor_copy(out=IDB[:], in_=IDF[:]), reads=[bC], writes=[bC])
        kb.op("dve", lambda e: e.memset(ONESB[:], 1.0), writes=[bC])
        kb.op("dve", lambda e: e.memset(EPSC[:], EPS), writes=[bC])

        def pvc(name, a, b):
            o, w = pvo[name]
            return PVEC[:, o + a:o + b]

        with contextlib.ExitStack() as ph:
            CT = sb("CT", [128, NCH, 2], F32, ph)
            CTB = sb("CTB", [128, NCH, 2], BF16, ph)
            WM = [sb(f"WM{i}", [128, NCH, 1024], BF16, ph) for i in range(2)]
            bWM = [Buf(), Buf()]
            bCT = Buf()
            kb.dma("sp", CT[:], dr["condT"], writes=[bCT])
            kb.op("act", lambda e: e.activation(out=CTB[:], in_=CT[:], func=AF.Silu), reads=[bCT], writes=[bCT])
            it = 0
            for l in range(DEPTH):
                for g in range(6):
                    s = it % 2
                    it += 1
                    kb.dma("pool", WM[s][:], dr["w_mod"][l, g], writes=[bWM[s]])
                    pi = next_ps()
                    for fc in range(8):
                        for kc in range(NCH):
                            kb.op("pe", lambda e, fc=fc, kc=kc, s=s, pi=pi: e.matmul(
                                PS[pi][:, fc * 2:fc * 2 + 2], WM[s][:, kc, fc * 128:(fc + 1) * 128], CTB[:, kc, :],
                                start=(kc == 0), stop=(kc == NCH - 1)),
                                reads=[bWM[s], bCT], writes=[bPS[pi]])
                    o, _ = pvo["b_mod"]
                    bm = PVEC[:, o + l * 48 + g * 8:o + l * 48 + g * 8 + 8]
                    kb.op("dve", lambda e, pi=pi, l=l, g=g, bm=bm: e.tensor_tensor(
                        out=MOD[:, l, g * 8:(g + 1) * 8, :],
                        in0=PS[pi][:, 0:16].rearrange("p (f j) -> p f j", j=2),
                        in1=bm.unsqueeze(2).to_broadcast([128, 8, 2]), op=ALU.add),
                        reads=[bPS[pi], bC], writes=[bMOD])
            kb.barrier()

        def set_ab(l, j):
            def mk(e):
                return None
            for half, nname in ((0, "norm1_g"), (1, "norm2_g")):
                sh = MOD[:, l, (3 * half + 0) * 8:(3 * half + 1) * 8, j]
                sc = MOD[:, l, (3 * half + 1) * 8:(3 * half + 2) * 8, j]
                gg = MOD[:, l, (3 * half + 2) * 8:(3 * half + 3) * 8, j]
                ng = pvc(nname, l * 8, l * 8 + 8)
                kb.op("dve", lambda e, sc=sc, ng=ng, half=half: e.scalar_tensor_tensor(
                    out=AB[:, 3 * half + 0, :], in0=sc, scalar=1.0, in1=ng, op0=ALU.add, op1=ALU.mult),
                    reads=[bMOD, bC], writes=[bAB])
                kb.op("dve", lambda e, sh=sh, half=half: e.tensor_copy(out=AB[:, 3 * half + 1, :], in_=sh),
                      reads=[bMOD], writes=[bAB])
                kb.op("dve", lambda e, gg=gg, half=half: e.tensor_copy(out=AB[:, 3 * half + 2, :], in_=gg),
                      reads=[bMOD], writes=[bAB])

        def norm_phase(T, a_idx, b_idx, router=None):
            nt = T // 512
            with contextlib.ExitStack() as ph:
                SQ = [sb(f"SQ{i}", [128, NCH, 512], BF16, ph) for i in range(2)]
                RS = [sb(f"RS{i}", [128, 512], F32, ph) for i in range(2)]
                TM = [sb(f"TM{i}", [128, NCH, 512], F32, ph) for i in range(2)]
                bSQ = [Buf(), Buf()]
                bRS = [Buf(), Buf()]
                bTM = [Buf(), Buf()]
                if router is not None:
                    H32 = [sb(f"H32{i}", [128, NCH, 512], F32, ph) for i in range(2)]
                    bH32 = [Buf(), Buf()]
                    LG = sb("LG", [128, 8], F32, ph)
                    MX = sb("MX", [128, 8], F32, ph)
                    NM = sb("NM", [128, 1], F32, ph)
                    MK = sb("MK", [128, 8], F32, ph)
                    EX = sb("EX", [128, 8], F32, ph)
                    DN = sb("DN", [128, 1], F32, ph)
                    bR = Buf()
                for tt in range(nt):
                    s = tt % 2
                    ts = slice(tt * 512, (tt + 1) * 512)
                    xb = [bX[c][tt] for c in range(NCH)]
                    kb.op("act", lambda e, s=s, ts=ts: e.activation(out=SQ[s][:], in_=Xh[0][:, :, ts], func=AF.Square),
                          reads=xb, writes=[bSQ[s]])
                    pi = next_ps()
                    for c in range(NCH):
                        kb.op("pe", lambda e, c=c, s=s, pi=pi: e.matmul(
                            PS[pi][:, :], ONESB[:, :], SQ[s][:, c, :], start=(c == 0), stop=(c == NCH - 1)),
                            reads=[bSQ[s], bC], writes=[bPS[pi]])
                    kb.op("act", lambda e, s=s, pi=pi: e.activation(
                        out=RS[s][:], in_=PS[pi][:, :], func=AF.Sqrt, bias=EPSC[:, 0:1], scale=1.0 / D),
                        reads=[bPS[pi], bC], writes=[bRS[s]])
                    kb.op("dve", lambda e, s=s: e.reciprocal(out=RS[s][:], in_=RS[s][:]),
                        reads=[bRS[s]], writes=[bRS[s]])
                    kb.op("dve", lambda e, s=s, ts=ts: e.tensor_tensor(
                        out=TM[s][:], in0=Xh[0][:, :, ts], in1=RS[s][:].unsqueeze(1).to_broadcast([128, NCH, 512]),
                        op=ALU.mult), reads=xb + [bRS[s]], writes=[bTM[s]])
                    for c in range(NCH):
                        if router is None:
                            kb.op("act", lambda e, c=c, s=s, ts=ts: e.activation(
                                out=HN[:, c, ts], in_=TM[s][:, c, :], func=AF.Identity,
                                scale=AB[:, a_idx, c:c + 1], bias=AB[:, b_idx, c:c + 1]),
                                reads=[bTM[s], bAB], writes=[bHN[tt]])
                        else:
                            kb.op("act", lambda e, c=c, s=s: e.activation(
                                out=H32[s][:, c, :], in_=TM[s][:, c, :], func=AF.Identity,
                                scale=AB[:, a_idx, c:c + 1], bias=AB[:, b_idx, c:c + 1]),
                                reads=[bTM[s], bAB], writes=[bH32[s]])
                    if router is not None:
                        WR, GT, bGT, bWR = router
                        kb.op("pool", lambda e, s=s, ts=ts: e.tensor_copy(out=HN[:, :, ts], in_=H32[s][:]),
                              reads=[bH32[s]], writes=[bHN[tt]])
                        for q in range(4):
                            ch = tt * 4 + q
                            pi = next_ps()
                            for kc in range(NCH):
                                kb.op("pe", lambda e, kc=kc, s=s, q=q, pi=pi: e.matmul(
                                    PS[pi][:, 0:8], H32[s][:, kc, q * 128:(q + 1) * 128], WR[:, kc, :],
                                    start=(kc == 0), stop=(kc == NCH - 1)),
                                    reads=[bH32[s], bWR], writes=[bPS[pi]])
                            kb.op("dve", lambda e, pi=pi: e.tensor_copy(out=LG[:], in_=PS[pi][:, 0:8]),
                                  reads=[bPS[pi]], writes=[bR])
                            kb.op("dve", lambda e: e.max(out=MX[:], in_=LG[:]), reads=[bR], writes=[bR])
                            kb.op("dve", lambda e: e.tensor_scalar_mul(out=NM[:], in0=MX[:, 0:1], scalar1=-1.0),
                                  reads=[bR], writes=[bR])
                            kb.op("dve", lambda e: e.tensor_scalar(
                                out=MK[:], in0=LG[:], scalar1=MX[:, 1:2], scalar2=None, op0=ALU.is_ge),
                                reads=[bR], writes=[bR])
                            kb.op("act", lambda e: e.activation(out=EX[:], in_=LG[:], func=AF.Exp, bias=NM[:, 0:1], scale=1.0),
                                  reads=[bR], writes=[bR])
                            kb.op("dve", lambda e: e.tensor_tensor(out=EX[:], in0=EX[:], in1=MK[:], op=ALU.mult),
                                  reads=[bR], writes=[bR])
                            kb.op("dve", lambda e: e.reduce_sum(out=DN[:], in_=EX[:], axis=AX.X),
                                  reads=[bR], writes=[bR])
                            kb.op("dve", lambda e: e.reciprocal(out=DN[:], in_=DN[:]), reads=[bR], writes=[bR])
                            kb.op("dve", lambda e, ch=ch: e.tensor_scalar_mul(out=GT[:, ch, :], in0=EX[:], scalar1=DN[:, 0:1]),
                                  reads=[bR], writes=[bGT])
                kb.barrier()

        def glu_phase(T, gu_src, d_src, njs, G, g_idx, ph, gate=None):
            nt = T // 512
            RA, bRA, RB, bRB, HP, bHP, SG, bSG, T2, bT2, st = ph
            for g0 in range(0, njs, G):
                js = list(range(g0, min(njs, g0 + G)))
                for j in js:
                    sa = st["a"] % len(RA)
                    st["a"] += 1
                    kb.dma("pool", RA[sa][:], gu_src[j], writes=[bRA[sa]])
                    for tt in range(nt):
                        ts = slice(tt * 512, (tt + 1) * 512)
                        pg, pu = next_ps(), next_ps()
                        for (pi, w) in ((pg, 0), (pu, 1)):
                            for kc in range(NCH):
                                kb.op("pe", lambda e, pi=pi, w=w, kc=kc, sa=sa, ts=ts: e.matmul(
                                    PS[pi][:, :], RA[sa][:, w, kc, :], HN[:, kc, ts],
                                    start=(kc == 0), stop=(kc == NCH - 1)),
                                    reads=[bRA[sa], bHN[tt]], writes=[bPS[pi]])
                        ss = st["s"] % 2
                        st["s"] += 1
                        kb.op("act", lambda e, ss=ss, pg=pg: e.activation(out=SG[ss][:], in_=PS[pg][:, :], func=AF.Silu),
                              reads=[bPS[pg]], writes=[bSG[ss]])
                        jj = j - g0
                        if gate is None:
                            kb.op("dve", lambda e, ss=ss, pu=pu, jj=jj, ts=ts: e.tensor_tensor(
                                out=HP[:, jj, ts], in0=PS[pu][:, :], in1=SG[ss][:], op=ALU.mult),
                                reads=[bPS[pu], bSG[ss]], writes=[bHP[jj][tt]])
                        else:
                            GE, bGE = gate
                            kb.op("dve", lambda e, ss=ss, pu=pu, ts=ts: e.tensor_tensor(
                                out=T2[ss][:], in0=PS[pu][:, :], in1=GE[:, ts], op=ALU.mult),
                                reads=[bPS[pu], bGE], writes=[bT2[ss]])
                            kb.op("dve", lambda e, ss=ss, jj=jj, ts=ts: e.tensor_tensor(
                                out=HP[:, jj, ts], in0=T2[ss][:], in1=SG[ss][:], op=ALU.mult),
                                reads=[bT2[ss], bSG[ss]], writes=[bHP[jj][tt]])
                sbs = []
                for j in js:
                    s_b = st["b"] % len(RB)
                    st["b"] += 1
                    kb.dma("pool", RB[s_b][:], d_src[j], writes=[bRB[s_b]])
                    sbs.append(s_b)
                for tt in range(nt):
                    ts = slice(tt * 512, (tt + 1) * 512)
                    for dc in range(NCH):
                        pi = next_ps()
                        for n, j in enumerate(js):
                            jj = j - g0
                            kb.op("pe", lambda e, pi=pi, n=n, jj=jj, dc=dc, ts=ts, s_b=sbs[n]: e.matmul(
                                PS[pi][:, :], RB[s_b][:, dc * 128:(dc + 1) * 128], HP[:, jj, ts],
                                start=(n == 0), stop=(n == len(js) - 1)),
                                reads=[bRB[sbs[n]], bHP[jj][tt]], writes=[bPS[pi]])
                        kb.op("dve", lambda e, pi=pi, dc=dc, ts=ts: e.scalar_tensor_tensor(
                            out=Xh[0][:, dc, ts], in0=PS[pi][:, :], scalar=AB[:, g_idx, dc:dc + 1], in1=Xh[0][:, dc, ts],
                            op0=ALU.mult, op1=ALU.add),
                            reads=[bPS[pi], bAB, bX[dc][tt]], writes=[bX[dc][tt]])

        def glu_alloc(ph, T, G):
            RA = [sb(f"RA{i}", [128, 2, NCH, 128], BF16, ph) for i in range(4)]
            RB = [sb(f"RB{i}", [128, D], BF16, ph) for i in range(G + 3)]
            HP = sb("HP", [128, G, T], BF16, ph)
            SG = [sb(f"SG{i}", [128, 512], F32, ph) for i in range(2)]
            T2 = [sb(f"T2{i}", [128, 512], F32, ph) for i in range(2)]
            return (RA, [Buf() for _ in RA], RB, [Buf() for _ in RB], HP,
                    [[Buf() for _ in range(4)] for _ in range(G)], SG, [Buf(), Buf()], T2, [Buf(), Buf()],
                    {"a": 0, "b": 0, "s": 0})

        def ffn_dense(T, l2):
            with contextlib.ExitStack() as ph:
                t = glu_alloc(ph, T, 11)
                glu_phase(T, dr["ffn_gu"][l2], dr["ffn_d"][l2], 22, 11, 5, t)
                kb.barrier()

        def moe(T, l2, GT, bGT):
            nchk = T // 128
            with contextlib.ExitStack() as ph:
                t = glu_alloc(ph, T, 7)
                GE = [sb(f"GE{i}", [128, T], F32, ph) for i in range(2)]
                bGE = [Buf(), Buf()]
                GX = [sb(f"GX{i}", [128, 128], F32, ph) for i in range(2)]
                bGX = [Buf(), Buf()]
                for ex in range(NE):
                    s = ex % 2
                    for tt in range(T // 512):
                        pi = next_ps()
                        for q in range(4):
                            ch = tt * 4 + q
                            sx = ch % 2
                            kb.op("dve", lambda e, sx=sx, ch=ch, ex=ex: e.tensor_copy(
                                out=GX[sx][:], in_=GT[:, ch, ex:ex + 1].to_broadcast([128, 128])),
                                reads=[bGT], writes=[bGX[sx]])
                            kb.op("pe", lambda e, sx=sx, q=q, pi=pi: e.matmul(
                                PS[pi][:, q * 128:(q + 1) * 128], GX[sx][:], IDF[:], start=True, stop=True),
                                reads=[bGX[sx], bC], writes=[bPS[pi]])
                        kb.op("act", lambda e, s=s, tt=tt, pi=pi: e.copy(out=GE[s][:, tt * 512:(tt + 1) * 512], in_=PS[pi][:, :]),
                              reads=[bPS[pi]], writes=[bGE[s]])
                    glu_phase(T, dr["moe_gu"][l2, ex], dr["moe_d"][l2, ex], 28, 7, 5, t, gate=(GE[s], bGE[s]))
                kb.barrier()

        RC = sb("RC", [128, 643], F32)
        kb.dma("sp", RC[:], dr["rc"], writes=[bC])
        DIFF, MA, MB_, POS1, POSR = (RC[:, i * 128:(i + 1) * 128] for i in range(5))
        PIDX, PREV, C128 = RC[:, 640:641], RC[:, 641:642], RC[:, 642:643]
        NLG = sb("NLG", [128, 16], F32)
        o_lg = pvo["lg"][0]
        LGt = PVEC[:, o_lg:o_lg + 16]
        kb.op("dve", lambda e: e.tensor_scalar_mul(out=NLG[:], in0=LGt, scalar1=-1.0), reads=[bC], writes=[bC])
        XS = nc.dram_tensor("XS", [128, NCH, TS], F32, kind="Internal").ap()
        YTD = nc.dram_tensor("YTD", [128, 16, TS], BF16, kind="Internal").ap()
        bXS, bYTD = Buf(), Buf()
        PSB = [p[:].bitcast(BF16) for p in PS]
        state_toks = []
        dbg_toks = []

        def retention_core(T, L, nseq, j, sample, pname):
            NCk, N, nt = T // 128, L // 128, T // 512
            with contextlib.ExitStack() as ph:
                QF = sb("QF", [128, 2, T], BF16, ph)
                QB = sb("QB", [128, 2, T], BF16, ph)
                KT = sb("KT", [128, 2, T], BF16, ph)
                KF = sb("KF", [128, NCk, 256], BF16, ph)
                KBt = sb("KBt", [128, NCk, 256], BF16, ph)
                V = sb("V", [128, NCk, 512], BF16, ph)
                SG = sb("SG", [128, 4, T], BF16, ph)
                CB = sb("CB", [128, NCk, 512], BF16, ph)
                S32 = [sb(f"S32{i}", [128, 2, 512], F32, ph) for i in range(2)]
                S16 = [sb(f"S16{i}", [128, 2, 512], BF16, ph) for i in range(2)]
                WU = [sb(f"WU{i}", [128, NCH, 128], BF16, ph) for i in range(4)]
                WV = sb("WV", [128, NCH, 512], BF16, ph)
                MT = sb("MT", [128, 128], F32, ph)
                XIF = sb("XIF", [128, 128], F32, ph)
                XIB = sb("XIB", [128, 128], F32, ph)
                E1 = sb("E1", [128, 128], F32, ph)
                E2 = sb("E2", [128, 128], F32, ph)
                KD = sb("KD", [128, 4], F32, ph)
                TT = [sb(f"TT{i}", [128, 512], F32, ph) for i in range(6)]
                ST = [sb(f"ST{i}", [128, 128], BF16, ph) for i in range(2)]
                ON = [sb(f"ON{i}", [128, 512], BF16, ph) for i in range(2)]
                STS = sb("STS", [128, 6], F32, ph)
                MV = sb("MV", [128, 2], F32, ph)
                RSD = sb("RSD", [128, 1], F32, ph)
                NMR = sb("NMR", [128, 1], F32, ph)
                bQF, bQB, bKT, bKF, bKBt, bV, bSG, bCB = (Buf() for _ in range(8))
                bS32, bS16 = [Buf(), Buf()], [Buf(), Buf()]
                bWU, bWV = [Buf() for _ in WU], Buf()
                bK, bTT, bST, bON, bGN = Buf(), [Buf() for _ in TT], [Buf(), Buf()], [Buf(), Buf()], Buf()
                if sample:
                    COS = sb("COS", [128, T], F32, ph)
                    SIN = sb("SIN", [128, T], F32, ph)
                    bRT = Buf()
                    kb.dma("sp", COS[:], dr["rope_cos"], writes=[bRT])
                    kb.dma("sp", SIN[:], dr["rope_sin"], writes=[bRT])
                wu_i = [0]

                def load_unit(h, u):
                    k = wu_i[0] % 4
                    wu_i[0] += 1
                    kb.dma("pool", WU[k][:], dr["ret_wqkg"][j, h, u], writes=[bWU[k]])
                    return k

                def proj_fm(k, tt, pi):
                    ts = slice(tt * 512, (tt + 1) * 512)
                    for kc in range(NCH):
                        kb.op("pe", lambda e, kc=kc: e.matmul(PS[pi][:, :], WU[k][:, kc, :], HN[:, kc, ts],
                                                               start=(kc == 0), stop=(kc == NCH - 1)),
                              reads=[bWU[k], bHN[tt]], writes=[bPS[pi]])

                for h in range(RH):
                    lgf = LGt[:, (j * 4 + h) * 2:(j * 4 + h) * 2 + 1]
                    lgb = LGt[:, (j * 4 + h) * 2 + 1:(j * 4 + h) * 2 + 2]
                    nlgf = NLG[:, (j * 4 + h) * 2:(j * 4 + h) * 2 + 1]
                    nlgb = NLG[:, (j * 4 + h) * 2 + 1:(j * 4 + h) * 2 + 2]
                    kb.op("act", lambda e: e.activation(out=E1[:], in_=DIFF, func=AF.Exp, scale=lgf), reads=[bC], writes=[bK])
                    kb.op("dve", lambda e: e.tensor_tensor(out=E1[:], in0=E1[:], in1=MA, op=ALU.mult), reads=[bK, bC], writes=[bK])
                    kb.op("act", lambda e: e.activation(out=E2[:], in_=DIFF, func=AF.Exp, scale=nlgb), reads=[bC, bK], writes=[bK])
                    kb.op("dve", lambda e: e.tensor_tensor(out=E2[:], in0=E2[:], in1=MB_, op=ALU.mult), reads=[bK, bC], writes=[bK])
                    kb.op("dve", lambda e: e.tensor_tensor(out=E1[:], in0=E1[:], in1=E2[:], op=ALU.add), reads=[bK], writes=[bK])
                    kb.op("act", lambda e: e.activation(out=E2[:], in_=POS1, func=AF.Exp, scale=nlgf), reads=[bC, bK], writes=[bK])
                    kb.op("dve", lambda e: e.tensor_tensor(out=MT[:], in0=E1[:], in1=E2[:], op=ALU.mult), reads=[bK], writes=[bK])
                    kb.op("act", lambda e: e.activation(out=XIF[:], in_=POS1, func=AF.Exp, scale=lgf), reads=[bC, bK], writes=[bK])
                    kb.op("act", lambda e: e.activation(out=XIB[:], in_=POSR, func=AF.Exp, scale=lgb), reads=[bC, bK], writes=[bK])
                    kb.op("act", lambda e: e.activation(out=KD[:, 0:1], in_=PREV, func=AF.Exp, scale=lgf), reads=[bC, bK], writes=[bK])
                    kb.op("act", lambda e: e.activation(out=KD[:, 1:2], in_=PIDX, func=AF.Exp, scale=lgb), reads=[bC, bK], writes=[bK])
                    kb.op("act", lambda e: e.activation(out=KD[:, 2:3], in_=C128, func=AF.Exp, scale=lgf), reads=[bC, bK], writes=[bK])
                    kb.op("act", lambda e: e.activation(out=KD[:, 3:4], in_=C128, func=AF.Exp, scale=lgb), reads=[bC, bK], writes=[bK])
                    for typ in range(2):
                        ka, kb_ = load_unit(h, typ * 2), load_unit(h, typ * 2 + 1)
                        for tt in range(nt):
                            ts = slice(tt * 512, (tt + 1) * 512)
                            pa, pb = next_ps(), next_ps()
                            proj_fm(ka, tt, pa)
                            proj_fm(kb_, tt, pb)
                            A2, B2 = TT[4], TT[5]
                            if sample:
                                kb.op("dve", lambda e: e.tensor_tensor(out=TT[0][:], in0=PS[pa][:, :], in1=COS[:, ts], op=ALU.mult),
                                      reads=[bPS[pa], bRT], writes=[bTT[0]])
                                kb.op("dve", lambda e: e.tensor_tensor(out=TT[1][:], in0=PS[pb][:, :], in1=SIN[:, ts], op=ALU.mult),
                                      reads=[bPS[pb], bRT], writes=[bTT[1]])
                                kb.op("dve", lambda e: e.tensor_tensor(out=TT[2][:], in0=PS[pa][:, :], in1=SIN[:, ts], op=ALU.mult),
                                      reads=[bPS[pa], bRT], writes=[bTT[2]])
                                kb.op("dve", lambda e: e.tensor_tensor(out=TT[3][:], in0=PS[pb][:, :], in1=COS[:, ts], op=ALU.mult),
                                      reads=[bPS[pb], bRT], writes=[bTT[3]])
                                kb.op("pool", lambda e: e.tensor_tensor(out=A2[:], in0=TT[0][:], in1=TT[1][:], op=ALU.subtract),
                                      reads=[bTT[0], bTT[1]], writes=[bTT[4]])
                                kb.op("pool", lambda e: e.tensor_tensor(out=B2[:], in0=TT[2][:], in1=TT[3][:], op=ALU.add),
                                      reads=[bTT[2], bTT[3]], writes=[bTT[5]])
                            else:
                                kb.op("act", lambda e: e.copy(out=A2[:], in_=PS[pa][:, :]), reads=[bPS[pa]], writes=[bTT[4]])
                                kb.op("act", lambda e: e.copy(out=B2[:], in_=PS[pb][:, :]), reads=[bPS[pb]], writes=[bTT[5]])
                            for half, src, bsrc in ((0, A2, bTT[4]), (1, B2, bTT[5])):
                                s3 = src[:, :].rearrange("p (a b) -> p a b", b=128)
                                if typ == 0:
                                    for dst, bdst, xi, eng in ((QF, bQF, XIF, "dve"), (QB, bQB, XIB, "pool")):
                                        kb.op(eng, lambda e, dst=dst, xi=xi, half=half, s3=s3: e.tensor_tensor(
                                            out=dst[:, half, ts].rearrange("p (a b) -> p a b", b=128), in0=s3,
                                            in1=xi[:, :].unsqueeze(1).to_broadcast([128, 4, 128]), op=ALU.mult),
                                            reads=[bsrc, bK], writes=[bdst])
                                else:
                                    kb.op("act", lambda e, half=half, src=src: e.mul(out=KT[:, half, ts], in_=src[:, :], mul=1.0 / 16.0),
                                          reads=[bsrc], writes=[bKT])
                    for u in range(4):
                        k = load_unit(h, 4 + u)
                        for tt in range(nt):
                            ts = slice(tt * 512, (tt + 1) * 512)
                            pi = next_ps()
                            proj_fm(k, tt, pi)
                            kb.op("act", lambda e, u=u, pi=pi, ts=ts: e.activation(out=SG[:, u, ts], in_=PS[pi][:, :], func=AF.Silu),
                                  reads=[bPS[pi]], writes=[bSG])
                    kb.dma("pool", WV[:], dr["ret_wv"][j, h], writes=[bWV])
                    for g in range(NCk):
                        cs = slice(g * 128, (g + 1) * 128)
                        pi = next_ps()
                        for kc in range(NCH):
                            kb.op("pe", lambda e, kc=kc, pi=pi, cs=cs: e.matmul(PS[pi][:, :], HN[:, kc, cs], WV[:, kc, :],
                                                                              start=(kc == 0), stop=(kc == NCH - 1)),
                                  reads=[bWV, bHN[g // 4]], writes=[bPS[pi]])
                        kb.op("act", lambda e, g=g, pi=pi: e.copy(out=V[:, g, :], in_=PS[pi][:, :]), reads=[bPS[pi]], writes=[bV])
                    for g in range(NCk):
                        cs = slice(g * 128, (g + 1) * 128)
                        pi = next_ps()
                        for dd in range(2):
                            kb.op("pe", lambda e, dd=dd, pi=pi, cs=cs: e.transpose(
                                out=PSB[pi][:, dd * 128:(dd + 1) * 128], in_=KT[:, dd, cs], identity=IDB[:]),
                                reads=[bKT, bC], writes=[bPS[pi]])
                        kb.op("act", lambda e, g=g, pi=pi: e.activation(out=KF[:, g, :], in_=PSB[pi][:, 0:256], func=AF.Copy, scale=KD[:, 0:1]),
                              reads=[bPS[pi], bK], writes=[bKF])
                        kb.op("dve", lambda e, g=g, pi=pi: e.tensor_scalar_mul(out=KBt[:, g, :], in0=PSB[pi][:, 0:256], scalar1=KD[:, 1:2]),
                              reads=[bPS[pi], bK, bKF], writes=[bKBt])

                    def state_init(d, s):
                        if sample:
                            kb.dma("sp", S32[d][:], dr["srf" if d == 0 else "srb"][j, h], writes=[bS32[d]])
                        else:
                            kb.op("pool", lambda e: e.memset(S32[d][:], 0.0), writes=[bS32[d]])
                        kb.op("act", lambda e: e.copy(out=S16[d][:], in_=S32[d][:]), reads=[bS32[d]], writes=[bS16[d]])

                    def state_update(d, g, Kt, bKt):
                        for dd in range(2):
                            pd = next_ps()
                            kb.op("pe", lambda e, dd=dd, pd=pd: e.matmul(PS[pd][:, :], Kt[:, g, dd * 128:(dd + 1) * 128], V[:, g, :],
                                                                        start=True, stop=True),
                                  reads=[bKt, bV], writes=[bPS[pd]])
                            kb.op("dve", lambda e, dd=dd, pd=pd: e.scalar_tensor_tensor(
                                out=S32[d][:, dd, :], in0=S32[d][:, dd, :], scalar=KD[:, 2 + d:3 + d], in1=PS[pd][:, :],
                                op0=ALU.mult, op1=ALU.add), reads=[bPS[pd], bS32[d], bK], writes=[bS32[d]])
                        kb.op("act", lambda e: e.copy(out=S16[d][:], in_=S32[d][:]), reads=[bS32[d]], writes=[bS16[d]])

                    def state_out(d, s):
                        if not sample:
                            nm = "nsf" if d == 0 else "nsb"
                            state_toks.append(kb.dma("sp", dr[nm][s, j, h], S32[d][:], reads=[bS32[d]]))

                    for s in range(nseq):
                        state_init(1, s)
                        for n in reversed(range(N)):
                            g = s * N + n
                            cs = slice(g * 128, (g + 1) * 128)
                            pc = next_ps()
                            for dd in range(2):
                                kb.op("pe", lambda e, dd=dd, pc=pc, cs=cs: e.matmul(PS[pc][:, :], QB[:, dd, cs], S16[1][:, dd, :],
                                                                                  start=(dd == 0), stop=(dd == 1)),
                                      reads=[bQB, bS16[1]], writes=[bPS[pc]])
                            kb.op("act", lambda e, g=g, pc=pc: e.copy(out=CB[:, g, :], in_=PS[pc][:, :]), reads=[bPS[pc]], writes=[bCB])
                            state_update(1, g, KBt, bKBt)
                        state_out(1, s)
                        state_init(0, s)
                        for n in range(N):
                            g = s * N + n
                            cs = slice(g * 128, (g + 1) * 128)
                            i2 = g % 2
                            p_s = next_ps()
                            for dd in range(2):
                                kb.op("pe", lambda e, dd=dd, p_s=p_s, cs=cs: e.matmul(PS[p_s][:, 0:128], KT[:, dd, cs], QF[:, dd, cs],
                                                                                    start=(dd == 0), stop=(dd == 1)),
                                      reads=[bKT, bQF], writes=[bPS[p_s]])
                            kb.op("dve", lambda e, i2=i2, p_s=p_s: e.tensor_tensor(out=ST[i2][:], in0=PS[p_s][:, 0:128], in1=MT[:], op=ALU.mult),
                                  reads=[bPS[p_s], bK], writes=[bST[i2]])
                            p_o = next_ps()
                            kb.op("pe", lambda e, i2=i2, p_o=p_o, g=g: e.matmul(PS[p_o][:, :], ST[i2][:], V[:, g, :], start=True, stop=False),
                                  reads=[bST[i2], bV], writes=[bPS[p_o]])
                            for dd in range(2):
                                kb.op("pe", lambda e, dd=dd, p_o=p_o, cs=cs: e.matmul(PS[p_o][:, :], QF[:, dd, cs], S16[0][:, dd, :],
                                                                                    start=False, stop=False),
                                      reads=[bQF, bS16[0]], writes=[bPS[p_o]])
                            kb.op("pe", lambda e, p_o=p_o, g=g: e.matmul(PS[p_o][:, :], IDB[:], CB[:, g, :], start=False, stop=True),
                                  reads=[bCB, bC], writes=[bPS[p_o]])
                            state_update(0, g, KF, bKF)
                            kb.op("dve", lambda e, p_o=p_o: e.bn_stats(out=STS[:], in_=PS[p_o][:, :]), reads=[bPS[p_o]], writes=[bGN])
                            kb.op("dve", lambda e: e.bn_aggr(out=MV[:], in_=STS[:]), reads=[bGN], writes=[bGN])
                            kb.op("act", lambda e: e.activation(out=RSD[:], in_=MV[:, 1:2], func=AF.Sqrt, bias=EPSC[:, 0:1], scale=1.0),
                                  reads=[bGN, bC], writes=[bGN])
                            kb.op("dve", lambda e: e.reciprocal(out=RSD[:], in_=RSD[:]), reads=[bGN], writes=[bGN])
                            kb.op("dve", lambda e: e.scalar_tensor_tensor(out=NMR[:], in0=MV[:, 0:1], scalar=-1.0, in1=RSD[:],
                                                                           op0=ALU.mult, op1=ALU.mult), reads=[bGN], writes=[bGN])
                            kb.op("act", lambda e, i2=i2, p_o=p_o: e.activation(out=ON[i2][:], in_=PS[p_o][:, :], func=AF.Identity,
                                                                                 scale=RSD[:, 0:1], bias=NMR[:, 0:1]),
                                  reads=[bPS[p_o], bGN], writes=[bON[i2]])
                            p_t = next_ps()
                            for vv in range(4):
                                kb.op("pe", lambda e, vv=vv, p_t=p_t, i2=i2: e.transpose(
                                    out=PSB[p_t][:, vv * 128:(vv + 1) * 128], in_=ON[i2][:, vv * 128:(vv + 1) * 128], identity=IDB[:]),
                                    reads=[bON[i2], bC], writes=[bPS[p_t]])
                            kb.op("dve", lambda e, p_t=p_t, cs=cs: e.tensor_tensor(
                                out=SG[:, :, cs], in0=PSB[p_t][:, 0:512].rearrange("p (a b) -> p a b", b=128), in1=SG[:, :, cs], op=ALU.mult),
                                reads=[bPS[p_t], bSG], writes=[bSG])
                        state_out(0, s)
                    kb.dma("sp", YTD[:, h * 4:(h + 1) * 4, 0:T], SG[:, :, :], reads=[bSG], writes=[bYTD])
                kb.barrier()

        def hyena_core(T, L, nseq, j, sample, pname):
            NCk, NQ, nt = T // 128, L // 128, T // 512
            PI = math.pi
            ps_n[0] = 4
            with contextlib.ExitStack() as ph:
                W3 = sb("W3", [64, 4096], F32, ph)
                HD = sb("HD", [64, L], F32, ph)
                HYF = sb("HYF", [64, 4], F32, ph)
                ONESF = sb("ONESF", [128, 128], F32, ph)
                bHY = Buf()
                kb.dma("sp", W3[:], dr["hy_w3"][j], writes=[bHY])
                kb.dma("sp", HYF[:], dr["hyf"][j], writes=[bHY])
                kb.op("dve", lambda e: e.memset(ONESF[:], 1.0), writes=[bHY])
                with contextlib.ExitStack() as p0:
                    ZF = sb("ZF", [33, L], F32, p0)
                    W1 = sb("W1", [33, 64], F32, p0)
                    W2 = sb("W2", [64, 64], F32, p0)
                    H1 = sb("H1", [64, L], F32, p0)
                    AR = [sb(f"AR{i}", [64, 512], F32, p0) for i in range(3)]
                    bAR, bH1 = Buf(), Buf()
                    kb.dma("sp", ZF[:], dr["zf_" + pname], writes=[bHY])
                    kb.dma("sp", W1[:], dr["hy_w1"][j], writes=[bHY])
                    kb.dma("sp", W2[:], dr["hy_w2"][j], writes=[bHY])
                    for lay in range(2):
                        src, Wl, dst, bdst = ((ZF, W1, H1, bH1), (H1, W2, HD, bHY))[lay]
                        bsrc = bHY if lay == 0 else bH1
                        for c0 in range(0, L, 512):
                            w = min(512, L - c0)
                            pi = next_ps()
                            kb.op("pe", lambda e: e.matmul(PS[pi][0:64, 0:w], Wl[:, :], src[:, c0:c0 + w], start=True, stop=True),
                                  reads=[bHY, bsrc], writes=[bPS[pi]])
                            kb.op("dve", lambda e: e.tensor_scalar(out=AR[0][:, 0:w], in0=PS[pi][0:64, 0:w], scalar1=HYF[:, lay:lay + 1],
                                                                    scalar2=HYF[:, 2 + lay:3 + lay], op0=ALU.add, op1=ALU.mult),
                                  reads=[bPS[pi], bHY], writes=[bAR])
                            kb.op("dve", lambda e: e.tensor_scalar(out=AR[1][:, 0:w], in0=AR[0][:, 0:w], scalar1=PI, scalar2=-2.0 * PI,
                                                                    op0=ALU.is_gt, op1=ALU.mult), reads=[bAR], writes=[bAR])
                            kb.op("dve", lambda e: e.tensor_scalar(out=AR[2][:, 0:w], in0=AR[0][:, 0:w], scalar1=-PI, scalar2=2.0 * PI,
                                                                    op0=ALU.is_lt, op1=ALU.mult), reads=[bAR], writes=[bAR])
                            kb.op("dve", lambda e: e.tensor_tensor(out=AR[0][:, 0:w], in0=AR[0][:, 0:w], in1=AR[1][:, 0:w], op=ALU.add),
                                  reads=[bAR], writes=[bAR])
                            kb.op("dve", lambda e: e.tensor_tensor(out=AR[0][:, 0:w], in0=AR[0][:, 0:w], in1=AR[2][:, 0:w], op=ALU.add),
                                  reads=[bAR], writes=[bAR])
                            kb.op("act", lambda e: e.activation(out=dst[:, c0:c0 + w], in_=AR[0][:, 0:w], func=AF.Sin),
                                  reads=[bAR], writes=[bdst])
                    kb.barrier()
                WINC = sb("WINC", [128, NQ, 128], F32, ph)
                FW = [sb(f"FW{i}", [128, 4, 128], F32, ph) for i in range(2)]
                ABS_ = [sb(f"ABS{i}", [128, 4, 128], F32, ph) for i in range(2)]
                HS = sb("HS", [128, 2, NQ, 128], BF16, ph)
                HDF = sb("HDF", [128, 2, NQ, 128], BF16, ph)
                RN = sb("RN", [128, 2, 128], F32, ph)
                P32 = sb("P32", [128, T], F32, ph)
                U32 = sb("U32", [128, T], F32, ph)
                UU = [sb(f"UU{i}", [128, T], BF16, ph) for i in range(3)]
                ZN = [sb(f"ZN{i}", [128, T], BF16, ph) for i in range(2)]
                ZH = [sb(f"ZH{i}", [128, NCk, 256], BF16, ph) for i in range(2)]
                FU = [sb(f"FU{i}", [128, NQ, 128], BF16, ph) for i in range(3)]
                GU = [sb(f"GU{i}", [128, L], BF16, ph) for i in range(3)]
                WI = [sb(f"WI{i}", [128, NCH, 128], BF16, ph) for i in range(3)]
                AK = [sb(f"AK{i}", [128, 2, 128], F32, ph) for i in range(2)]
                PT = [sb(f"PT{i}", [128, 128], F32, ph) for i in range(4)]
                PQ = sb("PQ", [128, NQ, 2, nseq, 128], BF16, ph)
                R32 = [sb(f"R32{i}", [128, 512], F32, ph) for i in range(2)]
                bWIN, bFW, bABS, bHS, bRN, bP32, bU32 = Buf(), [Buf(), Buf()], [Buf(), Buf()], Buf(), Buf(), Buf(), Buf()
                bUU, bZN, bZH = [Buf() for _ in UU], [Buf(), Buf()], [Buf(), Buf()]
                bFU, bGU, bWI = [Buf() for _ in FU], [Buf() for _ in GU], [Buf() for _ in WI]
                bAK, bPT, bPQ, bR32 = [Buf(), Buf()], [Buf() for _ in PT], Buf(), [Buf(), Buf()]
                cnt = {"f": 0, "g": 0, "w": 0}
                o_bi, o_cw, o_cb, o_fb = pvo["hy_b_in"][0], pvo["hy_cw"][0], pvo["hy_cb"][0], pvo["hy_fb"][0]
                W3v = W3[:, :].rearrange("p (a c) -> p a c", a=4)
                hy_stage = cfg.get("hy_stage", 9)
                for cc in range(cfg.get("hy_ncc", NCH) if hy_stage >= 2 else 0):
                    kb.dma("sp", WINC[:], dr["win_" + pname][cc], writes=[bWIN])
                    skp = cfg.get("hy_skip", [])
                    for tc in range(0 if "filt" in skp else NQ):
                        i2 = tc % 2
                        pi = next_ps()
                        kb.op("pe", lambda e: e.matmul(PS[pi][:, :].rearrange("p (a c) -> p a c", a=4), HD[:, tc * 128:(tc + 1) * 128],
                                                        W3v[:, :, cc * 128:(cc + 1) * 128], start=True, stop=True),
                              reads=[bHY], writes=[bPS[pi]])
                        kb.op("dve", lambda e: e.tensor_tensor(out=FW[i2][:], in0=PS[pi][:, :].rearrange("p (a c) -> p a c", a=4),
                                                                in1=WINC[:, tc, :].unsqueeze(1).to_broadcast([128, 4, 128]), op=ALU.mult),
                              reads=[bPS[pi], bWIN], writes=[bFW[i2]])
                        if tc == 0:
                            kb.op("dve", lambda e: e.memset(FW[i2][0:1, 2:4, :], 0.0), reads=[bFW[i2]], writes=[bFW[i2]])
                        kb.op("pool", lambda e: e.tensor_tensor(out=HS[:, :, tc, :], in0=FW[i2][:, 0:2, :], in1=FW[i2][:, 2:4, :], op=ALU.add),
                              reads=[bFW[i2]], writes=[bHS])
                        kb.op("pool", lambda e: e.tensor_tensor(out=HDF[:, :, tc, :], in0=FW[i2][:, 0:2, :], in1=FW[i2][:, 2:4, :], op=ALU.subtract),
                              reads=[bFW[i2]], writes=[bHS])
                        kb.op("act", lambda e: e.activation(out=ABS_[i2][:], in_=FW[i2][:], func=AF.Abs),
                              reads=[bFW[i2]], writes=[bABS[i2]])
                        kb.op("pe", lambda e: e.matmul(PS[4][:, :], ONESF[:, :], ABS_[i2][:].rearrange("p a c -> p (a c)"),
                                                        start=(tc == 0), stop=(tc == NQ - 1)),
                              reads=[bABS[i2], bHY], writes=[bPS[4]])
                    if "filt" not in skp:
                        kb.op("act", lambda e: e.copy(out=RN[:], in_=PS[4][:, 0:256].rearrange("p (a c) -> p a c", a=2)),
                              reads=[bPS[4]], writes=[bRN])
                        kb.op("dve", lambda e: e.tensor_tensor(out=RN[:], in0=RN[:],
                                                                in1=PS[4][:, 256:512].rearrange("p (a c) -> p a c", a=2), op=ALU.add),
                              reads=[bPS[4], bRN], writes=[bRN])
                    else:
                        kb.op("dve", lambda e: e.memset(RN[:], 1.0), writes=[bRN])
                    kb.op("dve", lambda e: e.tensor_scalar_add(out=RN[:], in0=RN[:], scalar1=EPS), reads=[bRN], writes=[bRN])
                    kb.op("dve", lambda e: e.reciprocal(out=RN[:], in_=RN[:]), reads=[bRN], writes=[bRN])
                    for part in range(3 if (hy_stage >= 3 and "proj" not in skp) else 0):
                        col = part * 8 + cc
                        k = cnt["w"] % 3
                        cnt["w"] += 1
                        kb.dma("pool", WI[k][:], dr["hy_wi"][j, cc, part], writes=[bWI[k]])
                        for tt in range(nt):
                            ts = slice(tt * 512, (tt + 1) * 512)
                            pi = next_ps()
                            for kc in range(NCH):
                                kb.op("pe", lambda e: e.matmul(PS[pi][:, :], WI[k][:, kc, :], HN[:, kc, ts], start=(kc == 0), stop=(kc == NCH - 1)),
                                      reads=[bWI[k], bHN[tt]], writes=[bPS[pi]])
                            kb.op("act", lambda e: e.activation(out=P32[:, ts], in_=PS[pi][:, :], func=AF.Identity,
                                                                 bias=PVEC[:, o_bi + j * 24 + col:o_bi + j * 24 + col + 1], scale=1.0),
                                  reads=[bPS[pi], bC], writes=[bP32])
                        cw = [PVEC[:, o_cw + (j * 3 + tap) * 24 + col:o_cw + (j * 3 + tap) * 24 + col + 1] for tap in range(3)]
                        cbias = PVEC[:, o_cb + j * 24 + col:o_cb + j * 24 + col + 1]
                        kb.op("act", lambda e: e.activation(out=U32[:, 0:T], in_=P32[:, 0:T], func=AF.Identity, bias=cbias, scale=cw[1]),
                              reads=[bP32, bC], writes=[bU32])
                        for s in range(nseq):
                            a, b = s * L, (s + 1) * L
                            kb.op("dve", lambda e: e.scalar_tensor_tensor(out=U32[:, a + 1:b], in0=P32[:, a:b - 1], scalar=cw[0],
                                                                           in1=U32[:, a + 1:b], op0=ALU.mult, op1=ALU.add),
                                  reads=[bP32, bU32, bC], writes=[bU32])
                            kb.op("dve", lambda e: e.scalar_tensor_tensor(out=U32[:, a:b - 1], in0=P32[:, a + 1:b], scalar=cw[2],
                                                                           in1=U32[:, a:b - 1], op0=ALU.mult, op1=ALU.add),
                                  reads=[bP32, bU32, bC], writes=[bU32])
                        kb.op("pool", lambda e: e.tensor_copy(out=UU[part][:], in_=U32[:, 0:T]), reads=[bU32], writes=[bUU[part]])
                    zin, bzin = UU[0], bUU[0]
                    for o in range(2 if hy_stage >= 4 else 0):
                        for g in range(0 if "tr" in skp else NCk):
                            pi = next_ps()
                            kb.op("pe", lambda e: e.transpose(out=PSB[pi][:, 0:128], in_=zin[:, g * 128:(g + 1) * 128], identity=IDB[:]),
                                  reads=[bzin, bC], writes=[bPS[pi]])
                            kb.op("act", lambda e: e.copy(out=ZH[0][:, g, 0:128], in_=PSB[pi][:, 0:128]), reads=[bPS[pi]], writes=[bZH[0]])
                            kb.op("dve", lambda e: e.tensor_copy(out=ZH[1][:, g, 0:128], in_=ZH[0][:, g, 0:128]), reads=[bZH[0]], writes=[bZH[1]])
                        for s in range(nseq):
                            kb.op("pool", lambda e: e.tensor_copy(out=ZH[0][:, s * NQ:(s + 1) * NQ, 128:256], in_=HS[:, o, :, :]),
                                  reads=[bHS], writes=[bZH[0]])
                            kb.op("pool", lambda e: e.tensor_copy(out=ZH[1][:, s * NQ:(s + 1) * NQ, 128:256], in_=HDF[:, o, :, :]),
                                  reads=[bHS], writes=[bZH[1]])
                        for kq in range(0 if "fwd" in skp else NQ):
                            for s in range(nseq):
                                for trig in range(2):
                                    kf = cnt["f"] % 3
                                    cnt["f"] += 1
                                    kb.dma("pool", FU[kf][:], dr["dft_" + pname][trig, kq], writes=[bFU[kf]])
                                    pi = next_ps()
                                    for tc in range(NQ):
                                        kb.op("pe", lambda e: e.matmul(PS[pi][:, 0:256], FU[kf][:, tc, :], ZH[trig][:, s * NQ + tc, :],
                                                                        start=(tc == 0), stop=(tc == NQ - 1)),
                                              reads=[bFU[kf], bZH[trig]], writes=[bPS[pi]])
                                    kb.op("act", lambda e: e.copy(out=AK[trig][:].rearrange("p a c -> p (a c)"), in_=PS[pi][:, 0:256]),
                                          reads=[bPS[pi]], writes=[bAK[trig]])
                                    kb.op("dve", lambda e: e.tensor_tensor(out=AK[trig][:, 1, :], in0=AK[trig][:, 1, :], in1=RN[:, o, :], op=ALU.mult),
                                          reads=[bAK[trig], bRN], writes=[bAK[trig]])
                                A_, Kc, B_, Ks = AK[0][:, 0, :], AK[0][:, 1, :], AK[1][:, 0, :], AK[1][:, 1, :]
                                kb.op("dve", lambda e: e.tensor_tensor(out=PT[0][:], in0=A_, in1=Kc, op=ALU.mult), reads=bAK, writes=[bPT[0]])
                                kb.op("pool", lambda e: e.tensor_tensor(out=PT[1][:], in0=B_, in1=Ks, op=ALU.mult), reads=bAK, writes=[bPT[1]])
                                kb.op("dve", lambda e: e.tensor_tensor(out=PT[2][:], in0=B_, in1=Kc, op=ALU.mult), reads=bAK, writes=[bPT[2]])
                                kb.op("pool", lambda e: e.tensor_tensor(out=PT[3][:], in0=A_, in1=Ks, op=ALU.mult), reads=bAK, writes=[bPT[3]])
                                kb.op("dve", lambda e: e.tensor_tensor(out=PQ[:, kq, 0, s, :], in0=PT[0][:], in1=PT[1][:], op=ALU.subtract),
                                      reads=[bPT[0], bPT[1]], writes=[bPQ])
                                kb.op("pool", lambda e: e.tensor_tensor(out=PQ[:, kq, 1, s, :], in0=PT[2][:], in1=PT[3][:], op=ALU.add),
                                      reads=[bPT[2], bPT[3]], writes=[bPQ])
                        if cfg.get("hy_dbg") and cc == 0 and o == 0:
                            dbg_toks.append(kb.dma("sp", dr["dbg_hd"][0:64, 0:L], HD[:, :], reads=[bHY]))
                            dbg_toks.append(kb.dma("sp", dr["dbg_rn"][:, :], RN[:].rearrange("p a c -> p (a c)"), reads=[bRN]))
                            dbg_toks.append(kb.dma("pool", dr["dbg_pq"][:, 0:NQ * 2 * nseq * 128],
                                                   PQ[:].rearrange("p a b c d -> p (a b c d)"), reads=[bPQ]))
                            dbg_toks.append(kb.dma("pool", dr["dbg_zh"][:, 0:NCk * 256],
                                                   ZH[0][:].rearrange("p a b -> p (a b)"), reads=[bZH[0]]))
                        wt = min(512, L)
                        accs = [(s, c0) for s in range(nseq) for c0 in range(0, L, wt)]
                        assert len(accs) == 4
                        if hy_stage < 5:
                            continue
                        for kq in range(NQ):
                            for trig in range(2):
                                kg = cnt["g"] % 3
                                cnt["g"] += 1
                                kb.dma("pool", GU[kg][:], dr["idft_" + pname][trig, kq], writes=[bGU[kg]])
                                for ai, (s, c0) in enumerate(accs):
                                    if cfg.get("hy_nomm") or ai >= cfg.get("hy_nacc", 4):
                                        continue
                                    ab = cfg.get("hy_accbase", 4)
                                    hv = cfg.get("hy_var", 0)
                                    lh = IDB[:] if hv == 1 else PQ[:, kq, trig, s, :]
                                    rh = UU[0][:, c0:c0 + wt] if hv == 2 else GU[kg][:, c0:c0 + wt]
                                    kb.op("pe", lambda e: e.matmul(PS[ab + ai][:, 0:wt], lh, rh,
                                                                    start=(kq == 0 and trig == 0), stop=(kq == NQ - 1 and trig == 1)),
                                          reads=[bPQ, bGU[kg]], writes=[bPS[ab + ai]])
                        fb = PVEC[:, o_fb + (j * 2 + o) * 8 + cc:o_fb + (j * 2 + o) * 8 + cc + 1]
                        if hy_stage < 6:
                            continue
                        for ai, (s, c0) in enumerate(accs):
                            ts = slice(s * L + c0, s * L + c0 + wt)
                            r = ai % 2
                            kb.op("dve", lambda e: e.scalar_tensor_tensor(out=R32[r][:, 0:wt], in0=zin[:, ts], scalar=fb, in1=PS[4 + ai][:, 0:wt],
                                                                           op0=ALU.mult, op1=ALU.add),
                                  reads=[bzin, bPS[4 + ai], bC], writes=[bR32[r]])
                            kb.op("pool", lambda e: e.tensor_tensor(out=ZN[o][:, ts], in0=R32[r][:, 0:wt], in1=UU[o + 1][:, ts], op=ALU.mult),
                                  reads=[bR32[r], bUU[o + 1]], writes=[bZN[o]])
                        zin, bzin = ZN[o], bZN[o]
                    kb.dma("sp", YTD[:, cc, 0:T], ZN[1][:, :], reads=[bZN[1]], writes=[bYTD])
                kb.barrier()
            ps_n[0] = 8

        def outproj(T, w_src, nk, g_idx, scale_cols=None, bias_col=None):
            nt = T // 512
            with contextlib.ExitStack() as ph:
                SRC = sb("SRC", [128, nk, T], BF16, ph)
                WO = sb("WO", [128, nk, D], BF16, ph)
                bSRC, bWO = Buf(), [Buf() for _ in range(nk)]
                GBt = sb("GBt", [128, NCH], F32, ph)
                bGB = Buf()
                if bias_col is not None:
                    kb.op("dve", lambda e: e.tensor_tensor(out=GBt[:], in0=AB[:, g_idx, :], in1=bias_col, op=ALU.mult),
                          reads=[bAB, bC], writes=[bGB])
                kb.dma("sp", SRC[:], YTD[:, 0:nk, 0:T], reads=[bYTD], writes=[bSRC])
                for kc in range(nk):
                    kb.dma("pool", WO[:, kc, :], w_src[kc], writes=[bWO[kc]])
                    if scale_cols is not None:
                        kb.op("dve", lambda e, kc=kc: e.tensor_scalar_mul(out=WO[:, kc, :], in0=WO[:, kc, :], scalar1=scale_cols[:, kc:kc + 1]),
                              reads=[bWO[kc], bC], writes=[bWO[kc]])
                for tt in range(nt):
                    ts = slice(tt * 512, (tt + 1) * 512)
                    for dc in range(NCH):
                        pi = next_ps()
                        for kc in range(nk):
                            kb.op("pe", lambda e, kc=kc, pi=pi, dc=dc, ts=ts: e.matmul(
                                PS[pi][:, :], WO[:, kc, dc * 128:(dc + 1) * 128], SRC[:, kc, ts], start=(kc == 0), stop=(kc == nk - 1)),
                                reads=[bWO[kc], bSRC], writes=[bPS[pi]])
                        kb.op("dve", lambda e, pi=pi, dc=dc, ts=ts: e.scalar_tensor_tensor(
                            out=Xh[0][:, dc, ts], in0=PS[pi][:, :], scalar=AB[:, g_idx, dc:dc + 1], in1=Xh[0][:, dc, ts],
                            op0=ALU.mult, op1=ALU.add),
                            reads=[bPS[pi], bAB, bX[dc][tt]], writes=[bX[dc][tt]])
                        if bias_col is not None:
                            kb.op("dve", lambda e, dc=dc, ts=ts: e.tensor_scalar(
                                out=Xh[0][:, dc, ts], in0=Xh[0][:, dc, ts], scalar1=GBt[:, dc:dc + 1], scalar2=None, op0=ALU.add),
                                reads=[bGB, bX[dc][tt]], writes=[bX[dc][tt]])
                kb.barrier()

        def final_norm(T, pname):
            nt = T // 512
            with contextlib.ExitStack() as ph:
                kb.op("dve", lambda e: e.tensor_copy(out=AB[:, 6, :], in_=pvc("final_g", 0, 8)), reads=[bC], writes=[bAB])
                SQ = [sb(f"FSQ{i}", [128, NCH, 512], BF16, ph) for i in range(2)]
                RS = [sb(f"FRS{i}", [128, 512], F32, ph) for i in range(2)]
                TM = [sb(f"FTM{i}", [128, NCH, 512], F32, ph) for i in range(2)]
                bSQ, bRS, bTM = [Buf(), Buf()], [Buf(), Buf()], [Buf(), Buf()]
                for tt in range(nt):
                    s = tt % 2
                    ts = slice(tt * 512, (tt + 1) * 512)
                    xb = [bX[c][tt] for c in range(NCH)]
                    kb.op("act", lambda e, s=s, ts=ts: e.activation(out=SQ[s][:], in_=Xh[0][:, :, ts], func=AF.Square),
                          reads=xb, writes=[bSQ[s]])
                    pi = next_ps()
                    for c in range(NCH):
                        kb.op("pe", lambda e, c=c, s=s, pi=pi: e.matmul(
                            PS[pi][:, :], ONESB[:, :], SQ[s][:, c, :], start=(c == 0), stop=(c == NCH - 1)),
                            reads=[bSQ[s], bC], writes=[bPS[pi]])
                    kb.op("act", lambda e, s=s, pi=pi: e.activation(
                        out=RS[s][:], in_=PS[pi][:, :], func=AF.Sqrt, bias=EPSC[:, 0:1], scale=1.0 / D),
                        reads=[bPS[pi], bC], writes=[bRS[s]])
                    kb.op("dve", lambda e, s=s: e.reciprocal(out=RS[s][:], in_=RS[s][:]), reads=[bRS[s]], writes=[bRS[s]])
                    kb.op("dve", lambda e, s=s, ts=ts: e.tensor_tensor(
                        out=TM[s][:], in0=Xh[0][:, :, ts], in1=RS[s][:].unsqueeze(1).to_broadcast([128, NCH, 512]),
                        op=ALU.mult), reads=xb + [bRS[s]], writes=[bTM[s]])
                    kb.op("dve", lambda e, s=s: e.tensor_tensor(
                        out=TM[s][:], in0=TM[s][:], in1=AB[:, 6, :].unsqueeze(2).to_broadcast([128, NCH, 512]),
                        op=ALU.mult), reads=[bTM[s], bAB], writes=[bTM[s]])
                    out_toks.append(kb.dma("sp", dr["yT_" + pname][:, :, ts], TM[s][:], reads=[bTM[s]]))
                kb.barrier()

        def load_x(T, src, rd=()):
            nt = T // 512
            for c in range(NCH):
                kb.dma("sp", Xh[0][:, c, 0:T], src[:, c, 0:T], reads=list(rd), writes=[bX[c][tt] for tt in range(nt)])

        def store_x(T):
            nt = T // 512
            for c in range(NCH):
                kb.dma("sp", XS[:, c, 0:T], Xh[0][:, c, 0:T], reads=[bX[c][tt] for tt in range(nt)], writes=[bXS])

        out_toks = []
        do_ffn = cfg.get("ffn", True)
        do_ret = cfg.get("ret", True)
        do_hy = cfg.get("hyena", True)
        passes = cfg.get("passes", ("p", "s"))
        for pname, T, j, L, nseq in (("p", TP, 0, 256, 4), ("s", TS, 1, 2048, 1)):
            if pname not in passes:
                continue
            sample = pname == "s"
            xsrc = dr["xT_" + pname]
            x_in_xs = False
            for l in range(DEPTH):
                l2 = l // 2
                set_ab(l, j)
                has_mixer = (do_ret if l % 2 == 0 else do_hy)
                if has_mixer:
                    with contextlib.ExitStack() as phx:
                        Xh[0] = sb("X", [128, NCH, TS], F32, phx)
                        load_x(T, XS if x_in_xs else xsrc, rd=[bXS] if x_in_xs else [])
                        norm_phase(T, 0, 1)
                    if l % 2 == 0:
                        retention_core(T, L, nseq, l2, sample, pname)
                    else:
                        hyena_core(T, L, nseq, l2, sample, pname)
                with contextlib.ExitStack() as phx:
                    Xh[0] = sb("X", [128, NCH, TS], F32, phx)
                    load_x(T, XS if x_in_xs else xsrc, rd=[bXS] if x_in_xs else [])
                    if has_mixer:
                        if l % 2 == 0:
                            o_ln = pvo["ret_ln_g"][0]
                            outproj(T, dr["ret_wo"][l2], 16, 2, scale_cols=PVEC[:, o_ln + l2 * 16:o_ln + l2 * 16 + 16])
                        else:
                            o_b = pvo["hy_b_out"][0]
                            outproj(T, dr["hy_wo"][l2], 8, 2, bias_col=PVEC[:, o_b + l2 * 8:o_b + l2 * 8 + 8])
                    if do_ffn:
                        if l % 2 == 0:
                            norm_phase(T, 3, 4)
                            ffn_dense(T, l2)
                        else:
                            with contextlib.ExitStack() as ph2:
                                WR = sb("WR", [128, NCH, NE], F32, ph2)
                                GT = sb("GT", [128, TS // 128, NE], F32, ph2)
                                bWR, bGT = Buf(), Buf()
                                kb.dma("sp", WR[:], dr["moe_r"][l2], writes=[bWR])
                                norm_phase(T, 3, 4, router=(WR, GT, bGT, bWR))
                                moe(T, l2, GT, bGT)
                    if l < DEPTH - 1:
                        store_x(T)
                        x_in_xs = True
                        kb.barrier()
                    else:
                        final_norm(T, pname)
        for t in out_toks + state_toks + dbg_toks:
            kb.wait("sp", t[0], t[1])
    return nc


def kernel(**inputs):
    cfg = inputs.pop("_cfg", {})
    inp = {k: np.asarray(v) for k, v in inputs.items()}
    shared = host_prep_shared(inp, cfg.get("ffn", True))
    if not cfg.get("ffn", True):
        shared = {k: v for k, v in shared.items() if not (k.startswith("ffn_") or k.startswith("moe_"))}
    ncores = cfg.get("ncores", 8)
    per_core = [host_prep(inp, i) for i in range(ncores)]
    shapes = {}
    for k, v in {**shared, **per_core[0]}.items():
        shapes[k] = (v.shape, "ExternalInput")
    shapes["yT_p"] = ((128, NCH, TP), "ExternalOutput")
    shapes["yT_s"] = ((128, NCH, TS), "ExternalOutput")
    shapes["nsf"] = ((4, 2, RH, 128, 2, RDV), "ExternalOutput")
    shapes["nsb"] = ((4, 2, RH, 128, 2, RDV), "ExternalOutput")
    if cfg.get("hy_dbg"):
        shapes["dbg_hd"] = ((128, 2048), "ExternalOutput")
        shapes["dbg_rn"] = ((128, 256), "ExternalOutput")
        shapes["dbg_pq"] = ((128, 8192), "ExternalOutput")
        shapes["dbg_zh"] = ((128, 4096), "ExternalOutput")
    nc = build_program(shapes, cfg)
    in_maps = [{**shared, **per_core[i]} for i in range(ncores)]
    res = run_bass_kernel_spmd(nc, in_maps, core_ids=list(range(ncores)))
    yp = np.zeros((32, 256, D), np.float32)
    ys = np.zeros((8, 2048, D), np.float32)
    nsf = np.zeros((32, 2, RH, RDK, RDV), np.float32)
    nsb = np.zeros((32, 2, RH, RDK, RDV), np.float32)
    for i in range(ncores):
        r = res.results[i]
        yp[4 * i:4 * i + 4] = r["yT_p"].transpose(2, 1, 0).reshape(4, 256, D)
        ys[i] = r["yT_s"].transpose(2, 1, 0).reshape(TS, D)
        nsf[4 * i:4 * i + 4] = r["nsf"].transpose(0, 1, 2, 4, 3, 5).reshape(4, 2, RH, RDK, RDV)
        nsb[4 * i:4 * i + 4] = r["nsb"].transpose(0, 1, 2, 4, 3, 5).reshape(4, 2, RH, RDK, RDV)
    return yp, ys, nsf, nsb
```

```python
import contextlib
import math
import numpy as np
import ml_dtypes
import concourse.bass as bass
import concourse.mybir as mybir
from concourse.bass_utils import run_bass_kernel_spmd

F32 = mybir.dt.float32
BF16 = mybir.dt.bfloat16
AF = mybir.ActivationFunctionType
ALU = mybir.AluOpType
AX = mybir.AxisListType

D = 1024
NCH = 8
DEPTH = 4
TP = 1024
TS = 2048
EPS = 1e-6
D_FF = 2816
NE = 8
D_FFE = 3584
RH, RDK, RDV = 4, 256, 512
EPOCH = 60000


class Buf:
    __slots__ = ("name", "w", "r")

    def __init__(self, name=""):
        self.name = name
        self.w = None
        self.r = {}


class KB:
    def __init__(self, nc):
        self.nc = nc
        self.E = {"pe": nc.tensor, "act": nc.scalar, "dve": nc.vector, "pool": nc.gpsimd, "sp": nc.sync}
        self.cnt = {e: 0 for e in ("pe", "act", "dve", "pool")}
        self.esem = {e: [] for e in self.cnt}
        self.waited = {e: {} for e in self.E}
        self.semobj = {}
        self.nsem = 0
        self.dq = {"sp": [], "pool": []}
        self.dq_i = {"sp": 0, "pool": 0}
        self.NDQ = 12
        self.last_tok = {}
        self.all_dma_toks = {}

    def _newsem(self, name):
        s = self.nc.alloc_semaphore(f"{name}_{self.nsem}")
        self.nsem += 1
        sid = self.nsem
        self.semobj[sid] = s
        return sid

    def wait(self, e, sid, val):
        if self.waited[e].get(sid, 0) >= val:
            return
        self.E[e].wait_ge(self.semobj[sid], val)
        self.waited[e][sid] = val

    def _deps(self, e, reads, writes):
        deps = {}

        def add(t):
            if t is None:
                return
            if deps.get(t[0], 0) < t[1]:
                deps[t[0]] = t[1]
        for b in reads:
            add(b.w)
        for b in writes:
            add(b.w)
            for s, v in b.r.items():
                add((s, v))
        own = set(self.esem[e]) if e in self.esem else set()
        for s, v in deps.items():
            if e == "pe" and s in own:
                continue
            self.wait(e, s, v)

    def _record(self, tok, reads, writes):
        for b in reads:
            if b.r.get(tok[0], 0) < tok[1]:
                b.r[tok[0]] = tok[1]
        for b in writes:
            b.w = tok
            b.r = {}

    def op(self, e, fn, reads=(), writes=()):
        self._deps(e, reads, writes)
        ins = fn(self.E[e])
        n = self.cnt[e]
        ep, val = n // EPOCH, n % EPOCH + 1
        while len(self.esem[e]) <= ep:
            self.esem[e].append(self._newsem(e))
        sid = self.esem[e][ep]
        ins.then_inc(self.semobj[sid], 1)
        self.cnt[e] = n + 1
        tok = (sid, val)
        self.last_tok[e] = tok
        self._record(tok, reads, writes)
        return tok

    def dma(self, q, out, in_, reads=(), writes=()):
        lst = self.dq[q]
        i = self.dq_i[q]
        self.dq_i[q] = i + 1
        k = i % self.NDQ
        if len(lst) <= k:
            lst.append([self._newsem("d" + q), 0])
        if lst[k][1] >= 4000:
            lst[k] = [self._newsem("d" + q), 0]
        slot = lst[k]
        if slot[1] > 0:
            self.wait(q, slot[0], 16 * slot[1])
        self._deps(q, reads, writes)
        ins = self.E[q].dma_start(out=out, in_=in_)
        slot[1] += 1
        ins.then_inc(self.semobj[slot[0]], 16)
        tok = (slot[0], 16 * slot[1])
        self.all_dma_toks[slot[0]] = tok[1]
        self._record(tok, reads, writes)
        return tok

    def barrier(self):
        toks = list(self.last_tok.values()) + [(s, v) for s, v in self.all_dma_toks.items()]
        for e in self.E:
            own = set(self.esem[e]) if e in self.esem else set()
            for s, v in toks:
                if s in own:
                    continue
                self.wait(e, s, v)


def fm(v):
    v = np.asarray(v, dtype=np.float32)
    n = v.shape[-1] // 128
    r = v.reshape(v.shape[:-1] + (n, 128))
    return np.ascontiguousarray(np.moveaxis(r, -1, 0))


def kmaj(w):
    K, Fd = w.shape
    return np.ascontiguousarray(w.reshape(K // 128, 128, Fd).transpose(1, 0, 2))


class PV:
    def __init__(self):
        self.cols = []
        self.off = {}
        self.n = 0

    def add(self, name, arr):
        arr = np.asarray(arr, dtype=np.float32).reshape(128, -1)
        self.off[name] = (self.n, arr.shape[1])
        self.cols.append(arr)
        self.n += arr.shape[1]

    def build(self):
        return np.ascontiguousarray(np.concatenate(self.cols, axis=1))


def pv_layout():
    off = {}
    n = 0
    for name, w in (("b_mod", DEPTH * 48), ("norm1_g", DEPTH * 8), ("norm2_g", DEPTH * 8), ("final_g", 8),
                    ("ret_ln_g", 32), ("lg", 16), ("hy_b_in", 48), ("hy_cw", 144), ("hy_cb", 48),
                    ("hy_fb", 32), ("hy_b_out", 16)):
        off[name] = (n, w)
        n += w
    return off, n


def host_prep(inp, core):
    m = {}
    xp = inp["x_prompt"][4 * core:4 * core + 4].reshape(TP, D)
    xs = inp["x_sample"][core].reshape(TS, D)
    m["xT_p"] = np.ascontiguousarray(xp.T.reshape(NCH, 128, TP).transpose(1, 0, 2))
    m["xT_s"] = np.ascontiguousarray(xs.T.reshape(NCH, 128, TS).transpose(1, 0, 2))
    for nm, key in (("srf", "state_ret_fwd"), ("srb", "state_ret_bwd")):
        m[nm] = np.ascontiguousarray(inp[key][core].reshape(2, RH, 2, 128, RDV).transpose(0, 1, 3, 2, 4))
    cond = np.stack([inp["c_ctx"], inp["c"][core]], axis=-1)
    m["condT"] = np.ascontiguousarray(cond.reshape(NCH, 128, 2).transpose(1, 0, 2))
    return m


def host_prep_shared(inp, with_ffn=True):
    m = {}
    pv = PV()
    pv.add("b_mod", fm(inp["b_mod"]))
    pv.add("norm1_g", fm(inp["norm1_g"]))
    pv.add("norm2_g", fm(inp["norm2_g"]))
    pv.add("final_g", fm(inp["final_g"]))
    pv.add("ret_ln_g", fm(inp["ret_ln_g"]))
    lg = np.stack([inp["ret_log_gamma_fwd"], inp["ret_log_gamma_bwd"]], axis=-1)
    pv.add("lg", np.broadcast_to(lg.reshape(1, 16), (128, 16)))
    pv.add("hy_b_in", fm(inp["hy_b_in"]))
    pv.add("hy_cw", fm(inp["hy_conv_w"]))
    pv.add("hy_cb", fm(inp["hy_conv_b"]))
    pv.add("hy_fb", fm(inp["hy_f_bias"]))
    pv.add("hy_b_out", fm(inp["hy_b_out"]))
    m["pvec"] = pv.build()
    m["hy_wi"] = np.ascontiguousarray(
        inp["hy_w_in"].reshape(2, NCH, 128, 3, 8, 128).transpose(0, 4, 3, 2, 1, 5))
    m["hy_wo"] = np.ascontiguousarray(inp["hy_w_out"].reshape(2, 8, 128, D))
    m["hy_w1"] = np.ascontiguousarray(inp["hy_f_w1"])
    m["hy_w2"] = np.ascontiguousarray(inp["hy_f_w2"])
    m["hy_w3"] = np.ascontiguousarray(inp["hy_f_w3"])
    m["hyf"] = np.ascontiguousarray(np.stack([inp["hy_f_b1"], inp["hy_f_b2"], inp["hy_f_freq"][:, 0], inp["hy_f_freq"][:, 1]], -1))
    for L, nm in ((256, "p"), (2048, "s")):
        t = np.arange(L, dtype=np.float32) / np.float32(L)
        bands = np.linspace(1e-4, 15.0, 16, dtype=np.float32)
        ang = (2.0 * math.pi) * t[:, None] * bands
        z = np.concatenate([t[:, None], np.cos(ang), -np.sin(ang)], -1)
        m["zf_" + nm] = np.ascontiguousarray(z.T.astype(np.float32))
        mn, mx = math.log(1e-2) / 0.3, math.log(1e-2) / 1.5
        deltas = np.abs(np.linspace(mn, mx, D, dtype=np.float32))
        wn = np.exp(-t[:, None] * deltas).astype(np.float32)
        m["win_" + nm] = np.ascontiguousarray(wn.reshape(L // 128, 128, NCH, 128).transpose(2, 1, 0, 3))
        N2 = 2 * L
        tt_ = np.arange(L, dtype=np.float64)
        om = 2.0 * math.pi * (np.arange(L, dtype=np.float64) + 0.5) / N2
        ph_ = tt_[:, None] * om[None, :]
        nq = L // 128
        def lay_f(a):
            return a.reshape(nq, 128, nq, 128).transpose(2, 1, 0, 3)
        m["dft_" + nm] = np.ascontiguousarray(np.stack([lay_f(np.cos(ph_)), lay_f(np.sin(ph_))]).astype(ml_dtypes.bfloat16))
        def lay_g(a):
            return (a.T * (2.0 / N2)).reshape(nq, 128, L)
        m["idft_" + nm] = np.ascontiguousarray(np.stack([lay_g(np.cos(ph_)), lay_g(np.sin(ph_))]).astype(ml_dtypes.bfloat16))
    qd, vd = RH * RDK, RH * RDV
    wi = inp["ret_w_in"]
    def units(w):
        n = w.shape[1] // 128
        return w.reshape(NCH, 128, n, 128).transpose(2, 1, 0, 3)
    wq = np.stack([np.stack([np.concatenate([
        units(wi[l][:, h * RDK:(h + 1) * RDK]),
        units(wi[l][:, qd + h * RDK:qd + (h + 1) * RDK]),
        units(wi[l][:, 2 * qd + vd + h * RDV:2 * qd + vd + (h + 1) * RDV])], axis=0)
        for h in range(RH)]) for l in range(2)])
    m["ret_wqkg"] = np.ascontiguousarray(wq)
    m["ret_wv"] = np.ascontiguousarray(np.stack([np.stack([
        wi[l][:, 2 * qd + h * RDV:2 * qd + (h + 1) * RDV].reshape(NCH, 128, RDV).transpose(1, 0, 2)
        for h in range(RH)]) for l in range(2)]))
    m["ret_wo"] = np.ascontiguousarray(inp["ret_w_out"].reshape(2, 16, 128, D))
    ar = np.arange(128, dtype=np.float32)
    diff = ar[None, :] - ar[:, None]
    rc = np.concatenate([diff, (diff >= 0).astype(np.float32), (diff <= 0).astype(np.float32),
                         np.broadcast_to(ar[None, :] + 1.0, (128, 128)), np.broadcast_to(128.0 - ar[None, :], (128, 128)),
                         ar[:, None], 127.0 - ar[:, None], np.full((128, 1), 128.0, np.float32)], axis=1)
    m["rc"] = np.ascontiguousarray(rc.astype(np.float32))
    rows = TS // 64
    r = np.repeat(np.arange(rows, dtype=np.float32), 64)
    col = np.tile(np.arange(64, dtype=np.float32), rows)
    inv = (10000.0 ** (-np.arange(64, dtype=np.float32) / 64)).astype(np.float32)
    ang = np.concatenate([r[:, None] * inv, col[:, None] * inv], axis=-1)
    m["rope_cos"] = np.ascontiguousarray(np.cos(ang).T.astype(np.float32))
    m["rope_sin"] = np.ascontiguousarray(np.sin(ang).T.astype(np.float32))
    m["w_mod"] = np.ascontiguousarray(
        inp["w_mod"].reshape(DEPTH, NCH, 128, 6, 1024).transpose(0, 3, 2, 1, 4))
    def gu(wg, wu, nj):
        a = wg.reshape(NCH, 128, nj, 128).transpose(2, 1, 0, 3)
        b = wu.reshape(NCH, 128, nj, 128).transpose(2, 1, 0, 3)
        return np.ascontiguousarray(np.stack([a, b], axis=2))
    if with_ffn:
        m["ffn_gu"] = np.stack([gu(inp["ffn_w_gate"][l], inp["ffn_w_up"][l], 22) for l in range(2)])
        m["ffn_d"] = np.ascontiguousarray(inp["ffn_w_down"].reshape(2, 22, 128, D))
        m["moe_gu"] = np.stack([np.stack([gu(inp["moe_w_gate"][l, e], inp["moe_w_up"][l, e], 28)
                                          for e in range(NE)]) for l in range(2)])
        m["moe_d"] = np.ascontiguousarray(inp["moe_w_down"].reshape(2, NE, 28, 128, D))
    m["moe_r"] = np.ascontiguousarray(
        inp["moe_w_router"].reshape(2, NCH, 128, NE).transpose(0, 2, 1, 3))
    m["ident"] = np.eye(128, dtype=np.float32)
    return m


def build_program(shapes, cfg):
    nc = bass.Bass("TRN2", target_bir_lowering=False)
    kb = KB(nc)
    dr = {}
    for name, (shp, kind) in shapes.items():
        dt_ = BF16 if name.startswith("dft_") or name.startswith("idft_") else F32
        dr[name] = nc.dram_tensor(name, list(shp), dt_, kind=kind).ap()
    pvo, pvn = pv_layout()

    es = contextlib.ExitStack()
    with es:
        uid = [0]

        def sb(name, shape, dt, st=None):
            uid[0] += 1
            return (st or es).enter_context(nc.sbuf_tensor(f"{name}_{uid[0]}", list(shape), dt))

        Xh = [None]
        HN = sb("HN", [128, NCH, TS], BF16)
        PVEC = sb("PVEC", [128, pvn], F32)
        MOD = sb("MOD", [128, DEPTH, 48, 2], F32)
        AB = sb("AB", [128, 8, NCH], F32)
        IDF = sb("IDF", [128, 128], F32)
        IDB = sb("IDB", [128, 128], BF16)
        ONESB = sb("ONESB", [128, 128], BF16)
        EPSC = sb("EPSC", [128, 1], F32)
        PS = [es.enter_context(nc.psum_tensor(f"ps{i}", [128, 512], F32)) for i in range(8)]
        bPS = [Buf(f"ps{i}") for i in range(8)]
        bX = [[Buf() for _ in range(4)] for _ in range(NCH)]
        bHN = [Buf() for _ in range(4)]
        bC = Buf("consts")
        bMOD = Buf("mod")
        bAB = Buf("ab")
        ps_rr = [0]

        ps_n = [8]

        def next_ps():
            i = ps_rr[0] % ps_n[0]
            ps_rr[0] += 1
            return i

        kb.dma("sp", PVEC[:], dr["pvec"], writes=[bC])
        kb.dma("sp", IDF[:], dr["ident"], writes=[bC])
        kb.op("dve", lambda e: e.tensor_copy(out=IDB[:], in_=IDF[:]), reads=[bC], writes=[bC])
        kb.op("dve", lambda e: e.memset(ONESB[:], 1.0), writes=[bC])
        kb.op("dve", lambda e: e.memset(EPSC[:], EPS), writes=[bC])

        def pvc(name, a, b):
            o, w = pvo[name]
            return PVEC[:, o + a:o + b]

        with contextlib.ExitStack() as ph:
            CT = sb("CT", [128, NCH, 2], F32, ph)
            CTB = sb("CTB", [128, NCH, 2], BF16, ph)
            WM = [sb(f"WM{i}", [128, NCH, 1024], BF16, ph) for i in range(2)]
            bWM = [Buf(), Buf()]
            bCT = Buf()
            kb.dma("sp", CT[:], dr["condT"], writes=[bCT])
            kb.op("act", lambda e: e.activation(out=CTB[:], in_=CT[:], func=AF.Silu), reads=[bCT], writes=[bCT])
            it = 0
            for l in range(DEPTH):
                for g in range(6):
                    s = it % 2
                    it += 1
                    kb.dma("pool", WM[s][:], dr["w_mod"][l, g], writes=[bWM[s]])
                    pi = next_ps()
                    for fc in range(8):
                        for kc in range(NCH):
                            kb.op("pe", lambda e, fc=fc, kc=kc, s=s, pi=pi: e.matmul(
                                PS[pi][:, fc * 2:fc * 2 + 2], WM[s][:, kc, fc * 128:(fc + 1) * 128], CTB[:, kc, :],
                                start=(kc == 0), stop=(kc == NCH - 1)),
                                reads=[bWM[s], bCT], writes=[bPS[pi]])
                    o, _ = pvo["b_mod"]
                    bm = PVEC[:, o + l * 48 + g * 8:o + l * 48 + g * 8 + 8]
                    kb.op("dve", lambda e, pi=pi, l=l, g=g, bm=bm: e.tensor_tensor(
                        out=MOD[:, l, g * 8:(g + 1) * 8, :],
                        in0=PS[pi][:, 0:16].rearrange("p (f j) -> p f j", j=2),
                        in1=bm.unsqueeze(2).to_broadcast([128, 8, 2]), op=ALU.add),
                        reads=[bPS[pi], bC], writes=[bMOD])
            kb.barrier()

        def set_ab(l, j):
            def mk(e):
                return None
            for half, nname in ((0, "norm1_g"), (1, "norm2_g")):
                sh = MOD[:, l, (3 * half + 0) * 8:(3 * half + 1) * 8, j]
                sc = MOD[:, l, (3 * half + 1) * 8:(3 * half + 2) * 8, j]
                gg = MOD[:, l, (3 * half + 2) * 8:(3 * half + 3) * 8, j]
                ng = pvc(nname, l * 8, l * 8 + 8)
                kb.op("dve", lambda e, sc=sc, ng=ng, half=half: e.scalar_tensor_tensor(
                    out=AB[:, 3 * half + 0, :], in0=sc, scalar=1.0, in1=ng, op0=ALU.add, op1=ALU.mult),
                    reads=[bMOD, bC], writes=[bAB])
                kb.op("dve", lambda e, sh=sh, half=half: e.tensor_copy(out=AB[:, 3 * half + 1, :], in_=sh),
                      reads=[bMOD], writes=[bAB])
                kb.op("dve", lambda e, gg=gg, half=half: e.tensor_copy(out=AB[:, 3 * half + 2, :], in_=gg),
                      reads=[bMOD], writes=[bAB])

        def norm_phase(T, a_idx, b_idx, router=None):
            nt = T // 512
            with contextlib.ExitStack() as ph:
                SQ = [sb(f"SQ{i}", [128, NCH, 512], BF16, ph) for i in range(2)]
                RS = [sb(f"RS{i}", [128, 512], F32, ph) for i in range(2)]
                TM = [sb(f"TM{i}", [128, NCH, 512], F32, ph) for i in range(2)]
                bSQ = [Buf(), Buf()]
                bRS = [Buf(), Buf()]
                bTM = [Buf(), Buf()]
                if router is not None:
                    H32 = [sb(f"H32{i}", [128, NCH, 512], F32, ph) for i in range(2)]
                    bH32 = [Buf(), Buf()]
                    LG = sb("LG", [128, 8], F32, ph)
                    MX = sb("MX", [128, 8], F32, ph)
                    NM = sb("NM", [128, 1], F32, ph)
                    MK = sb("MK", [128, 8], F32, ph)
                    EX = sb("EX", [128, 8], F32, ph)
                    DN = sb("DN", [128, 1], F32, ph)
                    bR = Buf()
                for tt in range(nt):
                    s = tt % 2
                    ts = slice(tt * 512, (tt + 1) * 512)
                    xb = [bX[c][tt] for c in range(NCH)]
                    kb.op("act", lambda e, s=s, ts=ts: e.activation(out=SQ[s][:], in_=Xh[0][:, :, ts], func=AF.Square),
                          reads=xb, writes=[bSQ[s]])
                    pi = next_ps()
                    for c in range(NCH):
                        kb.op("pe", lambda e, c=c, s=s, pi=pi: e.matmul(
                            PS[pi][:, :], ONESB[:, :], SQ[s][:, c, :], start=(c == 0), stop=(c == NCH - 1)),
                            reads=[bSQ[s], bC], writes=[bPS[pi]])
                    kb.op("act", lambda e, s=s, pi=pi: e.activation(
                        out=RS[s][:], in_=PS[pi][:, :], func=AF.Sqrt, bias=EPSC[:, 0:1], scale=1.0 / D),
                        reads=[bPS[pi], bC], writes=[bRS[s]])
                    kb.op("dve", lambda e, s=s: e.reciprocal(out=RS[s][:], in_=RS[s][:]),
                        reads=[bRS[s]], writes=[bRS[s]])
                    kb.op("dve", lambda e, s=s, ts=ts: e.tensor_tensor(
                        out=TM[s][:], in0=Xh[0][:, :, ts], in1=RS[s][:].unsqueeze(1).to_broadcast([128, NCH, 512]),
                        op=ALU.mult), reads=xb + [bRS[s]], writes=[bTM[s]])
                    for c in range(NCH):
                        if router is None:
                            kb.op("act", lambda e, c=c, s=s, ts=ts: e.activation(
                                out=HN[:, c, ts], in_=TM[s][:, c, :], func=AF.Identity,
                                scale=AB[:, a_idx, c:c + 1], bias=AB[:, b_idx, c:c + 1]),
                                reads=[bTM[s], bAB], writes=[bHN[tt]])
                        else:
                            kb.op("act", lambda e, c=c, s=s: e.activation(
                                out=H32[s][:, c, :], in_=TM[s][:, c, :], func=AF.Identity,
                                scale=AB[:, a_idx, c:c + 1], bias=AB[:, b_idx, c:c + 1]),
                                reads=[bTM[s], bAB], writes=[bH32[s]])
                    if router is not None:
                        WR, GT, bGT, bWR = router
                        kb.op("pool", lambda e, s=s, ts=ts: e.tensor_copy(out=HN[:, :, ts], in_=H32[s][:]),
                              reads=[bH32[s]], writes=[bHN[tt]])
                        for q in range(4):
                            ch = tt * 4 + q
                            pi = next_ps()
                            for kc in range(NCH):
                                kb.op("pe", lambda e, kc=kc, s=s, q=q, pi=pi: e.matmul(
                                    PS[pi][:, 0:8], H32[s][:, kc, q * 128:(q + 1) * 128], WR[:, kc, :],
                                    start=(kc == 0), stop=(kc == NCH - 1)),
                                    reads=[bH32[s], bWR], writes=[bPS[pi]])
                            kb.op("dve", lambda e, pi=pi: e.tensor_copy(out=LG[:], in_=PS[pi][:, 0:8]),
                                  reads=[bPS[pi]], writes=[bR])
                            kb.op("dve", lambda e: e.max(out=MX[:], in_=LG[:]), reads=[bR], writes=[bR])
                            kb.op("dve", lambda e: e.tensor_scalar_mul(out=NM[:], in0=MX[:, 0:1], scalar1=-1.0),
                                  reads=[bR], writes=[bR])
                            kb.op("dve", lambda e: e.tensor_scalar(
                                out=MK[:], in0=LG[:], scalar1=MX[:, 1:2], scalar2=None, op0=ALU.is_ge),
                                reads=[bR], writes=[bR])
                            kb.op("act", lambda e: e.activation(out=EX[:], in_=LG[:], func=AF.Exp, bias=NM[:, 0:1], scale=1.0),
                                  reads=[bR], writes=[bR])
                            kb.op("dve", lambda e: e.tensor_tensor(out=EX[:], in0=EX[:], in1=MK[:], op=ALU.mult),
                                  reads=[bR], writes=[bR])
                            kb.op("dve", lambda e: e.reduce_sum(out=DN[:], in_=EX[:], axis=AX.X),
                                  reads=[bR], writes=[bR])
                            kb.op("dve", lambda e: e.reciprocal(out=DN[:], in_=DN[:]), reads=[bR], writes=[bR])
                            kb.op("dve", lambda e, ch=ch: e.tensor_scalar_mul(out=GT[:, ch, :], in0=EX[:], scalar1=DN[:, 0:1]),
                                  reads=[bR], writes=[bGT])
                kb.barrier()

        def glu_phase(T, gu_src, d_src, njs, G, g_idx, ph, gate=None):
            nt = T // 512
            RA, bRA, RB, bRB, HP, bHP, SG, bSG, T2, bT2, st = ph
            for g0 in range(0, njs, G):
                js = list(range(g0, min(njs, g0 + G)))
                for j in js:
                    sa = st["a"] % len(RA)
                    st["a"] += 1
                    kb.dma("pool", RA[sa][:], gu_src[j], writes=[bRA[sa]])
                    for tt in range(nt):
                        ts = slice(tt * 512, (tt + 1) * 512)
                        pg, pu = next_ps(), next_ps()
                        for (pi, w) in ((pg, 0), (pu, 1)):
                            for kc in range(NCH):
                                kb.op("pe", lambda e, pi=pi, w=w, kc=kc, sa=sa, ts=ts: e.matmul(
                                    PS[pi][:, :], RA[sa][:, w, kc, :], HN[:, kc, ts],
                                    start=(kc == 0), stop=(kc == NCH - 1)),
                                    reads=[bRA[sa], bHN[tt]], writes=[bPS[pi]])
                        ss = st["s"] % 2
                        st["s"] += 1
                        kb.op("act", lambda e, ss=ss, pg=pg: e.activation(out=SG[ss][:], in_=PS[pg][:, :], func=AF.Silu),
                              reads=[bPS[pg]], writes=[bSG[ss]])
                        jj = j - g0
                        if gate is None:
                            kb.op("dve", lambda e, ss=ss, pu=pu, jj=jj, ts=ts: e.tensor_tensor(
                                out=HP[:, jj, ts], in0=PS[pu][:, :], in1=SG[ss][:], op=ALU.mult),
                                reads=[bPS[pu], bSG[ss]], writes=[bHP[jj][tt]])
                        else:
                            GE, bGE = gate
                            kb.op("dve", lambda e, ss=ss, pu=pu, ts=ts: e.tensor_tensor(
                                out=T2[ss][:], in0=PS[pu][:, :], in1=GE[:, ts], op=ALU.mult),
                                reads=[bPS[pu], bGE], writes=[bT2[ss]])
                            kb.op("dve", lambda e, ss=ss, jj=jj, ts=ts: e.tensor_tensor(
                                out=HP[:, jj, ts], in0=T2[ss][:], in1=SG[ss][:], op=ALU.mult),
                                reads=[bT2[ss], bSG[ss]], writes=[bHP[jj][tt]])
                sbs = []
                for j in js:
                    s_b = st["b"] % len(RB)
                    st["b"] += 1
                    kb.dma("pool", RB[s_b][:], d_src[j], writes=[bRB[s_b]])
                    sbs.append(s_b)
                for tt in range(nt):
                    ts = slice(tt * 512, (tt + 1) * 512)
                    for dc in range(NCH):
                        pi = next_ps()
                        for n, j in enumerate(js):
                            jj = j - g0
                            kb.op("pe", lambda e, pi=pi, n=n, jj=jj, dc=dc, ts=ts, s_b=sbs[n]: e.matmul(
                                PS[pi][:, :], RB[s_b][:, dc * 128:(dc + 1) * 128], HP[:, jj, ts],
                                start=(n == 0), stop=(n == len(js) - 1)),
                                reads=[bRB[sbs[n]], bHP[jj][tt]], writes=[bPS[pi]])
                        kb.op("dve", lambda e, pi=pi, dc=dc, ts=ts: e.scalar_tensor_tensor(
                            out=Xh[0][:, dc, ts], in0=PS[pi][:, :], scalar=AB[:, g_idx, dc:dc + 1], in1=Xh[0][:, dc, ts],
                            op0=ALU.mult, op1=ALU.add),
                            reads=[bPS[pi], bAB, bX[dc][tt]], writes=[bX[dc][tt]])

        def glu_alloc(ph, T, G):
            RA = [sb(f"RA{i}", [128, 2, NCH, 128], BF16, ph) for i in range(4)]
            RB = [sb(f"RB{i}", [128, D], BF16, ph) for i in range(G + 3)]
            HP = sb("HP", [128, G, T], BF16, ph)
            SG = [sb(f"SG{i}", [128, 512], F32, ph) for i in range(2)]
            T2 = [sb(f"T2{i}", [128, 512], F32, ph) for i in range(2)]
            return (RA, [Buf() for _ in RA], RB, [Buf() for _ in RB], HP,
                    [[Buf() for _ in range(4)] for _ in range(G)], SG, [Buf(), Buf()], T2, [Buf(), Buf()],
                    {"a": 0, "b": 0, "s": 0})

        def ffn_dense(T, l2):
            with contextlib.ExitStack() as ph:
                t = glu_alloc(ph, T, 11)
                glu_phase(T, dr["ffn_gu"][l2], dr["ffn_d"][l2], 22, 11, 5, t)
                kb.barrier()

        def moe(T, l2, GT, bGT):
            nchk = T // 128
            with contextlib.ExitStack() as ph:
                t = glu_alloc(ph, T, 7)
                GE = [sb(f"GE{i}", [128, T], F32, ph) for i in range(2)]
                bGE = [Buf(), Buf()]
                GX = [sb(f"GX{i}", [128, 128], F32, ph) for i in range(2)]
                bGX = [Buf(), Buf()]
                for ex in range(NE):
                    s = ex % 2
                    for tt in range(T // 512):
                        pi = next_ps()
                        for q in range(4):
                            ch = tt * 4 + q
                            sx = ch % 2
                            kb.op("dve", lambda e, sx=sx, ch=ch, ex=ex: e.tensor_copy(
                                out=GX[sx][:], in_=GT[:, ch, ex:ex + 1].to_broadcast([128, 128])),
                                reads=[bGT], writes=[bGX[sx]])
                            kb.op("pe", lambda e, sx=sx, q=q, pi=pi: e.matmul(
                                PS[pi][:, q * 128:(q + 1) * 128], GX[sx][:], IDF[:], start=True, stop=True),
                                reads=[bGX[sx], bC], writes=[bPS[pi]])
                        kb.op("act", lambda e, s=s, tt=tt, pi=pi: e.copy(out=GE[s][:, tt * 512:(tt + 1) * 512], in_=PS[pi][:, :]),
                              reads=[bPS[pi]], writes=[bGE[s]])
                    glu_phase(T, dr["moe_gu"][l2, ex], dr["moe_d"][l2, ex], 28, 7, 5, t, gate=(GE[s], bGE[s]))
                kb.barrier()

        RC = sb("RC", [128, 643], F32)
        kb.dma("sp", RC[:], dr["rc"], writes=[bC])
        DIFF, MA, MB_, POS1, POSR = (RC[:, i * 128:(i + 1) * 128] for i in range(5))
        PIDX, PREV, C128 = RC[:, 640:641], RC[:, 641:642], RC[:, 642:643]
        NLG = sb("NLG", [128, 16], F32)
        o_lg = pvo["lg"][0]
        LGt = PVEC[:, o_lg:o_lg + 16]
        kb.op("dve", lambda e: e.tensor_scalar_mul(out=NLG[:], in0=LGt, scalar1=-1.0), reads=[bC], writes=[bC])
        XS = nc.dram_tensor("XS", [128, NCH, TS], F32, kind="Internal").ap()
        YTD = nc.dram_tensor("YTD", [128, 16, TS], BF16, kind="Internal").ap()
        bXS, bYTD = Buf(), Buf()
        PSB = [p[:].bitcast(BF16) for p in PS]
        state_toks = []
        dbg_toks = []

        def retention_core(T, L, nseq, j, sample, pname):
            NCk, N, nt = T // 128, L // 128, T // 512
            with contextlib.ExitStack() as ph:
                QF = sb("QF", [128, 2, T], BF16, ph)
                QB = sb("QB", [128, 2, T], BF16, ph)
                KT = sb("KT", [128, 2, T], BF16, ph)
                KF = sb("KF", [128, NCk, 256], BF16, ph)
                KBt = sb("KBt", [128, NCk, 256], BF16, ph)
                V = sb("V", [128, NCk, 512], BF16, ph)
                SG = sb("SG", [128, 4, T], BF16, ph)
                CB = sb("CB", [128, NCk, 512], BF16, ph)
                S32 = [sb(f"S32{i}", [128, 2, 512], F32, ph) for i in range(2)]
                S16 = [sb(f"S16{i}", [128, 2, 512], BF16, ph) for i in range(2)]
                WU = [sb(f"WU{i}", [128, NCH, 128], BF16, ph) for i in range(4)]
                WV = sb("WV", [128, NCH, 512], BF16, ph)
                MT = sb("MT", [128, 128], F32, ph)
                XIF = sb("XIF", [128, 128], F32, ph)
                XIB = sb("XIB", [128, 128], F32, ph)
                E1 = sb("E1", [128, 128], F32, ph)
                E2 = sb("E2", [128, 128], F32, ph)
                KD = sb("KD", [128, 4], F32, ph)
                TT = [sb(f"TT{i}", [128, 512], F32, ph) for i in range(6)]
                ST = [sb(f"ST{i}", [128, 128], BF16, ph) for i in range(2)]
                ON = [sb(f"ON{i}", [128, 512], BF16, ph) for i in range(2)]
                STS = sb("STS", [128, 6], F32, ph)
                MV = sb("MV", [128, 2], F32, ph)
                RSD = sb("RSD", [128, 1], F32, ph)
                NMR = sb("NMR", [128, 1], F32, ph)
                bQF, bQB, bKT, bKF, bKBt, bV, bSG, bCB = (Buf() for _ in range(8))
                bS32, bS16 = [Buf(), Buf()], [Buf(), Buf()]
                bWU, bWV = [Buf() for _ in WU], Buf()
                bK, bTT, bST, bON, bGN = Buf(), [Buf() for _ in TT], [Buf(), Buf()], [Buf(), Buf()], Buf()
                if sample:
                    COS = sb("COS", [128, T], F32, ph)
                    SIN = sb("SIN", [128, T], F32, ph)
                    bRT = Buf()
                    kb.dma("sp", COS[:], dr["rope_cos"], writes=[bRT])
                    kb.dma("sp", SIN[:], dr["rope_sin"], writes=[bRT])
                wu_i = [0]

                def load_unit(h, u):
                    k = wu_i[0] % 4
                    wu_i[0] += 1
                    kb.dma("pool", WU[k][:], dr["ret_wqkg"][j, h, u], writes=[bWU[k]])
                    return k

                def proj_fm(k, tt, pi):
                    ts = slice(tt * 512, (tt + 1) * 512)
                    for kc in range(NCH):
                        kb.op("pe", lambda e, kc=kc: e.matmul(PS[pi][:, :], WU[k][:, kc, :], HN[:, kc, ts],
                                                               start=(kc == 0), stop=(kc == NCH - 1)),
                              reads=[bWU[k], bHN[tt]], writes=[bPS[pi]])

                for h in range(RH):
                    lgf = LGt[:, (j * 4 + h) * 2:(j * 4 + h) * 2 + 1]
                    lgb = LGt[:, (j * 4 + h) * 2 + 1:(j * 4 + h) * 2 + 2]
                    nlgf = NLG[:, (j * 4 + h) * 2:(j * 4 + h) * 2 + 1]
                    nlgb = NLG[:, (j * 4 + h) * 2 + 1:(j * 4 + h) * 2 + 2]
                    kb.op("act", lambda e: e.activation(out=E1[:], in_=DIFF, func=AF.Exp, scale=lgf), reads=[bC], writes=[bK])
                    kb.op("dve", lambda e: e.tensor_tensor(out=E1[:], in0=E1[:], in1=MA, op=ALU.mult), reads=[bK, bC], writes=[bK])
                    kb.op("act", lambda e: e.activation(out=E2[:], in_=DIFF, func=AF.Exp, scale=nlgb), reads=[bC, bK], writes=[bK])
                    kb.op("dve", lambda e: e.tensor_tensor(out=E2[:], in0=E2[:], in1=MB_, op=ALU.mult), reads=[bK, bC], writes=[bK])
                    kb.op("dve", lambda e: e.tensor_tensor(out=E1[:], in0=E1[:], in1=E2[:], op=ALU.add), reads=[bK], writes=[bK])
                    kb.op("act", lambda e: e.activation(out=E2[:], in_=POS1, func=AF.Exp, scale=nlgf), reads=[bC, bK], writes=[bK])
                    kb.op("dve", lambda e: e.tensor_tensor(out=MT[:], in0=E1[:], in1=E2[:], op=ALU.mult), reads=[bK], writes=[bK])
                    kb.op("act", lambda e: e.activation(out=XIF[:], in_=POS1, func=AF.Exp, scale=lgf), reads=[bC, bK], writes=[bK])
                    kb.op("act", lambda e: e.activation(out=XIB[:], in_=POSR, func=AF.Exp, scale=lgb), reads=[bC, bK], writes=[bK])
                    kb.op("act", lambda e: e.activation(out=KD[:, 0:1], in_=PREV, func=AF.Exp, scale=lgf), reads=[bC, bK], writes=[bK])
                    kb.op("act", lambda e: e.activation(out=KD[:, 1:2], in_=PIDX, func=AF.Exp, scale=lgb), reads=[bC, bK], writes=[bK])
                    kb.op("act", lambda e: e.activation(out=KD[:, 2:3], in_=C128, func=AF.Exp, scale=lgf), reads=[bC, bK], writes=[bK])
                    kb.op("act", lambda e: e.activation(out=KD[:, 3:4], in_=C128, func=AF.Exp, scale=lgb), reads=[bC, bK], writes=[bK])
                    for typ in range(2):
                        ka, kb_ = load_unit(h, typ * 2), load_unit(h, typ * 2 + 1)
                        for tt in range(nt):
                            ts = slice(tt * 512, (tt + 1) * 512)
                            pa, pb = next_ps(), next_ps()
                            proj_fm(ka, tt, pa)
                            proj_fm(kb_, tt, pb)
                            A2, B2 = TT[4], TT[5]
                            if sample:
                                kb.op("dve", lambda e: e.tensor_tensor(out=TT[0][:], in0=PS[pa][:, :], in1=COS[:, ts], op=ALU.mult),
                                      reads=[bPS[pa], bRT], writes=[bTT[0]])
                                kb.op("dve", lambda e: e.tensor_tensor(out=TT[1][:], in0=PS[pb][:, :], in1=SIN[:, ts], op=ALU.mult),
                                      reads=[bPS[pb], bRT], writes=[bTT[1]])
                                kb.op("dve", lambda e: e.tensor_tensor(out=TT[2][:], in0=PS[pa][:, :], in1=SIN[:, ts], op=ALU.mult),
                                      reads=[bPS[pa], bRT], writes=[bTT[2]])
                                kb.op("dve", lambda e: e.tensor_tensor(out=TT[3][:], in0=PS[pb][:, :], in1=COS[:, ts], op=ALU.mult),
                                      reads=[bPS[pb], bRT], writes=[bTT[3]])
                                kb.op("pool", lambda e: e.tensor_tensor(out=A2[:], in0=TT[0][:], in1=TT[1][:], op=ALU.subtract),
                                      reads=[bTT[0], bTT[1]], writes=[bTT[4]])
                                kb.op("pool", lambda e: e.tensor_tensor(out=B2[:], in0=TT[2][:], in1=TT[3][:], op=ALU.add),
                                      reads=[bTT[2], bTT[3]], writes=[bTT[5]])
                            else:
                                kb.op("act", lambda e: e.copy(out=A2[:], in_=PS[pa][:, :]), reads=[bPS[pa]], writes=[bTT[4]])
                                kb.op("act", lambda e: e.copy(out=B2[:], in_=PS[pb][:, :]), reads=[bPS[pb]], writes=[bTT[5]])
                            for half, src, bsrc in ((0, A2, bTT[4]), (1, B2, bTT[5])):
                                s3 = src[:, :].rearrange("p (a b) -> p a b", b=128)
                                if typ == 0:
                                    for dst, bdst, xi, eng in ((QF, bQF, XIF, "dve"), (QB, bQB, XIB, "pool")):
                                        kb.op(eng, lambda e, dst=dst, xi=xi, half=half, s3=s3: e.tensor_tensor(
                                            out=dst[:, half, ts].rearrange("p (a b) -> p a b", b=128), in0=s3,
                                            in1=xi[:, :].unsqueeze(1).to_broadcast([128, 4, 128]), op=ALU.mult),
                                            reads=[bsrc, bK], writes=[bdst])
                                else:
                                    kb.op("act", lambda e, half=half, src=src: e.mul(out=KT[:, half, ts], in_=src[:, :], mul=1.0 / 16.0),
                                          reads=[bsrc], writes=[bKT])
                    for u in range(4):
                        k = load_unit(h, 4 + u)
                        for tt in range(nt):
                            ts = slice(tt * 512, (tt + 1) * 512)
                            pi = next_ps()
                            proj_fm(k, tt, pi)
                            kb.op("act", lambda e, u=u, pi=pi, ts=ts: e.activation(out=SG[:, u, ts], in_=PS[pi][:, :], func=AF.Silu),
                                  reads=[bPS[pi]], writes=[bSG])
                    kb.dma("pool", WV[:], dr["ret_wv"][j, h], writes=[bWV])
                    for g in range(NCk):
                        cs = slice(g * 128, (g + 1) * 128)
                        pi = next_ps()
                        for kc in range(NCH):
                            kb.op("pe", lambda e, kc=kc, pi=pi, cs=cs: e.matmul(PS[pi][:, :], HN[:, kc, cs], WV[:, kc, :],
                                                                              start=(kc == 0), stop=(kc == NCH - 1)),
                                  reads=[bWV, bHN[g // 4]], writes=[bPS[pi]])
                        kb.op("act", lambda e, g=g, pi=pi: e.copy(out=V[:, g, :], in_=PS[pi][:, :]), reads=[bPS[pi]], writes=[bV])
                    for g in range(NCk):
                        cs = slice(g * 128, (g + 1) * 128)
                        pi = next_ps()
                        for dd in range(2):
                            kb.op("pe", lambda e, dd=dd, pi=pi, cs=cs: e.transpose(
                                out=PSB[pi][:, dd * 128:(dd + 1) * 128], in_=KT[:, dd, cs], identity=IDB[:]),
                                reads=[bKT, bC], writes=[bPS[pi]])
                        kb.op("act", lambda e, g=g, pi=pi: e.activation(out=KF[:, g, :], in_=PSB[pi][:, 0:256], func=AF.Copy, scale=KD[:, 0:1]),
                              reads=[bPS[pi], bK], writes=[bKF])
                        kb.op("dve", lambda e, g=g, pi=pi: e.tensor_scalar_mul(out=KBt[:, g, :], in0=PSB[pi][:, 0:256], scalar1=KD[:, 1:2]),
                              reads=[bPS[pi], bK, bKF], writes=[bKBt])

                    def state_init(d, s):
                        if sample:
                            kb.dma("sp", S32[d][:], dr["srf" if d == 0 else "srb"][j, h], writes=[bS32[d]])
                        else:
                            kb.op("pool", lambda e: e.memset(S32[d][:], 0.0), writes=[bS32[d]])
                        kb.op("act", lambda e: e.copy(out=S16[d][:], in_=S32[d][:]), reads=[bS32[d]], writes=[bS16[d]])

                    def state_update(d, g, Kt, bKt):
                        for dd in range(2):
                            pd = next_ps()
                            kb.op("pe", lambda e, dd=dd, pd=pd: e.matmul(PS[pd][:, :], Kt[:, g, dd * 128:(dd + 1) * 128], V[:, g, :],
                                                                        start=True, stop=True),
                                  reads=[bKt, bV], writes=[bPS[pd]])
                            kb.op("dve", lambda e, dd=dd, pd=pd: e.scalar_tensor_tensor(
                                out=S32[d][:, dd, :], in0=S32[d][:, dd, :], scalar=KD[:, 2 + d:3 + d], in1=PS[pd][:, :],
                                op0=ALU.mult, op1=ALU.add), reads=[bPS[pd], bS32[d], bK], writes=[bS32[d]])
                        kb.op("act", lambda e: e.copy(out=S16[d][:], in_=S32[d][:]), reads=[bS32[d]], writes=[bS16[d]])

                    def state_out(d, s):
                        if not sample:
                            nm = "nsf" if d == 0 else "nsb"
                            state_toks.append(kb.dma("sp", dr[nm][s, j, h], S32[d][:], reads=[bS32[d]]))

                    for s in range(nseq):
                        state_init(1, s)
                        for n in reversed(range(N)):
                            g = s * N + n
                            cs = slice(g * 128, (g + 1) * 128)
                            pc = next_ps()
                            for dd in range(2):
                                kb.op("pe", lambda e, dd=dd, pc=pc, cs=cs: e.matmul(PS[pc][:, :], QB[:, dd, cs], S16[1][:, dd, :],
                                                                                  start=(dd == 0), stop=(dd == 1)),
                                      reads=[bQB, bS16[1]], writes=[bPS[pc]])
                            kb.op("act", lambda e, g=g, pc=pc: e.copy(out=CB[:, g, :], in_=PS[pc][:, :]), reads=[bPS[pc]], writes=[bCB])
                            state_update(1, g, KBt, bKBt)
                        state_out(1, s)
                        state_init(0, s)
                        def stage_a(n):
                            g = s * N + n
                            cs = slice(g * 128, (g + 1) * 128)
                            i2 = g % 2
                            p_s = next_ps()
                            for dd in range(2):
                                kb.op("pe", lambda e: e.matmul(PS[p_s][:, 0:128], KT[:, dd, cs], QF[:, dd, cs],
                                                                start=(dd == 0), stop=(dd == 1)),
                                      reads=[bKT, bQF], writes=[bPS[p_s]])
                            kb.op("dve", lambda e: e.tensor_tensor(out=ST[i2][:], in0=PS[p_s][:, 0:128], in1=MT[:], op=ALU.mult),
                                  reads=[bPS[p_s], bK], writes=[bST[i2]])
                            p_o = next_ps()
                            kb.op("pe", lambda e: e.matmul(PS[p_o][:, :], ST[i2][:], V[:, g, :], start=True, stop=False),
                                  reads=[bST[i2], bV], writes=[bPS[p_o]])
                            for dd in range(2):
                                kb.op("pe", lambda e: e.matmul(PS[p_o][:, :], QF[:, dd, cs], S16[0][:, dd, :], start=False, stop=False),
                                      reads=[bQF, bS16[0]], writes=[bPS[p_o]])
                            kb.op("pe", lambda e: e.matmul(PS[p_o][:, :], IDB[:], CB[:, g, :], start=False, stop=True),
                                  reads=[bCB, bC], writes=[bPS[p_o]])
                            state_update(0, g, KF, bKF)
                            return (cs, i2, p_o)

                        def stage_b(ctx):
                            cs, i2, p_o = ctx
                            kb.op("dve", lambda e: e.bn_stats(out=STS[:], in_=PS[p_o][:, :]), reads=[bPS[p_o]], writes=[bGN])
                            kb.op("dve", lambda e: e.bn_aggr(out=MV[:], in_=STS[:]), reads=[bGN], writes=[bGN])
                            kb.op("act", lambda e: e.activation(out=RSD[:], in_=MV[:, 1:2], func=AF.Sqrt, bias=EPSC[:, 0:1], scale=1.0),
                                  reads=[bGN, bC], writes=[bGN])
                            kb.op("dve", lambda e: e.reciprocal(out=RSD[:], in_=RSD[:]), reads=[bGN], writes=[bGN])
                            kb.op("dve", lambda e: e.scalar_tensor_tensor(out=NMR[:], in0=MV[:, 0:1], scalar=-1.0, in1=RSD[:],
                                                                           op0=ALU.mult, op1=ALU.mult), reads=[bGN], writes=[bGN])
                            kb.op("act", lambda e: e.activation(out=ON[i2][:], in_=PS[p_o][:, :], func=AF.Identity,
                                                                 scale=RSD[:, 0:1], bias=NMR[:, 0:1]),
                                  reads=[bPS[p_o], bGN], writes=[bON[i2]])
                            p_t = next_ps()
                            for vv in range(4):
                                kb.op("pe", lambda e: e.transpose(
                                    out=PSB[p_t][:, vv * 128:(vv + 1) * 128], in_=ON[i2][:, vv * 128:(vv + 1) * 128], identity=IDB[:]),
                                    reads=[bON[i2], bC], writes=[bPS[p_t]])
                            kb.op("dve", lambda e: e.tensor_tensor(
                                out=SG[:, :, cs], in0=PSB[p_t][:, 0:512].rearrange("p (a b) -> p a b", b=128), in1=SG[:, :, cs], op=ALU.mult),
                                reads=[bPS[p_t], bSG], writes=[bSG])

                        prev = None
                        for n in range(N):
                            cur = stage_a(n)
                            if prev is not None:
                                stage_b(prev)
                            prev = cur
                        stage_b(prev)
                        state_out(0, s)
                    kb.dma("sp", YTD[:, h * 4:(h + 1) * 4, 0:T], SG[:, :, :], reads=[bSG], writes=[bYTD])
                kb.barrier()

        def hyena_core(T, L, nseq, j, sample, pname):
            NCk, NQ, nt = T // 128, L // 128, T // 512
            PI = math.pi
            ps_n[0] = 4
            with contextlib.ExitStack() as ph:
                W3 = sb("W3", [64, 4096], F32, ph)
                HD = sb("HD", [64, L], F32, ph)
                HYF = sb("HYF", [64, 4], F32, ph)
                ONESF = sb("ONESF", [128, 128], F32, ph)
                bHY = Buf()
                kb.dma("sp", W3[:], dr["hy_w3"][j], writes=[bHY])
                kb.dma("sp", HYF[:], dr["hyf"][j], writes=[bHY])
                kb.op("dve", lambda e: e.memset(ONESF[:], 1.0), writes=[bHY])
                with contextlib.ExitStack() as p0:
                    ZF = sb("ZF", [33, L], F32, p0)
                    W1 = sb("W1", [33, 64], F32, p0)
                    W2 = sb("W2", [64, 64], F32, p0)
                    H1 = sb("H1", [64, L], F32, p0)
                    AR = [sb(f"AR{i}", [64, 512], F32, p0) for i in range(3)]
                    bAR, bH1 = Buf(), Buf()
                    kb.dma("sp", ZF[:], dr["zf_" + pname], writes=[bHY])
                    kb.dma("sp", W1[:], dr["hy_w1"][j], writes=[bHY])
                    kb.dma("sp", W2[:], dr["hy_w2"][j], writes=[bHY])
                    for lay in range(2):
                        src, Wl, dst, bdst = ((ZF, W1, H1, bH1), (H1, W2, HD, bHY))[lay]
                        bsrc = bHY if lay == 0 else bH1
                        for c0 in range(0, L, 512):
                            w = min(512, L - c0)
                            pi = next_ps()
                            kb.op("pe", lambda e: e.matmul(PS[pi][0:64, 0:w], Wl[:, :], src[:, c0:c0 + w], start=True, stop=True),
                                  reads=[bHY, bsrc], writes=[bPS[pi]])
                            kb.op("dve", lambda e: e.tensor_scalar(out=AR[0][:, 0:w], in0=PS[pi][0:64, 0:w], scalar1=HYF[:, lay:lay + 1],
                                                                    scalar2=HYF[:, 2 + lay:3 + lay], op0=ALU.add, op1=ALU.mult),
                                  reads=[bPS[pi], bHY], writes=[bAR])
                            kb.op("dve", lambda e: e.tensor_scalar(out=AR[1][:, 0:w], in0=AR[0][:, 0:w], scalar1=PI, scalar2=-2.0 * PI,
                                                                    op0=ALU.is_gt, op1=ALU.mult), reads=[bAR], writes=[bAR])
                            kb.op("dve", lambda e: e.tensor_scalar(out=AR[2][:, 0:w], in0=AR[0][:, 0:w], scalar1=-PI, scalar2=2.0 * PI,
                                                                    op0=ALU.is_lt, op1=ALU.mult), reads=[bAR], writes=[bAR])
                            kb.op("dve", lambda e: e.tensor_tensor(out=AR[0][:, 0:w], in0=AR[0][:, 0:w], in1=AR[1][:, 0:w], op=ALU.add),
                                  reads=[bAR], writes=[bAR])
                            kb.op("dve", lambda e: e.tensor_tensor(out=AR[0][:, 0:w], in0=AR[0][:, 0:w], in1=AR[2][:, 0:w], op=ALU.add),
                                  reads=[bAR], writes=[bAR])
                            kb.op("act", lambda e: e.activation(out=dst[:, c0:c0 + w], in_=AR[0][:, 0:w], func=AF.Sin),
                                  reads=[bAR], writes=[bdst])
                    kb.barrier()
                WINC = sb("WINC", [128, NQ, 128], F32, ph)
                FW = [sb(f"FW{i}", [128, 4, 128], F32, ph) for i in range(2)]
                ABS_ = [sb(f"ABS{i}", [128, 4, 128], F32, ph) for i in range(2)]
                HS = sb("HS", [128, 2, NQ, 128], BF16, ph)
                HDF = sb("HDF", [128, 2, NQ, 128], BF16, ph)
                RN = sb("RN", [128, 2, 128], F32, ph)
                P32 = sb("P32", [128, T], F32, ph)
                U32 = sb("U32", [128, T], F32, ph)
                UU = [sb(f"UU{i}", [128, T], BF16, ph) for i in range(3)]
                ZN = [sb(f"ZN{i}", [128, T], BF16, ph) for i in range(2)]
                ZH = [sb(f"ZH{i}", [128, NCk, 256], BF16, ph) for i in range(2)]
                FU = [sb(f"FU{i}", [128, NQ, 128], BF16, ph) for i in range(3)]
                GU = [sb(f"GU{i}", [128, L], BF16, ph) for i in range(3)]
                WI = [sb(f"WI{i}", [128, NCH, 128], BF16, ph) for i in range(3)]
                AK = [sb(f"AK{i}", [128, 2, 128], F32, ph) for i in range(2)]
                PT = [sb(f"PT{i}", [128, 128], F32, ph) for i in range(4)]
                PQ = sb("PQ", [128, NQ, 2, nseq, 128], BF16, ph)
                R32 = [sb(f"R32{i}", [128, 512], F32, ph) for i in range(2)]
                bWIN, bFW, bABS, bHS, bRN, bP32, bU32 = Buf(), [Buf(), Buf()], [Buf(), Buf()], Buf(), Buf(), Buf(), Buf()
                bUU, bZN, bZH = [Buf() for _ in UU], [Buf(), Buf()], [Buf(), Buf()]
                bFU, bGU, bWI = [Buf() for _ in FU], [Buf() for _ in GU], [Buf() for _ in WI]
                bAK, bPT, bPQ, bR32 = [Buf(), Buf()], [Buf() for _ in PT], Buf(), [Buf(), Buf()]
                cnt = {"f": 0, "g": 0, "w": 0}
                resident = (L == 256)
                if resident:
                    FUALL = sb("FUALL", [128, 2, NQ, NQ, 128], BF16, ph)
                    GUALL = sb("GUALL", [128, 2, NQ, L], BF16, ph)
                    bTAB = Buf()
                    for trig in range(2):
                        for kq in range(NQ):
                            kb.dma("sp", FUALL[:, trig, kq], dr["dft_" + pname][trig, kq], writes=[bTAB])
                            kb.dma("sp", GUALL[:, trig, kq], dr["idft_" + pname][trig, kq], writes=[bTAB])
                o_bi, o_cw, o_cb, o_fb = pvo["hy_b_in"][0], pvo["hy_cw"][0], pvo["hy_cb"][0], pvo["hy_fb"][0]
                W3v = W3[:, :].rearrange("p (a c) -> p a c", a=4)
                hy_stage = cfg.get("hy_stage", 9)
                for cc in range(cfg.get("hy_ncc", NCH) if hy_stage >= 2 else 0):
                    kb.dma("sp", WINC[:], dr["win_" + pname][cc], writes=[bWIN])
                    skp = cfg.get("hy_skip", [])
                    for tc in range(0 if "filt" in skp else NQ):
                        i2 = tc % 2
                        pi = next_ps()
                        kb.op("pe", lambda e: e.matmul(PS[pi][:, :].rearrange("p (a c) -> p a c", a=4), HD[:, tc * 128:(tc + 1) * 128],
                                                        W3v[:, :, cc * 128:(cc + 1) * 128], start=True, stop=True),
                              reads=[bHY], writes=[bPS[pi]])
                        kb.op("dve", lambda e: e.tensor_tensor(out=FW[i2][:], in0=PS[pi][:, :].rearrange("p (a c) -> p a c", a=4),
                                                                in1=WINC[:, tc, :].unsqueeze(1).to_broadcast([128, 4, 128]), op=ALU.mult),
                              reads=[bPS[pi], bWIN], writes=[bFW[i2]])
                        if tc == 0:
                            kb.op("dve", lambda e: e.memset(FW[i2][0:1, 2:4, :], 0.0), reads=[bFW[i2]], writes=[bFW[i2]])
                        kb.op("pool", lambda e: e.tensor_tensor(out=HS[:, :, tc, :], in0=FW[i2][:, 0:2, :], in1=FW[i2][:, 2:4, :], op=ALU.add),
                              reads=[bFW[i2]], writes=[bHS])
                        kb.op("pool", lambda e: e.tensor_tensor(out=HDF[:, :, tc, :], in0=FW[i2][:, 0:2, :], in1=FW[i2][:, 2:4, :], op=ALU.subtract),
                              reads=[bFW[i2]], writes=[bHS])
                        kb.op("act", lambda e: e.activation(out=ABS_[i2][:], in_=FW[i2][:], func=AF.Abs),
                              reads=[bFW[i2]], writes=[bABS[i2]])
                        kb.op("pe", lambda e: e.matmul(PS[4][:, :], ONESF[:, :], ABS_[i2][:].rearrange("p a c -> p (a c)"),
                                                        start=(tc == 0), stop=(tc == NQ - 1)),
                              reads=[bABS[i2], bHY], writes=[bPS[4]])
                    if "filt" not in skp:
                        kb.op("act", lambda e: e.copy(out=RN[:], in_=PS[4][:, 0:256].rearrange("p (a c) -> p a c", a=2)),
                              reads=[bPS[4]], writes=[bRN])
                        kb.op("dve", lambda e: e.tensor_tensor(out=RN[:], in0=RN[:],
                                                                in1=PS[4][:, 256:512].rearrange("p (a c) -> p a c", a=2), op=ALU.add),
                              reads=[bPS[4], bRN], writes=[bRN])
                    else:
                        kb.op("dve", lambda e: e.memset(RN[:], 1.0), writes=[bRN])
                    kb.op("dve", lambda e: e.tensor_scalar_add(out=RN[:], in0=RN[:], scalar1=EPS), reads=[bRN], writes=[bRN])
                    kb.op("dve", lambda e: e.reciprocal(out=RN[:], in_=RN[:]), reads=[bRN], writes=[bRN])
                    for part in range(3 if (hy_stage >= 3 and "proj" not in skp) else 0):
                        col = part * 8 + cc
                        k = cnt["w"] % 3
                        cnt["w"] += 1
                        kb.dma("pool", WI[k][:], dr["hy_wi"][j, cc, part], writes=[bWI[k]])
                        for tt in range(nt):
                            ts = slice(tt * 512, (tt + 1) * 512)
                            pi = next_ps()
                            for kc in range(NCH):
                                kb.op("pe", lambda e: e.matmul(PS[pi][:, :], WI[k][:, kc, :], HN[:, kc, ts], start=(kc == 0), stop=(kc == NCH - 1)),
                                      reads=[bWI[k], bHN[tt]], writes=[bPS[pi]])
                            kb.op("act", lambda e: e.activation(out=P32[:, ts], in_=PS[pi][:, :], func=AF.Identity,
                                                                 bias=PVEC[:, o_bi + j * 24 + col:o_bi + j * 24 + col + 1], scale=1.0),
                                  reads=[bPS[pi], bC], writes=[bP32])
                        cw = [PVEC[:, o_cw + (j * 3 + tap) * 24 + col:o_cw + (j * 3 + tap) * 24 + col + 1] for tap in range(3)]
                        cbias = PVEC[:, o_cb + j * 24 + col:o_cb + j * 24 + col + 1]
                        kb.op("act", lambda e: e.activation(out=U32[:, 0:T], in_=P32[:, 0:T], func=AF.Identity, bias=cbias, scale=cw[1]),
                              reads=[bP32, bC], writes=[bU32])
                        for s in range(nseq):
                            a, b = s * L, (s + 1) * L
                            kb.op("dve", lambda e: e.scalar_tensor_tensor(out=U32[:, a + 1:b], in0=P32[:, a:b - 1], scalar=cw[0],
                                                                           in1=U32[:, a + 1:b], op0=ALU.mult, op1=ALU.add),
                                  reads=[bP32, bU32, bC], writes=[bU32])
                            kb.op("dve", lambda e: e.scalar_tensor_tensor(out=U32[:, a:b - 1], in0=P32[:, a + 1:b], scalar=cw[2],
                                                                           in1=U32[:, a:b - 1], op0=ALU.mult, op1=ALU.add),
                                  reads=[bP32, bU32, bC], writes=[bU32])
                        kb.op("pool", lambda e: e.tensor_copy(out=UU[part][:], in_=U32[:, 0:T]), reads=[bU32], writes=[bUU[part]])
                    zin, bzin = UU[0], bUU[0]
                    for o in range(2 if hy_stage >= 4 else 0):
                        for g4 in range(0, 0 if "tr" in skp else NCk, 4):
                            pi = next_ps()
                            for q4 in range(4):
                                g = g4 + q4
                                kb.op("pe", lambda e: e.transpose(out=PSB[pi][:, q4 * 128:(q4 + 1) * 128], in_=zin[:, g * 128:(g + 1) * 128],
                                                                   identity=IDB[:]), reads=[bzin, bC], writes=[bPS[pi]])
                            kb.op("act", lambda e: e.copy(out=ZH[0][:, g4:g4 + 4, 0:128],
                                                           in_=PSB[pi][:, 0:512].rearrange("p (a b) -> p a b", b=128)),
                                  reads=[bPS[pi]], writes=[bZH[0]])
                            kb.op("dve", lambda e: e.tensor_copy(out=ZH[1][:, g4:g4 + 4, 0:128], in_=ZH[0][:, g4:g4 + 4, 0:128]),
                                  reads=[bZH[0]], writes=[bZH[1]])
                        for s in range(nseq):
                            kb.op("pool", lambda e: e.tensor_copy(out=ZH[0][:, s * NQ:(s + 1) * NQ, 128:256], in_=HS[:, o, :, :]),
                                  reads=[bHS], writes=[bZH[0]])
                            kb.op("pool", lambda e: e.tensor_copy(out=ZH[1][:, s * NQ:(s + 1) * NQ, 128:256], in_=HDF[:, o, :, :]),
                                  reads=[bHS], writes=[bZH[1]])
                        for kq in range(0 if "fwd" in skp else NQ):
                            for s in range(nseq):
                                for trig in range(2):
                                    if resident:
                                        fu_t, fu_b = FUALL[:, trig, kq], bTAB
                                    else:
                                        kf = cnt["f"] % 3
                                        cnt["f"] += 1
                                        kb.dma("sp", FU[kf][:], dr["dft_" + pname][trig, kq], writes=[bFU[kf]])
                                        fu_t, fu_b = FU[kf], bFU[kf]
                                    pi = next_ps()
                                    for tc in range(NQ):
                                        kb.op("pe", lambda e: e.matmul(PS[pi][:, 0:256], fu_t[:, tc, :], ZH[trig][:, s * NQ + tc, :],
                                                                        start=(tc == 0), stop=(tc == NQ - 1)),
                                              reads=[fu_b, bZH[trig]], writes=[bPS[pi]])
                                    kb.op("act", lambda e: e.copy(out=AK[trig][:].rearrange("p a c -> p (a c)"), in_=PS[pi][:, 0:256]),
                                          reads=[bPS[pi]], writes=[bAK[trig]])
                                    kb.op("dve", lambda e: e.tensor_tensor(out=AK[trig][:, 1, :], in0=AK[trig][:, 1, :], in1=RN[:, o, :], op=ALU.mult),
                                          reads=[bAK[trig], bRN], writes=[bAK[trig]])
                                A_, Kc, B_, Ks = AK[0][:, 0, :], AK[0][:, 1, :], AK[1][:, 0, :], AK[1][:, 1, :]
                                kb.op("dve", lambda e: e.tensor_tensor(out=PT[0][:], in0=A_, in1=Kc, op=ALU.mult), reads=bAK, writes=[bPT[0]])
                                kb.op("pool", lambda e: e.tensor_tensor(out=PT[1][:], in0=B_, in1=Ks, op=ALU.mult), reads=bAK, writes=[bPT[1]])
                                kb.op("dve", lambda e: e.tensor_tensor(out=PT[2][:], in0=B_, in1=Kc, op=ALU.mult), reads=bAK, writes=[bPT[2]])
                                kb.op("pool", lambda e: e.tensor_tensor(out=PT[3][:], in0=A_, in1=Ks, op=ALU.mult), reads=bAK, writes=[bPT[3]])
                                kb.op("dve", lambda e: e.tensor_tensor(out=PQ[:, kq, 0, s, :], in0=PT[0][:], in1=PT[1][:], op=ALU.subtract),
                                      reads=[bPT[0], bPT[1]], writes=[bPQ])
                                kb.op("pool", lambda e: e.tensor_tensor(out=PQ[:, kq, 1, s, :], in0=PT[2][:], in1=PT[3][:], op=ALU.add),
                                      reads=[bPT[2], bPT[3]], writes=[bPQ])
                        if cfg.get("hy_dbg") and cc == 0 and o == 0:
                            dbg_toks.append(kb.dma("sp", dr["dbg_hd"][0:64, 0:L], HD[:, :], reads=[bHY]))
                            dbg_toks.append(kb.dma("sp", dr["dbg_rn"][:, :], RN[:].rearrange("p a c -> p (a c)"), reads=[bRN]))
                            dbg_toks.append(kb.dma("pool", dr["dbg_pq"][:, 0:NQ * 2 * nseq * 128],
                                                   PQ[:].rearrange("p a b c d -> p (a b c d)"), reads=[bPQ]))
                            dbg_toks.append(kb.dma("pool", dr["dbg_zh"][:, 0:NCk * 256],
                                                   ZH[0][:].rearrange("p a b -> p (a b)"), reads=[bZH[0]]))
                        wt = min(512, L)
                        accs = [(s, c0) for s in range(nseq) for c0 in range(0, L, wt)]
                        assert len(accs) == 4
                        if hy_stage < 5:
                            continue
                        for kq in range(NQ):
                            for trig in range(2):
                                if resident:
                                    gu_t, gu_b = GUALL[:, trig, kq], bTAB
                                else:
                                    kg = cnt["g"] % 3
                                    cnt["g"] += 1
                                    kb.dma("sp", GU[kg][:], dr["idft_" + pname][trig, kq], writes=[bGU[kg]])
                                    gu_t, gu_b = GU[kg], bGU[kg]
                                for ai, (s, c0) in enumerate(accs):
                                    if cfg.get("hy_nomm") or ai >= cfg.get("hy_nacc", 4):
                                        continue
                                    ab = cfg.get("hy_accbase", 4)
                                    hv = cfg.get("hy_var", 0)
                                    lh = IDB[:] if hv == 1 else PQ[:, kq, trig, s, :]
                                    rh = UU[0][:, c0:c0 + wt] if hv == 2 else gu_t[:, c0:c0 + wt]
                                    kb.op("pe", lambda e: e.matmul(PS[ab + ai][:, 0:wt], lh, rh,
                                                                    start=(kq == 0 and trig == 0), stop=(kq == NQ - 1 and trig == 1)),
                                          reads=[bPQ, gu_b], writes=[bPS[ab + ai]])
                        fb = PVEC[:, o_fb + (j * 2 + o) * 8 + cc:o_fb + (j * 2 + o) * 8 + cc + 1]
                        if hy_stage < 6:
                            continue
                        for ai, (s, c0) in enumerate(accs):
                            ts = slice(s * L + c0, s * L + c0 + wt)
                            r = ai % 2
                            kb.op("dve", lambda e: e.scalar_tensor_tensor(out=R32[r][:, 0:wt], in0=zin[:, ts], scalar=fb, in1=PS[4 + ai][:, 0:wt],
                                                                           op0=ALU.mult, op1=ALU.add),
                                  reads=[bzin, bPS[4 + ai], bC], writes=[bR32[r]])
                            kb.op("pool", lambda e: e.tensor_tensor(out=ZN[o][:, ts], in0=R32[r][:, 0:wt], in1=UU[o + 1][:, ts], op=ALU.mult),
                                  reads=[bR32[r], bUU[o + 1]], writes=[bZN[o]])
                        zin, bzin = ZN[o], bZN[o]
                    kb.dma("sp", YTD[:, cc, 0:T], ZN[1][:, :], reads=[bZN[1]], writes=[bYTD])
                kb.barrier()
            ps_n[0] = 8

        def outproj(T, w_src, nk, g_idx, scale_cols=None, bias_col=None):
            nt = T // 512
            with contextlib.ExitStack() as ph:
                SRC = sb("SRC", [128, nk, T], BF16, ph)
                WO = sb("WO", [128, nk, D], BF16, ph)
                bSRC, bWO = Buf(), [Buf() for _ in range(nk)]
                GBt = sb("GBt", [128, NCH], F32, ph)
                bGB = Buf()
                if bias_col is not None:
                    kb.op("dve", lambda e: e.tensor_tensor(out=GBt[:], in0=AB[:, g_idx, :], in1=bias_col, op=ALU.mult),
                          reads=[bAB, bC], writes=[bGB])
                kb.dma("sp", SRC[:], YTD[:, 0:nk, 0:T], reads=[bYTD], writes=[bSRC])
                for kc in range(nk):
                    kb.dma("pool", WO[:, kc, :], w_src[kc], writes=[bWO[kc]])
                    if scale_cols is not None:
                        kb.op("dve", lambda e, kc=kc: e.tensor_scalar_mul(out=WO[:, kc, :], in0=WO[:, kc, :], scalar1=scale_cols[:, kc:kc + 1]),
                              reads=[bWO[kc], bC], writes=[bWO[kc]])
                for tt in range(nt):
                    ts = slice(tt * 512, (tt + 1) * 512)
                    for dc in range(NCH):
                        pi = next_ps()
                        for kc in range(nk):
                            kb.op("pe", lambda e, kc=kc, pi=pi, dc=dc, ts=ts: e.matmul(
                                PS[pi][:, :], WO[:, kc, dc * 128:(dc + 1) * 128], SRC[:, kc, ts], start=(kc == 0), stop=(kc == nk - 1)),
                                reads=[bWO[kc], bSRC], writes=[bPS[pi]])
                        kb.op("dve", lambda e, pi=pi, dc=dc, ts=ts: e.scalar_tensor_tensor(
                            out=Xh[0][:, dc, ts], in0=PS[pi][:, :], scalar=AB[:, g_idx, dc:dc + 1], in1=Xh[0][:, dc, ts],
                            op0=ALU.mult, op1=ALU.add),
                            reads=[bPS[pi], bAB, bX[dc][tt]], writes=[bX[dc][tt]])
                        if bias_col is not None:
                            kb.op("dve", lambda e, dc=dc, ts=ts: e.tensor_scalar(
                                out=Xh[0][:, dc, ts], in0=Xh[0][:, dc, ts], scalar1=GBt[:, dc:dc + 1], scalar2=None, op0=ALU.add),
                                reads=[bGB, bX[dc][tt]], writes=[bX[dc][tt]])
                kb.barrier()

        def final_norm(T, pname):
            nt = T // 512
            with contextlib.ExitStack() as ph:
                kb.op("dve", lambda e: e.tensor_copy(out=AB[:, 6, :], in_=pvc("final_g", 0, 8)), reads=[bC], writes=[bAB])
                SQ = [sb(f"FSQ{i}", [128, NCH, 512], BF16, ph) for i in range(2)]
                RS = [sb(f"FRS{i}", [128, 512], F32, ph) for i in range(2)]
                TM = [sb(f"FTM{i}", [128, NCH, 512], F32, ph) for i in range(2)]
                bSQ, bRS, bTM = [Buf(), Buf()], [Buf(), Buf()], [Buf(), Buf()]
                for tt in range(nt):
                    s = tt % 2
                    ts = slice(tt * 512, (tt + 1) * 512)
                    xb = [bX[c][tt] for c in range(NCH)]
                    kb.op("act", lambda e, s=s, ts=ts: e.activation(out=SQ[s][:], in_=Xh[0][:, :, ts], func=AF.Square),
                          reads=xb, writes=[bSQ[s]])
                    pi = next_ps()
                    for c in range(NCH):
                        kb.op("pe", lambda e, c=c, s=s, pi=pi: e.matmul(
                            PS[pi][:, :], ONESB[:, :], SQ[s][:, c, :], start=(c == 0), stop=(c == NCH - 1)),
                            reads=[bSQ[s], bC], writes=[bPS[pi]])
                    kb.op("act", lambda e, s=s, pi=pi: e.activation(
                        out=RS[s][:], in_=PS[pi][:, :], func=AF.Sqrt, bias=EPSC[:, 0:1], scale=1.0 / D),
                        reads=[bPS[pi], bC], writes=[bRS[s]])
                    kb.op("dve", lambda e, s=s: e.reciprocal(out=RS[s][:], in_=RS[s][:]), reads=[bRS[s]], writes=[bRS[s]])
                    kb.op("dve", lambda e, s=s, ts=ts: e.tensor_tensor(
                        out=TM[s][:], in0=Xh[0][:, :, ts], in1=RS[s][:].unsqueeze(1).to_broadcast([128, NCH, 512]),
                        op=ALU.mult), reads=xb + [bRS[s]], writes=[bTM[s]])
                    kb.op("dve", lambda e, s=s: e.tensor_tensor(
                        out=TM[s][:], in0=TM[s][:], in1=AB[:, 6, :].unsqueeze(2).to_broadcast([128, NCH, 512]),
                        op=ALU.mult), reads=[bTM[s], bAB], writes=[bTM[s]])
                    out_toks.append(kb.dma("sp", dr["yT_" + pname][:, :, ts], TM[s][:], reads=[bTM[s]]))
                kb.barrier()

        def load_x(T, src, rd=()):
            nt = T // 512
            for c in range(NCH):
                kb.dma("sp", Xh[0][:, c, 0:T], src[:, c, 0:T], reads=list(rd), writes=[bX[c][tt] for tt in range(nt)])

        def store_x(T):
            nt = T // 512
            for c in range(NCH):
                kb.dma("sp", XS[:, c, 0:T], Xh[0][:, c, 0:T], reads=[bX[c][tt] for tt in range(nt)], writes=[bXS])

        out_toks = []
        do_ffn = cfg.get("ffn", True)
        do_ret = cfg.get("ret", True)
        do_hy = cfg.get("hyena", True)
        passes = cfg.get("passes", ("p", "s"))
        for pname, T, j, L, nseq in (("p", TP, 0, 256, 4), ("s", TS, 1, 2048, 1)):
            if pname not in passes:
                continue
            sample = pname == "s"
            xsrc = dr["xT_" + pname]
            x_in_xs = False
            for l in range(DEPTH):
                l2 = l // 2
                set_ab(l, j)
                has_mixer = (do_ret if l % 2 == 0 else do_hy)
                if has_mixer:
                    with contextlib.ExitStack() as phx:
                        Xh[0] = sb("X", [128, NCH, TS], F32, phx)
                        load_x(T, XS if x_in_xs else xsrc, rd=[bXS] if x_in_xs else [])
                        norm_phase(T, 0, 1)
                    if l % 2 == 0:
                        retention_core(T, L, nseq, l2, sample, pname)
                    else:
                        hyena_core(T, L, nseq, l2, sample, pname)
                with contextlib.ExitStack() as phx:
                    Xh[0] = sb("X", [128, NCH, TS], F32, phx)
                    load_x(T, XS if x_in_xs else xsrc, rd=[bXS] if x_in_xs else [])
                    if has_mixer:
                        if l % 2 == 0:
                            o_ln = pvo["ret_ln_g"][0]
                            outproj(T, dr["ret_wo"][l2], 16, 2, scale_cols=PVEC[:, o_ln + l2 * 16:o_ln + l2 * 16 + 16])
                        else:
                            o_b = pvo["hy_b_out"][0]
                            outproj(T, dr["hy_wo"][l2], 8, 2, bias_col=PVEC[:, o_b + l2 * 8:o_b + l2 * 8 + 8])
                    if do_ffn:
                        if l % 2 == 0:
                            norm_phase(T, 3, 4)
                            ffn_dense(T, l2)
                        else:
                            with contextlib.ExitStack() as ph2:
                                WR = sb("WR", [128, NCH, NE], F32, ph2)
                                GT = sb("GT", [128, TS // 128, NE], F32, ph2)
                                bWR, bGT = Buf(), Buf()
                                kb.dma("sp", WR[:], dr["moe_r"][l2], writes=[bWR])
                                norm_phase(T, 3, 4, router=(WR, GT, bGT, bWR))
                                moe(T, l2, GT, bGT)
                    if l < DEPTH - 1:
                        store_x(T)
                        x_in_xs = True
                        kb.barrier()
                    else:
                        final_norm(T, pname)
        for t in out_toks + state_toks + dbg_toks:
            kb.wait("sp", t[0], t[1])
    return nc


def kernel(**inputs):
    cfg = inputs.pop("_cfg", {})
    inp = {k: np.asarray(v) for k, v in inputs.items()}
    shared = host_prep_shared(inp, cfg.get("ffn", True))
    if not cfg.get("ffn", True):
        shared = {k: v for k, v in shared.items() if not (k.startswith("ffn_") or k.startswith("moe_"))}
    ncores = cfg.get("ncores", 8)
    per_core = [host_prep(inp, i) for i in range(ncores)]
    shapes = {}
    for k, v in {**shared, **per_core[0]}.items():
        shapes[k] = (v.shape, "ExternalInput")
    shapes["yT_p"] = ((128, NCH, TP), "ExternalOutput")
    shapes["yT_s"] = ((128, NCH, TS), "ExternalOutput")
    shapes["nsf"] = ((4, 2, RH, 128, 2, RDV), "ExternalOutput")
    shapes["nsb"] = ((4, 2, RH, 128, 2, RDV), "ExternalOutput")
    if cfg.get("hy_dbg"):
        shapes["dbg_hd"] = ((128, 2048), "ExternalOutput")
        shapes["dbg_rn"] = ((128, 256), "ExternalOutput")
        shapes["dbg_pq"] = ((128, 8192), "ExternalOutput")
        shapes["dbg_zh"] = ((128, 4096), "ExternalOutput")
    nc = build_program(shapes, cfg)
    in_maps = [{**shared, **per_core[i]} for i in range(ncores)]
    res = run_bass_kernel_spmd(nc, in_maps, core_ids=list(range(ncores)))
    yp = np.zeros((32, 256, D), np.float32)
    ys = np.zeros((8, 2048, D), np.float32)
    nsf = np.zeros((32, 2, RH, RDK, RDV), np.float32)
    nsb = np.zeros((32, 2, RH, RDK, RDV), np.float32)
    for i in range(ncores):
        r = res.results[i]
        yp[4 * i:4 * i + 4] = r["yT_p"].transpose(2, 1, 0).reshape(4, 256, D)
        ys[i] = r["yT_s"].transpose(2, 1, 0).reshape(TS, D)
        nsf[4 * i:4 * i + 4] = r["nsf"].transpose(0, 1, 2, 4, 3, 5).reshape(4, 2, RH, RDK, RDV)
        nsb[4 * i:4 * i + 4] = r["nsb"].transpose(0, 1, 2, 4, 3, 5).reshape(4, 2, RH, RDK, RDV)
    return yp, ys, nsf, nsb
```

```python
import contextlib
import math
import numpy as np
import ml_dtypes
import concourse.bass as bass
import concourse.mybir as mybir
from concourse.bass_utils import run_bass_kernel_spmd

F32 = mybir.dt.float32
BF16 = mybir.dt.bfloat16
AF = mybir.ActivationFunctionType
ALU = mybir.AluOpType
AX = mybir.AxisListType

D = 1024
NCH = 8
DEPTH = 4
TP = 1024
TS = 2048
EPS = 1e-6
D_FF = 2816
NE = 8
D_FFE = 3584
RH, RDK, RDV = 4, 256, 512
EPOCH = 60000


class Buf:
    __slots__ = ("name", "w", "r")

    def __init__(self, name=""):
        self.name = name
        self.w = None
        self.r = {}


class KB:
    def __init__(self, nc):
        self.nc = nc
        self.E = {"pe": nc.tensor, "act": nc.scalar, "dve": nc.vector, "pool": nc.gpsimd, "sp": nc.sync}
        self.cnt = {e: 0 for e in ("pe", "act", "dve", "pool")}
        self.esem = {e: [] for e in self.cnt}
        self.waited = {e: {} for e in self.E}
        self.semobj = {}
        self.nsem = 0
        self.dq = {"sp": [], "pool": []}
        self.dq_i = {"sp": 0, "pool": 0}
        self.NDQ = 12
        self.last_tok = {}
        self.all_dma_toks = {}

    def _newsem(self, name):
        s = self.nc.alloc_semaphore(f"{name}_{self.nsem}")
        self.nsem += 1
        sid = self.nsem
        self.semobj[sid] = s
        return sid

    def wait(self, e, sid, val):
        if self.waited[e].get(sid, 0) >= val:
            return
        self.E[e].wait_ge(self.semobj[sid], val)
        self.waited[e][sid] = val

    def _deps(self, e, reads, writes):
        deps = {}

        def add(t):
            if t is None:
                return
            if deps.get(t[0], 0) < t[1]:
                deps[t[0]] = t[1]
        for b in reads:
            add(b.w)
        for b in writes:
            add(b.w)
            for s, v in b.r.items():
                add((s, v))
        own = set(self.esem[e]) if e in self.esem else set()
        for s, v in deps.items():
            if e == "pe" and s in own:
                continue
            self.wait(e, s, v)

    def _record(self, tok, reads, writes):
        for b in reads:
            if b.r.get(tok[0], 0) < tok[1]:
                b.r[tok[0]] = tok[1]
        for b in writes:
            b.w = tok
            b.r = {}

    def op(self, e, fn, reads=(), writes=()):
        self._deps(e, reads, writes)
        ins = fn(self.E[e])
        n = self.cnt[e]
        ep, val = n // EPOCH, n % EPOCH + 1
        while len(self.esem[e]) <= ep:
            self.esem[e].append(self._newsem(e))
        sid = self.esem[e][ep]
        ins.then_inc(self.semobj[sid], 1)
        self.cnt[e] = n + 1
        tok = (sid, val)
        self.last_tok[e] = tok
        self._record(tok, reads, writes)
        return tok

    def dma(self, q, out, in_, reads=(), writes=()):
        lst = self.dq[q]
        i = self.dq_i[q]
        self.dq_i[q] = i + 1
        k = i % self.NDQ
        if len(lst) <= k:
            lst.append([self._newsem("d" + q), 0])
        if lst[k][1] >= 4000:
            lst[k] = [self._newsem("d" + q), 0]
        slot = lst[k]
        if slot[1] > 0:
            self.wait(q, slot[0], 16 * slot[1])
        self._deps(q, reads, writes)
        ins = self.E[q].dma_start(out=out, in_=in_)
        slot[1] += 1
        ins.then_inc(self.semobj[slot[0]], 16)
        tok = (slot[0], 16 * slot[1])
        self.all_dma_toks[slot[0]] = tok[1]
        self._record(tok, reads, writes)
        return tok

    def barrier(self):
        toks = list(self.last_tok.values()) + [(s, v) for s, v in self.all_dma_toks.items()]
        for e in self.E:
            own = set(self.esem[e]) if e in self.esem else set()
            for s, v in toks:
                if s in own:
                    continue
                self.wait(e, s, v)


def fm(v):
    v = np.asarray(v, dtype=np.float32)
    n = v.shape[-1] // 128
    r = v.reshape(v.shape[:-1] + (n, 128))
    return np.ascontiguousarray(np.moveaxis(r, -1, 0))


def kmaj(w):
    K, Fd = w.shape
    return np.ascontiguousarray(w.reshape(K // 128, 128, Fd).transpose(1, 0, 2))


class PV:
    def __init__(self):
        self.cols = []
        self.off = {}
        self.n = 0

    def add(self, name, arr):
        arr = np.asarray(arr, dtype=np.float32).reshape(128, -1)
        self.off[name] = (self.n, arr.shape[1])
        self.cols.append(arr)
        self.n += arr.shape[1]

    def build(self):
        return np.ascontiguousarray(np.concatenate(self.cols, axis=1))


def pv_layout():
    off = {}
    n = 0
    for name, w in (("b_mod", DEPTH * 48), ("norm1_g", DEPTH * 8), ("norm2_g", DEPTH * 8), ("final_g", 8),
                    ("ret_ln_g", 32), ("lg", 16), ("hy_b_in", 48), ("hy_cw", 144), ("hy_cb", 48),
                    ("hy_fb", 32), ("hy_b_out", 16)):
        off[name] = (n, w)
        n += w
    return off, n


def host_prep(inp, core):
    m = {}
    xp = inp["x_prompt"][4 * core:4 * core + 4].reshape(TP, D)
    xs = inp["x_sample"][core].reshape(TS, D)
    m["xT_p"] = np.ascontiguousarray(xp.T.reshape(NCH, 128, TP).transpose(1, 0, 2))
    m["xT_s"] = np.ascontiguousarray(xs.T.reshape(NCH, 128, TS).transpose(1, 0, 2))
    for nm, key in (("srf", "state_ret_fwd"), ("srb", "state_ret_bwd")):
        m[nm] = np.ascontiguousarray(inp[key][core].reshape(2, RH, 2, 128, RDV).transpose(0, 1, 3, 2, 4))
    cond = np.stack([inp["c_ctx"], inp["c"][core]], axis=-1)
    m["condT"] = np.ascontiguousarray(cond.reshape(NCH, 128, 2).transpose(1, 0, 2))
    return m


def host_prep_shared(inp, with_ffn=True):
    m = {}
    pv = PV()
    pv.add("b_mod", fm(inp["b_mod"]))
    pv.add("norm1_g", fm(inp["norm1_g"]))
    pv.add("norm2_g", fm(inp["norm2_g"]))
    pv.add("final_g", fm(inp["final_g"]))
    pv.add("ret_ln_g", fm(inp["ret_ln_g"]))
    lg = np.stack([inp["ret_log_gamma_fwd"], inp["ret_log_gamma_bwd"]], axis=-1)
    pv.add("lg", np.broadcast_to(lg.reshape(1, 16), (128, 16)))
    pv.add("hy_b_in", fm(inp["hy_b_in"]))
    pv.add("hy_cw", fm(inp["hy_conv_w"]))
    pv.add("hy_cb", fm(inp["hy_conv_b"]))
    pv.add("hy_fb", fm(inp["hy_f_bias"]))
    pv.add("hy_b_out", fm(inp["hy_b_out"]))
    m["pvec"] = pv.build()
    m["hy_wi"] = np.ascontiguousarray(
        inp["hy_w_in"].reshape(2, NCH, 128, 3, 8, 128).transpose(0, 4, 3, 2, 1, 5))
    m["hy_wo"] = np.ascontiguousarray(inp["hy_w_out"].reshape(2, 8, 128, D))
    m["hy_w1"] = np.ascontiguousarray(inp["hy_f_w1"])
    m["hy_w2"] = np.ascontiguousarray(inp["hy_f_w2"])
    m["hy_w3"] = np.ascontiguousarray(inp["hy_f_w3"])
    m["hyf"] = np.ascontiguousarray(np.stack([inp["hy_f_b1"], inp["hy_f_b2"], inp["hy_f_freq"][:, 0], inp["hy_f_freq"][:, 1]], -1))
    for L, nm in ((256, "p"), (2048, "s")):
        t = np.arange(L, dtype=np.float32) / np.float32(L)
        bands = np.linspace(1e-4, 15.0, 16, dtype=np.float32)
        ang = (2.0 * math.pi) * t[:, None] * bands
        z = np.concatenate([t[:, None], np.cos(ang), -np.sin(ang)], -1)
        m["zf_" + nm] = np.ascontiguousarray(z.T.astype(np.float32))
        mn, mx = math.log(1e-2) / 0.3, math.log(1e-2) / 1.5
        deltas = np.abs(np.linspace(mn, mx, D, dtype=np.float32))
        wn = np.exp(-t[:, None] * deltas).astype(np.float32)
        m["win_" + nm] = np.ascontiguousarray(wn.reshape(L // 128, 128, NCH, 128).transpose(2, 1, 0, 3))
        N2 = 2 * L
        tt_ = np.arange(L, dtype=np.float64)
        om = 2.0 * math.pi * (np.arange(L, dtype=np.float64) + 0.5) / N2
        ph_ = tt_[:, None] * om[None, :]
        nq = L // 128
        def lay_f(a):
            return a.reshape(nq, 128, nq, 128).transpose(2, 1, 0, 3)
        m["dft_" + nm] = np.ascontiguousarray(np.stack([lay_f(np.cos(ph_)), lay_f(np.sin(ph_))]).astype(ml_dtypes.bfloat16))
        def lay_g(a):
            return (a.T * (2.0 / N2)).reshape(nq, 128, L)
        m["idft_" + nm] = np.ascontiguousarray(np.stack([lay_g(np.cos(ph_)), lay_g(np.sin(ph_))]).astype(ml_dtypes.bfloat16))
    qd, vd = RH * RDK, RH * RDV
    wi = inp["ret_w_in"]
    def units(w):
        n = w.shape[1] // 128
        return w.reshape(NCH, 128, n, 128).transpose(2, 1, 0, 3)
    wq = np.stack([np.stack([np.concatenate([
        units(wi[l][:, h * RDK:(h + 1) * RDK]),
        units(wi[l][:, qd + h * RDK:qd + (h + 1) * RDK]),
        units(wi[l][:, 2 * qd + vd + h * RDV:2 * qd + vd + (h + 1) * RDV])], axis=0)
        for h in range(RH)]) for l in range(2)])
    m["ret_wqkg"] = np.ascontiguousarray(wq)
    m["ret_wv"] = np.ascontiguousarray(np.stack([np.stack([
        wi[l][:, 2 * qd + h * RDV:2 * qd + (h + 1) * RDV].reshape(NCH, 128, RDV).transpose(1, 0, 2)
        for h in range(RH)]) for l in range(2)]))
    m["ret_wo"] = np.ascontiguousarray(inp["ret_w_out"].reshape(2, 16, 128, D))
    ar = np.arange(128, dtype=np.float32)
    diff = ar[None, :] - ar[:, None]
    rc = np.concatenate([diff, (diff >= 0).astype(np.float32), (diff <= 0).astype(np.float32),
                         np.broadcast_to(ar[None, :] + 1.0, (128, 128)), np.broadcast_to(128.0 - ar[None, :], (128, 128)),
                         ar[:, None], 127.0 - ar[:, None], np.full((128, 1), 128.0, np.float32)], axis=1)
    m["rc"] = np.ascontiguousarray(rc.astype(np.float32))
    rows = TS // 64
    r = np.repeat(np.arange(rows, dtype=np.float32), 64)
    col = np.tile(np.arange(64, dtype=np.float32), rows)
    inv = (10000.0 ** (-np.arange(64, dtype=np.float32) / 64)).astype(np.float32)
    ang = np.concatenate([r[:, None] * inv, col[:, None] * inv], axis=-1)
    m["rope_cos"] = np.ascontiguousarray(np.cos(ang).T.astype(np.float32))
    m["rope_sin"] = np.ascontiguousarray(np.sin(ang).T.astype(np.float32))
    m["w_mod"] = np.ascontiguousarray(
        inp["w_mod"].reshape(DEPTH, NCH, 128, 6, 1024).transpose(0, 3, 2, 1, 4))
    def gu(wg, wu, nj):
        a = wg.reshape(NCH, 128, nj, 128).transpose(2, 1, 0, 3)
        b = wu.reshape(NCH, 128, nj, 128).transpose(2, 1, 0, 3)
        return np.ascontiguousarray(np.stack([a, b], axis=2))
    if with_ffn:
        m["ffn_gu"] = np.stack([gu(inp["ffn_w_gate"][l], inp["ffn_w_up"][l], 22) for l in range(2)])
        m["ffn_d"] = np.ascontiguousarray(inp["ffn_w_down"].reshape(2, 22, 128, D))
        m["moe_gu"] = np.stack([np.stack([gu(inp["moe_w_gate"][l, e], inp["moe_w_up"][l, e], 28)
                                          for e in range(NE)]) for l in range(2)])
        m["moe_d"] = np.ascontiguousarray(inp["moe_w_down"].reshape(2, NE, 28, 128, D))
    m["moe_r"] = np.ascontiguousarray(
        inp["moe_w_router"].reshape(2, NCH, 128, NE).transpose(0, 2, 1, 3))
    m["ident"] = np.eye(128, dtype=np.float32)
    return m


def build_program(shapes, cfg):
    nc = bass.Bass("TRN2", target_bir_lowering=False)
    kb = KB(nc)
    dr = {}
    for name, (shp, kind) in shapes.items():
        dt_ = BF16 if name.startswith("dft_") or name.startswith("idft_") else F32
        dr[name] = nc.dram_tensor(name, list(shp), dt_, kind=kind).ap()
    pvo, pvn = pv_layout()

    es = contextlib.ExitStack()
    with es:
        uid = [0]

        def sb(name, shape, dt, st=None):
            uid[0] += 1
            return (st or es).enter_context(nc.sbuf_tensor(f"{name}_{uid[0]}", list(shape), dt))

        Xh = [None]
        HN = sb("HN", [128, NCH, TS], BF16)
        PVEC = sb("PVEC", [128, pvn], F32)
        MOD = sb("MOD", [128, DEPTH, 48, 2], F32)
        AB = sb("AB", [128, 8, NCH], F32)
        IDF = sb("IDF", [128, 128], F32)
        IDB = sb("IDB", [128, 128], BF16)
        ONESB = sb("ONESB", [128, 128], BF16)
        EPSC = sb("EPSC", [128, 1], F32)
        PS = [es.enter_context(nc.psum_tensor(f"ps{i}", [128, 512], F32)) for i in range(8)]
        bPS = [Buf(f"ps{i}") for i in range(8)]
        bX = [[Buf() for _ in range(4)] for _ in range(NCH)]
        bHN = [Buf() for _ in range(4)]
        bC = Buf("consts")
        bMOD = Buf("mod")
        bAB = Buf("ab")
        ps_rr = [0]

        ps_n = [8]

        def next_ps():
            i = ps_rr[0] % ps_n[0]
            ps_rr[0] += 1
            return i

        kb.dma("sp", PVEC[:], dr["pvec"], writes=[bC])
        kb.dma("sp", IDF[:], dr["ident"], writes=[bC])
        kb.op("dve", lambda e: e.tensor_copy(out=IDB[:], in_=IDF[:]), reads=[bC], writes=[bC])
        kb.op("dve", lambda e: e.memset(ONESB[:], 1.0), writes=[bC])
        kb.op("dve", lambda e: e.memset(EPSC[:], EPS), writes=[bC])

        def pvc(name, a, b):
            o, w = pvo[name]
            return PVEC[:, o + a:o + b]

        with contextlib.ExitStack() as ph:
            CT = sb("CT", [128, NCH, 2], F32, ph)
            CTB = sb("CTB", [128, NCH, 2], BF16, ph)
            WM = [sb(f"WM{i}", [128, NCH, 1024], BF16, ph) for i in range(2)]
            bWM = [Buf(), Buf()]
            bCT = Buf()
            kb.dma("sp", CT[:], dr["condT"], writes=[bCT])
            kb.op("act", lambda e: e.activation(out=CTB[:], in_=CT[:], func=AF.Silu), reads=[bCT], writes=[bCT])
            it = 0
            for l in range(DEPTH):
                for g in range(6):
                    s = it % 2
                    it += 1
                    kb.dma("pool", WM[s][:], dr["w_mod"][l, g], writes=[bWM[s]])
                    pi = next_ps()
                    for fc in range(8):
                        for kc in range(NCH):
                            kb.op("pe", lambda e, fc=fc, kc=kc, s=s, pi=pi: e.matmul(
                                PS[pi][:, fc * 2:fc * 2 + 2], WM[s][:, kc, fc * 128:(fc + 1) * 128], CTB[:, kc, :],
                                start=(kc == 0), stop=(kc == NCH - 1)),
                                reads=[bWM[s], bCT], writes=[bPS[pi]])
                    o, _ = pvo["b_mod"]
                    bm = PVEC[:, o + l * 48 + g * 8:o + l * 48 + g * 8 + 8]
                    kb.op("dve", lambda e, pi=pi, l=l, g=g, bm=bm: e.tensor_tensor(
                        out=MOD[:, l, g * 8:(g + 1) * 8, :],
                        in0=PS[pi][:, 0:16].rearrange("p (f j) -> p f j", j=2),
                        in1=bm.unsqueeze(2).to_broadcast([128, 8, 2]), op=ALU.add),
                        reads=[bPS[pi], bC], writes=[bMOD])
            kb.barrier()

        def set_ab(l, j):
            def mk(e):
                return None
            for half, nname in ((0, "norm1_g"), (1, "norm2_g")):
                sh = MOD[:, l, (3 * half + 0) * 8:(3 * half + 1) * 8, j]
                sc = MOD[:, l, (3 * half + 1) * 8:(3 * half + 2) * 8, j]
                gg = MOD[:, l, (3 * half + 2) * 8:(3 * half + 3) * 8, j]
                ng = pvc(nname, l * 8, l * 8 + 8)
                kb.op("dve", lambda e, sc=sc, ng=ng, half=half: e.scalar_tensor_tensor(
                    out=AB[:, 3 * half + 0, :], in0=sc, scalar=1.0, in1=ng, op0=ALU.add, op1=ALU.mult),
                    reads=[bMOD, bC], writes=[bAB])
                kb.op("dve", lambda e, sh=sh, half=half: e.tensor_copy(out=AB[:, 3 * half + 1, :], in_=sh),
                      reads=[bMOD], writes=[bAB])
                kb.op("dve", lambda e, gg=gg, half=half: e.tensor_copy(out=AB[:, 3 * half + 2, :], in_=gg),
                      reads=[bMOD], writes=[bAB])

        def norm_phase(T, a_idx, b_idx, router=None):
            nt = T // 512
            with contextlib.ExitStack() as ph:
                SQ = [sb(f"SQ{i}", [128, NCH, 512], BF16, ph) for i in range(2)]
                RS = [sb(f"RS{i}", [128, 512], F32, ph) for i in range(2)]
                TM = [sb(f"TM{i}", [128, NCH, 512], F32, ph) for i in range(2)]
                bSQ = [Buf(), Buf()]
                bRS = [Buf(), Buf()]
                bTM = [Buf(), Buf()]
                if router is not None:
                    H32 = [sb(f"H32{i}", [128, NCH, 512], F32, ph) for i in range(2)]
                    bH32 = [Buf(), Buf()]
                    LG = sb("LG", [128, 8], F32, ph)
                    MX = sb("MX", [128, 8], F32, ph)
                    NM = sb("NM", [128, 1], F32, ph)
                    MK = sb("MK", [128, 8], F32, ph)
                    EX = sb("EX", [128, 8], F32, ph)
                    DN = sb("DN", [128, 1], F32, ph)
                    bR = Buf()
                for tt in range(nt):
                    s = tt % 2
                    ts = slice(tt * 512, (tt + 1) * 512)
                    xb = [bX[c][tt] for c in range(NCH)]
                    kb.op("act", lambda e, s=s, ts=ts: e.activation(out=SQ[s][:], in_=Xh[0][:, :, ts], func=AF.Square),
                          reads=xb, writes=[bSQ[s]])
                    pi = next_ps()
                    for c in range(NCH):
                        kb.op("pe", lambda e, c=c, s=s, pi=pi: e.matmul(
                            PS[pi][:, :], ONESB[:, :], SQ[s][:, c, :], start=(c == 0), stop=(c == NCH - 1)),
                            reads=[bSQ[s], bC], writes=[bPS[pi]])
                    kb.op("act", lambda e, s=s, pi=pi: e.activation(
                        out=RS[s][:], in_=PS[pi][:, :], func=AF.Sqrt, bias=EPSC[:, 0:1], scale=1.0 / D),
                        reads=[bPS[pi], bC], writes=[bRS[s]])
                    kb.op("dve", lambda e, s=s: e.reciprocal(out=RS[s][:], in_=RS[s][:]),
                        reads=[bRS[s]], writes=[bRS[s]])
                    kb.op("dve", lambda e, s=s, ts=ts: e.tensor_tensor(
                        out=TM[s][:], in0=Xh[0][:, :, ts], in1=RS[s][:].unsqueeze(1).to_broadcast([128, NCH, 512]),
                        op=ALU.mult), reads=xb + [bRS[s]], writes=[bTM[s]])
                    for c in range(NCH):
                        if router is None:
                            kb.op("act", lambda e, c=c, s=s, ts=ts: e.activation(
                                out=HN[:, c, ts], in_=TM[s][:, c, :], func=AF.Identity,
                                scale=AB[:, a_idx, c:c + 1], bias=AB[:, b_idx, c:c + 1]),
                                reads=[bTM[s], bAB], writes=[bHN[tt]])
                        else:
                            kb.op("act", lambda e, c=c, s=s: e.activation(
                                out=H32[s][:, c, :], in_=TM[s][:, c, :], func=AF.Identity,
                                scale=AB[:, a_idx, c:c + 1], bias=AB[:, b_idx, c:c + 1]),
                                reads=[bTM[s], bAB], writes=[bH32[s]])
                    if router is not None:
                        WR, GT, bGT, bWR = router
                        kb.op("pool", lambda e, s=s, ts=ts: e.tensor_copy(out=HN[:, :, ts], in_=H32[s][:]),
                              reads=[bH32[s]], writes=[bHN[tt]])
                        for q in range(4):
                            ch = tt * 4 + q
                            pi = next_ps()
                            for kc in range(NCH):
                                kb.op("pe", lambda e, kc=kc, s=s, q=q, pi=pi: e.matmul(
                                    PS[pi][:, 0:8], H32[s][:, kc, q * 128:(q + 1) * 128], WR[:, kc, :],
                                    start=(kc == 0), stop=(kc == NCH - 1)),
                                    reads=[bH32[s], bWR], writes=[bPS[pi]])
                            kb.op("dve", lambda e, pi=pi: e.tensor_copy(out=LG[:], in_=PS[pi][:, 0:8]),
                                  reads=[bPS[pi]], writes=[bR])
                            kb.op("dve", lambda e: e.max(out=MX[:], in_=LG[:]), reads=[bR], writes=[bR])
                            kb.op("dve", lambda e: e.tensor_scalar_mul(out=NM[:], in0=MX[:, 0:1], scalar1=-1.0),
                                  reads=[bR], writes=[bR])
                            kb.op("dve", lambda e: e.tensor_scalar(
                                out=MK[:], in0=LG[:], scalar1=MX[:, 1:2], scalar2=None, op0=ALU.is_ge),
                                reads=[bR], writes=[bR])
                            kb.op("act", lambda e: e.activation(out=EX[:], in_=LG[:], func=AF.Exp, bias=NM[:, 0:1], scale=1.0),
                                  reads=[bR], writes=[bR])
                            kb.op("dve", lambda e: e.tensor_tensor(out=EX[:], in0=EX[:], in1=MK[:], op=ALU.mult),
                                  reads=[bR], writes=[bR])
                            kb.op("dve", lambda e: e.reduce_sum(out=DN[:], in_=EX[:], axis=AX.X),
                                  reads=[bR], writes=[bR])
                            kb.op("dve", lambda e: e.reciprocal(out=DN[:], in_=DN[:]), reads=[bR], writes=[bR])
                            kb.op("dve", lambda e, ch=ch: e.tensor_scalar_mul(out=GT[:, ch, :], in0=EX[:], scalar1=DN[:, 0:1]),
                                  reads=[bR], writes=[bGT])
                kb.barrier()

        def glu_phase(T, gu_src, d_src, njs, G, g_idx, ph, gate=None):
            nt = T // 512
            RA, bRA, RB, bRB, HP, bHP, SG, bSG, T2, bT2, st = ph
            for g0 in range(0, njs, G):
                js = list(range(g0, min(njs, g0 + G)))
                for j in js:
                    sa = st["a"] % len(RA)
                    st["a"] += 1
                    kb.dma("pool", RA[sa][:], gu_src[j], writes=[bRA[sa]])
                    for tt in range(nt):
                        ts = slice(tt * 512, (tt + 1) * 512)
                        pg, pu = next_ps(), next_ps()
                        for (pi, w) in ((pg, 0), (pu, 1)):
                            for kc in range(NCH):
                                kb.op("pe", lambda e, pi=pi, w=w, kc=kc, sa=sa, ts=ts: e.matmul(
                                    PS[pi][:, :], RA[sa][:, w, kc, :], HN[:, kc, ts],
                                    start=(kc == 0), stop=(kc == NCH - 1)),
                                    reads=[bRA[sa], bHN[tt]], writes=[bPS[pi]])
                        ss = st["s"] % 2
                        st["s"] += 1
                        kb.op("act", lambda e, ss=ss, pg=pg: e.activation(out=SG[ss][:], in_=PS[pg][:, :], func=AF.Silu),
                              reads=[bPS[pg]], writes=[bSG[ss]])
                        jj = j - g0
                        if gate is None:
                            kb.op("dve", lambda e, ss=ss, pu=pu, jj=jj, ts=ts: e.tensor_tensor(
                                out=HP[:, jj, ts], in0=PS[pu][:, :], in1=SG[ss][:], op=ALU.mult),
                                reads=[bPS[pu], bSG[ss]], writes=[bHP[jj][tt]])
                        else:
                            GE, bGE = gate
                            kb.op("dve", lambda e, ss=ss, pu=pu, ts=ts: e.tensor_tensor(
                                out=T2[ss][:], in0=PS[pu][:, :], in1=GE[:, ts], op=ALU.mult),
                                reads=[bPS[pu], bGE], writes=[bT2[ss]])
                            kb.op("dve", lambda e, ss=ss, jj=jj, ts=ts: e.tensor_tensor(
                                out=HP[:, jj, ts], in0=T2[ss][:], in1=SG[ss][:], op=ALU.mult),
                                reads=[bT2[ss], bSG[ss]], writes=[bHP[jj][tt]])
                sbs = []
                for j in js:
                    s_b = st["b"] % len(RB)
                    st["b"] += 1
                    kb.dma("pool", RB[s_b][:], d_src[j], writes=[bRB[s_b]])
                    sbs.append(s_b)
                for tt in range(nt):
                    ts = slice(tt * 512, (tt + 1) * 512)
                    for dc in range(NCH):
                        pi = next_ps()
                        for n, j in enumerate(js):
                            jj = j - g0
                            kb.op("pe", lambda e, pi=pi, n=n, jj=jj, dc=dc, ts=ts, s_b=sbs[n]: e.matmul(
                                PS[pi][:, :], RB[s_b][:, dc * 128:(dc + 1) * 128], HP[:, jj, ts],
                                start=(n == 0), stop=(n == len(js) - 1)),
                                reads=[bRB[sbs[n]], bHP[jj][tt]], writes=[bPS[pi]])
                        kb.op("dve", lambda e, pi=pi, dc=dc, ts=ts: e.scalar_tensor_tensor(
                            out=Xh[0][:, dc, ts], in0=PS[pi][:, :], scalar=AB[:, g_idx, dc:dc + 1], in1=Xh[0][:, dc, ts],
                            op0=ALU.mult, op1=ALU.add),
                            reads=[bPS[pi], bAB, bX[dc][tt]], writes=[bX[dc][tt]])

        def glu_alloc(ph, T, G):
            RA = [sb(f"RA{i}", [128, 2, NCH, 128], BF16, ph) for i in range(4)]
            RB = [sb(f"RB{i}", [128, D], BF16, ph) for i in range(G + 3)]
            HP = sb("HP", [128, G, T], BF16, ph)
            SG = [sb(f"SG{i}", [128, 512], F32, ph) for i in range(2)]
            T2 = [sb(f"T2{i}", [128, 512], F32, ph) for i in range(2)]
            return (RA, [Buf() for _ in RA], RB, [Buf() for _ in RB], HP,
                    [[Buf() for _ in range(4)] for _ in range(G)], SG, [Buf(), Buf()], T2, [Buf(), Buf()],
                    {"a": 0, "b": 0, "s": 0})

        def ffn_dense(T, l2):
            with contextlib.ExitStack() as ph:
                t = glu_alloc(ph, T, 11)
                glu_phase(T, dr["ffn_gu"][l2], dr["ffn_d"][l2], 22, 11, 5, t)
                kb.barrier()

        def moe(T, l2, GT, bGT):
            nchk = T // 128
            with contextlib.ExitStack() as ph:
                t = glu_alloc(ph, T, 7)
                GE = [sb(f"GE{i}", [128, T], F32, ph) for i in range(2)]
                bGE = [Buf(), Buf()]
                GX = [sb(f"GX{i}", [128, 128], F32, ph) for i in range(2)]
                bGX = [Buf(), Buf()]
                for ex in range(NE):
                    s = ex % 2
                    for tt in range(T // 512):
                        pi = next_ps()
                        for q in range(4):
                            ch = tt * 4 + q
                            sx = ch % 2
                            kb.op("dve", lambda e, sx=sx, ch=ch, ex=ex: e.tensor_copy(
                                out=GX[sx][:], in_=GT[:, ch, ex:ex + 1].to_broadcast([128, 128])),
                                reads=[bGT], writes=[bGX[sx]])
                            kb.op("pe", lambda e, sx=sx, q=q, pi=pi: e.matmul(
                                PS[pi][:, q * 128:(q + 1) * 128], GX[sx][:], IDF[:], start=True, stop=True),
                                reads=[bGX[sx], bC], writes=[bPS[pi]])
                        kb.op("act", lambda e, s=s, tt=tt, pi=pi: e.copy(out=GE[s][:, tt * 512:(tt + 1) * 512], in_=PS[pi][:, :]),
                              reads=[bPS[pi]], writes=[bGE[s]])
                    glu_phase(T, dr["moe_gu"][l2, ex], dr["moe_d"][l2, ex], 28, 7, 5, t, gate=(GE[s], bGE[s]))
                kb.barrier()

        RC = sb("RC", [128, 643], F32)
        kb.dma("sp", RC[:], dr["rc"], writes=[bC])
        DIFF, MA, MB_, POS1, POSR = (RC[:, i * 128:(i + 1) * 128] for i in range(5))
        PIDX, PREV, C128 = RC[:, 640:641], RC[:, 641:642], RC[:, 642:643]
        NLG = sb("NLG", [128, 16], F32)
        o_lg = pvo["lg"][0]
        LGt = PVEC[:, o_lg:o_lg + 16]
        kb.op("dve", lambda e: e.tensor_scalar_mul(out=NLG[:], in0=LGt, scalar1=-1.0), reads=[bC], writes=[bC])
        XS = nc.dram_tensor("XS", [128, NCH, TS], F32, kind="Internal").ap()
        YTD = nc.dram_tensor("YTD", [128, 16, TS], BF16, kind="Internal").ap()
        bXS, bYTD = Buf(), Buf()
        PSB = [p[:].bitcast(BF16) for p in PS]
        state_toks = []
        dbg_toks = []

        def retention_core(T, L, nseq, j, sample, pname):
            NCk, N, nt = T // 128, L // 128, T // 512
            with contextlib.ExitStack() as ph:
                QF = sb("QF", [128, 2, T], BF16, ph)
                QB = sb("QB", [128, 2, T], BF16, ph)
                KT = sb("KT", [128, 2, T], BF16, ph)
                KF = sb("KF", [128, NCk, 256], BF16, ph)
                KBt = sb("KBt", [128, NCk, 256], BF16, ph)
                V = sb("V", [128, NCk, 512], BF16, ph)
                SG = sb("SG", [128, 4, T], BF16, ph)
                CB = sb("CB", [128, NCk, 512], BF16, ph)
                S32 = [[sb(f"S32{i}_{q}", [128, 2, 512], F32, ph) for q in range(nseq)] for i in range(2)]
                S16 = [[sb(f"S16{i}_{q}", [128, 2, 512], BF16, ph) for q in range(nseq)] for i in range(2)]
                WU = [sb(f"WU{i}", [128, NCH, 128], BF16, ph) for i in range(4)]
                WV = sb("WV", [128, NCH, 512], BF16, ph)
                MT = sb("MT", [128, 128], F32, ph)
                XIF = sb("XIF", [128, 128], F32, ph)
                XIB = sb("XIB", [128, 128], F32, ph)
                E1 = sb("E1", [128, 128], F32, ph)
                E2 = sb("E2", [128, 128], F32, ph)
                KD = sb("KD", [128, 4], F32, ph)
                TT = [sb(f"TT{i}", [128, 512], F32, ph) for i in range(6)]
                ST = [sb(f"ST{i}", [128, 128], BF16, ph) for i in range(2)]
                ON = [sb(f"ON{i}", [128, 512], BF16, ph) for i in range(2)]
                STS = sb("STS", [128, 6], F32, ph)
                MV = sb("MV", [128, 2], F32, ph)
                RSD = sb("RSD", [128, 1], F32, ph)
                NMR = sb("NMR", [128, 1], F32, ph)
                bQF, bQB, bKT, bKF, bKBt, bV, bSG, bCB = (Buf() for _ in range(8))
                bS32 = [[Buf() for _ in range(nseq)] for _ in range(2)]
                bS16 = [[Buf() for _ in range(nseq)] for _ in range(2)]
                bWU, bWV = [Buf() for _ in WU], Buf()
                bK, bTT, bST, bON, bGN = Buf(), [Buf() for _ in TT], [Buf(), Buf()], [Buf(), Buf()], Buf()
                if sample:
                    COS = sb("COS", [128, T], F32, ph)
                    SIN = sb("SIN", [128, T], F32, ph)
                    bRT = Buf()
                    kb.dma("sp", COS[:], dr["rope_cos"], writes=[bRT])
                    kb.dma("sp", SIN[:], dr["rope_sin"], writes=[bRT])
                wu_i = [0]

                def load_unit(h, u):
                    k = wu_i[0] % 4
                    wu_i[0] += 1
                    kb.dma("pool", WU[k][:], dr["ret_wqkg"][j, h, u], writes=[bWU[k]])
                    return k

                def proj_fm(k, tt, pi):
                    ts = slice(tt * 512, (tt + 1) * 512)
                    for kc in range(NCH):
                        kb.op("pe", lambda e, kc=kc: e.matmul(PS[pi][:, :], WU[k][:, kc, :], HN[:, kc, ts],
                                                               start=(kc == 0), stop=(kc == NCH - 1)),
                              reads=[bWU[k], bHN[tt]], writes=[bPS[pi]])

                for h in range(RH):
                    lgf = LGt[:, (j * 4 + h) * 2:(j * 4 + h) * 2 + 1]
                    lgb = LGt[:, (j * 4 + h) * 2 + 1:(j * 4 + h) * 2 + 2]
                    nlgf = NLG[:, (j * 4 + h) * 2:(j * 4 + h) * 2 + 1]
                    nlgb = NLG[:, (j * 4 + h) * 2 + 1:(j * 4 + h) * 2 + 2]
                    kb.op("act", lambda e: e.activation(out=E1[:], in_=DIFF, func=AF.Exp, scale=lgf), reads=[bC], writes=[bK])
                    kb.op("dve", lambda e: e.tensor_tensor(out=E1[:], in0=E1[:], in1=MA, op=ALU.mult), reads=[bK, bC], writes=[bK])
                    kb.op("act", lambda e: e.activation(out=E2[:], in_=DIFF, func=AF.Exp, scale=nlgb), reads=[bC, bK], writes=[bK])
                    kb.op("dve", lambda e: e.tensor_tensor(out=E2[:], in0=E2[:], in1=MB_, op=ALU.mult), reads=[bK, bC], writes=[bK])
                    kb.op("dve", lambda e: e.tensor_tensor(out=E1[:], in0=E1[:], in1=E2[:], op=ALU.add), reads=[bK], writes=[bK])
                    kb.op("act", lambda e: e.activation(out=E2[:], in_=POS1, func=AF.Exp, scale=nlgf), reads=[bC, bK], writes=[bK])
                    kb.op("dve", lambda e: e.tensor_tensor(out=MT[:], in0=E1[:], in1=E2[:], op=ALU.mult), reads=[bK], writes=[bK])
                    kb.op("act", lambda e: e.activation(out=XIF[:], in_=POS1, func=AF.Exp, scale=lgf), reads=[bC, bK], writes=[bK])
                    kb.op("act", lambda e: e.activation(out=XIB[:], in_=POSR, func=AF.Exp, scale=lgb), reads=[bC, bK], writes=[bK])
                    kb.op("act", lambda e: e.activation(out=KD[:, 0:1], in_=PREV, func=AF.Exp, scale=lgf), reads=[bC, bK], writes=[bK])
                    kb.op("act", lambda e: e.activation(out=KD[:, 1:2], in_=PIDX, func=AF.Exp, scale=lgb), reads=[bC, bK], writes=[bK])
                    kb.op("act", lambda e: e.activation(out=KD[:, 2:3], in_=C128, func=AF.Exp, scale=lgf), reads=[bC, bK], writes=[bK])
                    kb.op("act", lambda e: e.activation(out=KD[:, 3:4], in_=C128, func=AF.Exp, scale=lgb), reads=[bC, bK], writes=[bK])
                    for typ in range(2):
                        ka, kb_ = load_unit(h, typ * 2), load_unit(h, typ * 2 + 1)
                        for tt in range(nt):
                            ts = slice(tt * 512, (tt + 1) * 512)
                            pa, pb = next_ps(), next_ps()
                            proj_fm(ka, tt, pa)
                            proj_fm(kb_, tt, pb)
                            A2, B2 = TT[4], TT[5]
                            if sample:
                                kb.op("dve", lambda e: e.tensor_tensor(out=TT[0][:], in0=PS[pa][:, :], in1=COS[:, ts], op=ALU.mult),
                                      reads=[bPS[pa], bRT], writes=[bTT[0]])
                                kb.op("dve", lambda e: e.tensor_tensor(out=TT[1][:], in0=PS[pb][:, :], in1=SIN[:, ts], op=ALU.mult),
                                      reads=[bPS[pb], bRT], writes=[bTT[1]])
                                kb.op("dve", lambda e: e.tensor_tensor(out=TT[2][:], in0=PS[pa][:, :], in1=SIN[:, ts], op=ALU.mult),
                                      reads=[bPS[pa], bRT], writes=[bTT[2]])
                                kb.op("dve", lambda e: e.tensor_tensor(out=TT[3][:], in0=PS[pb][:, :], in1=COS[:, ts], op=ALU.mult),
                                      reads=[bPS[pb], bRT], writes=[bTT[3]])
                                kb.op("pool", lambda e: e.tensor_tensor(out=A2[:], in0=TT[0][:], in1=TT[1][:], op=ALU.subtract),
                                      reads=[bTT[0], bTT[1]], writes=[bTT[4]])
                                kb.op("pool", lambda e: e.tensor_tensor(out=B2[:], in0=TT[2][:], in1=TT[3][:], op=ALU.add),
                                      reads=[bTT[2], bTT[3]], writes=[bTT[5]])
                            else:
                                kb.op("act", lambda e: e.copy(out=A2[:], in_=PS[pa][:, :]), reads=[bPS[pa]], writes=[bTT[4]])
                                kb.op("act", lambda e: e.copy(out=B2[:], in_=PS[pb][:, :]), reads=[bPS[pb]], writes=[bTT[5]])
                            for half, src, bsrc in ((0, A2, bTT[4]), (1, B2, bTT[5])):
                                s3 = src[:, :].rearrange("p (a b) -> p a b", b=128)
                                if typ == 0:
                                    for dst, bdst, xi, eng in ((QF, bQF, XIF, "dve"), (QB, bQB, XIB, "pool")):
                                        kb.op(eng, lambda e, dst=dst, xi=xi, half=half, s3=s3: e.tensor_tensor(
                                            out=dst[:, half, ts].rearrange("p (a b) -> p a b", b=128), in0=s3,
                                            in1=xi[:, :].unsqueeze(1).to_broadcast([128, 4, 128]), op=ALU.mult),
                                            reads=[bsrc, bK], writes=[bdst])
                                else:
                                    kb.op("act", lambda e, half=half, src=src: e.mul(out=KT[:, half, ts], in_=src[:, :], mul=1.0 / 16.0),
                                          reads=[bsrc], writes=[bKT])
                    for u in range(4):
                        k = load_unit(h, 4 + u)
                        for tt in range(nt):
                            ts = slice(tt * 512, (tt + 1) * 512)
                            pi = next_ps()
                            proj_fm(k, tt, pi)
                            kb.op("act", lambda e, u=u, pi=pi, ts=ts: e.activation(out=SG[:, u, ts], in_=PS[pi][:, :], func=AF.Silu),
                                  reads=[bPS[pi]], writes=[bSG])
                    kb.dma("pool", WV[:], dr["ret_wv"][j, h], writes=[bWV])
                    for g in range(NCk):
                        cs = slice(g * 128, (g + 1) * 128)
                        pi = next_ps()
                        for kc in range(NCH):
                            kb.op("pe", lambda e, kc=kc, pi=pi, cs=cs: e.matmul(PS[pi][:, :], HN[:, kc, cs], WV[:, kc, :],
                                                                              start=(kc == 0), stop=(kc == NCH - 1)),
                                  reads=[bWV, bHN[g // 4]], writes=[bPS[pi]])
                        kb.op("act", lambda e, g=g, pi=pi: e.copy(out=V[:, g, :], in_=PS[pi][:, :]), reads=[bPS[pi]], writes=[bV])
                    for g in range(NCk):
                        cs = slice(g * 128, (g + 1) * 128)
                        pi = next_ps()
                        for dd in range(2):
                            kb.op("pe", lambda e, dd=dd, pi=pi, cs=cs: e.transpose(
                                out=PSB[pi][:, dd * 128:(dd + 1) * 128], in_=KT[:, dd, cs], identity=IDB[:]),
                                reads=[bKT, bC], writes=[bPS[pi]])
                        kb.op("act", lambda e, g=g, pi=pi: e.activation(out=KF[:, g, :], in_=PSB[pi][:, 0:256], func=AF.Copy, scale=KD[:, 0:1]),
                              reads=[bPS[pi], bK], writes=[bKF])
                        kb.op("dve", lambda e, g=g, pi=pi: e.tensor_scalar_mul(out=KBt[:, g, :], in0=PSB[pi][:, 0:256], scalar1=KD[:, 1:2]),
                              reads=[bPS[pi], bK, bKF], writes=[bKBt])

                    def state_init(d, s):
                        if sample:
                            kb.dma("sp", S32[d][s][:], dr["srf" if d == 0 else "srb"][j, h], writes=[bS32[d][s]])
                        else:
                            kb.op("pool", lambda e: e.memset(S32[d][s][:], 0.0), writes=[bS32[d][s]])
                        kb.op("act", lambda e: e.copy(out=S16[d][s][:], in_=S32[d][s][:]), reads=[bS32[d][s]], writes=[bS16[d][s]])

                    def state_update(d, s, g, Kt, bKt):
                        for dd in range(2):
                            pd = next_ps()
                            kb.op("pe", lambda e: e.matmul(PS[pd][:, :], Kt[:, g, dd * 128:(dd + 1) * 128], V[:, g, :], start=True, stop=True),
                                  reads=[bKt, bV], writes=[bPS[pd]])
                            kb.op("dve", lambda e: e.scalar_tensor_tensor(
                                out=S32[d][s][:, dd, :], in0=S32[d][s][:, dd, :], scalar=KD[:, 2 + d:3 + d], in1=PS[pd][:, :],
                                op0=ALU.mult, op1=ALU.add), reads=[bPS[pd], bS32[d][s], bK], writes=[bS32[d][s]])
                        kb.op("act", lambda e: e.copy(out=S16[d][s][:], in_=S32[d][s][:]), reads=[bS32[d][s]], writes=[bS16[d][s]])

                    def state_out(d, s):
                        if not sample:
                            nm = "nsf" if d == 0 else "nsb"
                            state_toks.append(kb.dma("sp", dr[nm][s, j, h], S32[d][s][:], reads=[bS32[d][s]]))

                    for s in range(nseq):
                        state_init(1, s)
                    for n in reversed(range(N)):
                        for s in range(nseq):
                            g = s * N + n
                            cs = slice(g * 128, (g + 1) * 128)
                            pc = next_ps()
                            for dd in range(2):
                                kb.op("pe", lambda e: e.matmul(PS[pc][:, :], QB[:, dd, cs], S16[1][s][:, dd, :], start=(dd == 0), stop=(dd == 1)),
                                      reads=[bQB, bS16[1][s]], writes=[bPS[pc]])
                            kb.op("act", lambda e: e.copy(out=CB[:, g, :], in_=PS[pc][:, :]), reads=[bPS[pc]], writes=[bCB])
                            state_update(1, s, g, KBt, bKBt)
                    for s in range(nseq):
                        state_out(1, s)
                        state_init(0, s)
                    rr = [0]

                    def stage_a(s, n):
                        g = s * N + n
                        cs = slice(g * 128, (g + 1) * 128)
                        i2 = rr[0] % 2
                        rr[0] += 1
                        p_s = next_ps()
                        for dd in range(2):
                            kb.op("pe", lambda e: e.matmul(PS[p_s][:, 0:128], KT[:, dd, cs], QF[:, dd, cs], start=(dd == 0), stop=(dd == 1)),
                                  reads=[bKT, bQF], writes=[bPS[p_s]])
                        kb.op("dve", lambda e: e.tensor_tensor(out=ST[i2][:], in0=PS[p_s][:, 0:128], in1=MT[:], op=ALU.mult),
                              reads=[bPS[p_s], bK], writes=[bST[i2]])
                        p_o = next_ps()
                        kb.op("pe", lambda e: e.matmul(PS[p_o][:, :], ST[i2][:], V[:, g, :], start=True, stop=False),
                              reads=[bST[i2], bV], writes=[bPS[p_o]])
                        for dd in range(2):
                            kb.op("pe", lambda e: e.matmul(PS[p_o][:, :], QF[:, dd, cs], S16[0][s][:, dd, :], start=False, stop=False),
                                  reads=[bQF, bS16[0][s]], writes=[bPS[p_o]])
                        kb.op("pe", lambda e: e.matmul(PS[p_o][:, :], IDB[:], CB[:, g, :], start=False, stop=True),
                              reads=[bCB, bC], writes=[bPS[p_o]])
                        state_update(0, s, g, KF, bKF)
                        return (cs, i2, p_o)

                    def stage_b(ctx):
                        cs, i2, p_o = ctx
                        kb.op("dve", lambda e: e.bn_stats(out=STS[:], in_=PS[p_o][:, :]), reads=[bPS[p_o]], writes=[bGN])
                        kb.op("dve", lambda e: e.bn_aggr(out=MV[:], in_=STS[:]), reads=[bGN], writes=[bGN])
                        kb.op("act", lambda e: e.activation(out=RSD[:], in_=MV[:, 1:2], func=AF.Sqrt, bias=EPSC[:, 0:1], scale=1.0),
                              reads=[bGN, bC], writes=[bGN])
                        kb.op("dve", lambda e: e.reciprocal(out=RSD[:], in_=RSD[:]), reads=[bGN], writes=[bGN])
                        kb.op("dve", lambda e: e.scalar_tensor_tensor(out=NMR[:], in0=MV[:, 0:1], scalar=-1.0, in1=RSD[:],
                                                                       op0=ALU.mult, op1=ALU.mult), reads=[bGN], writes=[bGN])
                        kb.op("act", lambda e: e.activation(out=ON[i2][:], in_=PS[p_o][:, :], func=AF.Identity,
                                                             scale=RSD[:, 0:1], bias=NMR[:, 0:1]),
                              reads=[bPS[p_o], bGN], writes=[bON[i2]])
                        p_t = next_ps()
                        for vv in range(4):
                            kb.op("pe", lambda e: e.transpose(
                                out=PSB[p_t][:, vv * 128:(vv + 1) * 128], in_=ON[i2][:, vv * 128:(vv + 1) * 128], identity=IDB[:]),
                                reads=[bON[i2], bC], writes=[bPS[p_t]])
                        kb.op("dve", lambda e: e.tensor_tensor(
                            out=SG[:, :, cs], in0=PSB[p_t][:, 0:512].rearrange("p (a b) -> p a b", b=128), in1=SG[:, :, cs], op=ALU.mult),
                            reads=[bPS[p_t], bSG], writes=[bSG])

                    prev = None
                    for n in range(N):
                        for s in range(nseq):
                            cur = stage_a(s, n)
                            if prev is not None:
                                stage_b(prev)
                            prev = cur
                    stage_b(prev)
                    for s in range(nseq):
                        state_out(0, s)
                    kb.dma("sp", YTD[:, h * 4:(h + 1) * 4, 0:T], SG[:, :, :], reads=[bSG], writes=[bYTD])
                kb.barrier()

        def hyena_core(T, L, nseq, j, sample, pname):
            NCk, NQ, nt = T // 128, L // 128, T // 512
            PI = math.pi
            ps_n[0] = 4
            with contextlib.ExitStack() as ph:
                W3 = sb("W3", [64, 4096], F32, ph)
                HD = sb("HD", [64, L], F32, ph)
                HYF = sb("HYF", [64, 4], F32, ph)
                ONESF = sb("ONESF", [128, 128], F32, ph)
                bHY = Buf()
                kb.dma("sp", W3[:], dr["hy_w3"][j], writes=[bHY])
                kb.dma("sp", HYF[:], dr["hyf"][j], writes=[bHY])
                kb.op("dve", lambda e: e.memset(ONESF[:], 1.0), writes=[bHY])
                with contextlib.ExitStack() as p0:
                    ZF = sb("ZF", [33, L], F32, p0)
                    W1 = sb("W1", [33, 64], F32, p0)
                    W2 = sb("W2", [64, 64], F32, p0)
                    H1 = sb("H1", [64, L], F32, p0)
                    AR = [sb(f"AR{i}", [64, 512], F32, p0) for i in range(3)]
                    bAR, bH1 = Buf(), Buf()
                    kb.dma("sp", ZF[:], dr["zf_" + pname], writes=[bHY])
                    kb.dma("sp", W1[:], dr["hy_w1"][j], writes=[bHY])
                    kb.dma("sp", W2[:], dr["hy_w2"][j], writes=[bHY])
                    for lay in range(2):
                        src, Wl, dst, bdst = ((ZF, W1, H1, bH1), (H1, W2, HD, bHY))[lay]
                        bsrc = bHY if lay == 0 else bH1
                        for c0 in range(0, L, 512):
                            w = min(512, L - c0)
                            pi = next_ps()
                            kb.op("pe", lambda e: e.matmul(PS[pi][0:64, 0:w], Wl[:, :], src[:, c0:c0 + w], start=True, stop=True),
                                  reads=[bHY, bsrc], writes=[bPS[pi]])
                            kb.op("dve", lambda e: e.tensor_scalar(out=AR[0][:, 0:w], in0=PS[pi][0:64, 0:w], scalar1=HYF[:, lay:lay + 1],
                                                                    scalar2=HYF[:, 2 + lay:3 + lay], op0=ALU.add, op1=ALU.mult),
                                  reads=[bPS[pi], bHY], writes=[bAR])
                            kb.op("dve", lambda e: e.tensor_scalar(out=AR[1][:, 0:w], in0=AR[0][:, 0:w], scalar1=PI, scalar2=-2.0 * PI,
                                                                    op0=ALU.is_gt, op1=ALU.mult), reads=[bAR], writes=[bAR])
                            kb.op("dve", lambda e: e.tensor_scalar(out=AR[2][:, 0:w], in0=AR[0][:, 0:w], scalar1=-PI, scalar2=2.0 * PI,
                                                                    op0=ALU.is_lt, op1=ALU.mult), reads=[bAR], writes=[bAR])
                            kb.op("dve", lambda e: e.tensor_tensor(out=AR[0][:, 0:w], in0=AR[0][:, 0:w], in1=AR[1][:, 0:w], op=ALU.add),
                                  reads=[bAR], writes=[bAR])
                            kb.op("dve", lambda e: e.tensor_tensor(out=AR[0][:, 0:w], in0=AR[0][:, 0:w], in1=AR[2][:, 0:w], op=ALU.add),
                                  reads=[bAR], writes=[bAR])
                            kb.op("act", lambda e: e.activation(out=dst[:, c0:c0 + w], in_=AR[0][:, 0:w], func=AF.Sin),
                                  reads=[bAR], writes=[bdst])
                    kb.barrier()
                WINC = sb("WINC", [128, NQ, 128], F32, ph)
                FW = [sb(f"FW{i}", [128, 4, 128], F32, ph) for i in range(2)]
                ABS_ = [sb(f"ABS{i}", [128, 4, 128], F32, ph) for i in range(2)]
                HS = sb("HS", [128, 2, NQ, 128], BF16, ph)
                HDF = sb("HDF", [128, 2, NQ, 128], BF16, ph)
                RN = sb("RN", [128, 2, 128], F32, ph)
                P32 = sb("P32", [128, T], F32, ph)
                U32 = sb("U32", [128, T], F32, ph)
                UU = [sb(f"UU{i}", [128, T], BF16, ph) for i in range(3)]
                ZN = [sb(f"ZN{i}", [128, T], BF16, ph) for i in range(2)]
                ZH = [sb(f"ZH{i}", [128, NCk, 256], BF16, ph) for i in range(2)]
                FU = [sb(f"FU{i}", [128, NQ, 128], BF16, ph) for i in range(3)]
                GU = [sb(f"GU{i}", [128, L], BF16, ph) for i in range(3)]
                WI = [sb(f"WI{i}", [128, NCH, 128], BF16, ph) for i in range(3)]
                AKK = [[sb(f"AK{q}{i}", [128, 2, 128], F32, ph) for i in range(2)] for q in range(2)]
                PTT = [[sb(f"PT{q}{i}", [128, 128], F32, ph) for i in range(4)] for q in range(2)]
                PQ = sb("PQ", [128, NQ, 2, nseq, 128], BF16, ph)
                R32 = [sb(f"R32{i}", [128, 512], F32, ph) for i in range(2)]
                bWIN, bFW, bABS, bHS, bRN, bP32, bU32 = Buf(), [Buf(), Buf()], [Buf(), Buf()], Buf(), Buf(), Buf(), Buf()
                bUU, bZN, bZH = [Buf() for _ in UU], [Buf(), Buf()], [Buf(), Buf()]
                bFU, bGU, bWI = [Buf() for _ in FU], [Buf() for _ in GU], [Buf() for _ in WI]
                bAKK = [[Buf(), Buf()] for _ in range(2)]
                bPTT = [[Buf() for _ in range(4)] for _ in range(2)]
                bPQ, bR32 = Buf(), [Buf(), Buf()]
                akc = [0]
                cnt = {"f": 0, "g": 0, "w": 0}
                if cfg.get("hy_dummy"):
                    DUMMY = sb("DUMMY", [128, cfg["hy_dummy"]], F32, ph)
                resident = (L == 256)
                if resident:
                    FUALL = sb("FUALL", [128, 2, NQ, NQ, 128], BF16, ph)
                    GUALL = sb("GUALL", [128, 2, NQ, L], BF16, ph)
                    bTAB = Buf()
                    for trig in range(2):
                        for kq in range(NQ):
                            kb.dma("sp", FUALL[:, trig, kq], dr["dft_" + pname][trig, kq], writes=[bTAB])
                            kb.dma("sp", GUALL[:, trig, kq], dr["idft_" + pname][trig, kq], writes=[bTAB])
                o_bi, o_cw, o_cb, o_fb = pvo["hy_b_in"][0], pvo["hy_cw"][0], pvo["hy_cb"][0], pvo["hy_fb"][0]
                W3v = W3[:, :].rearrange("p (a c) -> p a c", a=4)
                hy_stage = cfg.get("hy_stage", 9)
                for cc in range(cfg.get("hy_ncc", NCH) if hy_stage >= 2 else 0):
                    kb.dma("sp", WINC[:], dr["win_" + pname][cc], writes=[bWIN])
                    skp = cfg.get("hy_skip", [])
                    for tc in range(0 if "filt" in skp else NQ):
                        i2 = tc % 2
                        pi = next_ps()
                        kb.op("pe", lambda e: e.matmul(PS[pi][:, :].rearrange("p (a c) -> p a c", a=4), HD[:, tc * 128:(tc + 1) * 128],
                                                        W3v[:, :, cc * 128:(cc + 1) * 128], start=True, stop=True),
                              reads=[bHY], writes=[bPS[pi]])
                        kb.op("dve", lambda e: e.tensor_tensor(out=FW[i2][:], in0=PS[pi][:, :].rearrange("p (a c) -> p a c", a=4),
                                                                in1=WINC[:, tc, :].unsqueeze(1).to_broadcast([128, 4, 128]), op=ALU.mult),
                              reads=[bPS[pi], bWIN], writes=[bFW[i2]])
                        if tc == 0:
                            kb.op("dve", lambda e: e.memset(FW[i2][0:1, 2:4, :], 0.0), reads=[bFW[i2]], writes=[bFW[i2]])
                        kb.op("pool", lambda e: e.tensor_tensor(out=HS[:, :, tc, :], in0=FW[i2][:, 0:2, :], in1=FW[i2][:, 2:4, :], op=ALU.add),
                              reads=[bFW[i2]], writes=[bHS])
                        kb.op("pool", lambda e: e.tensor_tensor(out=HDF[:, :, tc, :], in0=FW[i2][:, 0:2, :], in1=FW[i2][:, 2:4, :], op=ALU.subtract),
                              reads=[bFW[i2]], writes=[bHS])
                        kb.op("act", lambda e: e.activation(out=ABS_[i2][:], in_=FW[i2][:], func=AF.Abs),
                              reads=[bFW[i2]], writes=[bABS[i2]])
                        kb.op("pe", lambda e: e.matmul(PS[4][:, :], ONESF[:, :], ABS_[i2][:].rearrange("p a c -> p (a c)"),
                                                        start=(tc == 0), stop=(tc == NQ - 1)),
                              reads=[bABS[i2], bHY], writes=[bPS[4]])
                    if "filt" not in skp:
                        kb.op("act", lambda e: e.copy(out=RN[:], in_=PS[4][:, 0:256].rearrange("p (a c) -> p a c", a=2)),
                              reads=[bPS[4]], writes=[bRN])
                        kb.op("dve", lambda e: e.tensor_tensor(out=RN[:], in0=RN[:],
                                                                in1=PS[4][:, 256:512].rearrange("p (a c) -> p a c", a=2), op=ALU.add),
                              reads=[bPS[4], bRN], writes=[bRN])
                    else:
                        kb.op("dve", lambda e: e.memset(RN[:], 1.0), writes=[bRN])
                    kb.op("dve", lambda e: e.tensor_scalar_add(out=RN[:], in0=RN[:], scalar1=EPS), reads=[bRN], writes=[bRN])
                    kb.op("dve", lambda e: e.reciprocal(out=RN[:], in_=RN[:]), reads=[bRN], writes=[bRN])
                    for part in range(3 if (hy_stage >= 3 and "proj" not in skp) else 0):
                        col = part * 8 + cc
                        k = cnt["w"] % 3
                        cnt["w"] += 1
                        kb.dma("pool", WI[k][:], dr["hy_wi"][j, cc, part], writes=[bWI[k]])
                        for tt in range(nt):
                            ts = slice(tt * 512, (tt + 1) * 512)
                            pi = next_ps()
                            for kc in range(NCH):
                                kb.op("pe", lambda e: e.matmul(PS[pi][:, :], WI[k][:, kc, :], HN[:, kc, ts], start=(kc == 0), stop=(kc == NCH - 1)),
                                      reads=[bWI[k], bHN[tt]], writes=[bPS[pi]])
                            kb.op("act", lambda e: e.activation(out=P32[:, ts], in_=PS[pi][:, :], func=AF.Identity,
                                                                 bias=PVEC[:, o_bi + j * 24 + col:o_bi + j * 24 + col + 1], scale=1.0),
                                  reads=[bPS[pi], bC], writes=[bP32])
                        cw = [PVEC[:, o_cw + (j * 3 + tap) * 24 + col:o_cw + (j * 3 + tap) * 24 + col + 1] for tap in range(3)]
                        cbias = PVEC[:, o_cb + j * 24 + col:o_cb + j * 24 + col + 1]
                        kb.op("act", lambda e: e.activation(out=U32[:, 0:T], in_=P32[:, 0:T], func=AF.Identity, bias=cbias, scale=cw[1]),
                              reads=[bP32, bC], writes=[bU32])
                        for s in range(nseq):
                            a, b = s * L, (s + 1) * L
                            kb.op("dve", lambda e: e.scalar_tensor_tensor(out=U32[:, a + 1:b], in0=P32[:, a:b - 1], scalar=cw[0],
                                                                           in1=U32[:, a + 1:b], op0=ALU.mult, op1=ALU.add),
                                  reads=[bP32, bU32, bC], writes=[bU32])
                            kb.op("dve", lambda e: e.scalar_tensor_tensor(out=U32[:, a:b - 1], in0=P32[:, a + 1:b], scalar=cw[2],
                                                                           in1=U32[:, a:b - 1], op0=ALU.mult, op1=ALU.add),
                                  reads=[bP32, bU32, bC], writes=[bU32])
                        kb.op("pool", lambda e: e.tensor_copy(out=UU[part][:], in_=U32[:, 0:T]), reads=[bU32], writes=[bUU[part]])
                    zin, bzin = UU[0], bUU[0]
                    for o in range(2 if hy_stage >= 4 else 0):
                        for g4 in range(0, 0 if "tr" in skp else NCk, 4):
                            pi = next_ps()
                            for q4 in range(4):
                                g = g4 + q4
                                kb.op("pe", lambda e: e.transpose(out=PSB[pi][:, q4 * 128:(q4 + 1) * 128], in_=zin[:, g * 128:(g + 1) * 128],
                                                                   identity=IDB[:]), reads=[bzin, bC], writes=[bPS[pi]])
                            kb.op("act", lambda e: e.copy(out=ZH[0][:, g4:g4 + 4, 0:128],
                                                           in_=PSB[pi][:, 0:512].rearrange("p (a b) -> p a b", b=128)),
                                  reads=[bPS[pi]], writes=[bZH[0]])
                            kb.op("dve", lambda e: e.tensor_copy(out=ZH[1][:, g4:g4 + 4, 0:128], in_=ZH[0][:, g4:g4 + 4, 0:128]),
                                  reads=[bZH[0]], writes=[bZH[1]])
                        for s in range(nseq):
                            kb.op("pool", lambda e: e.tensor_copy(out=ZH[0][:, s * NQ:(s + 1) * NQ, 128:256], in_=HS[:, o, :, :]),
                                  reads=[bHS], writes=[bZH[0]])
                            kb.op("pool", lambda e: e.tensor_copy(out=ZH[1][:, s * NQ:(s + 1) * NQ, 128:256], in_=HDF[:, o, :, :]),
                                  reads=[bHS], writes=[bZH[1]])
                        for kq in range(0 if "fwd" in skp else NQ):
                            for s in range(nseq):
                                AK, bAK = AKK[akc[0] % 2], bAKK[akc[0] % 2]
                                PT, bPT = PTT[akc[0] % 2], bPTT[akc[0] % 2]
                                akc[0] += 1
                                for trig in range(2):
                                    if resident:
                                        fu_t, fu_b = FUALL[:, trig, kq], bTAB
                                    else:
                                        kf = cnt["f"] % 3
                                        cnt["f"] += 1
                                        kb.dma("sp", FU[kf][:], dr["dft_" + pname][trig, kq], writes=[bFU[kf]])
                                        fu_t, fu_b = FU[kf], bFU[kf]
                                    pi = next_ps()
                                    for tc in range(NQ):
                                        kb.op("pe", lambda e: e.matmul(PS[pi][:, 0:256], fu_t[:, tc, :], ZH[trig][:, s * NQ + tc, :],
                                                                        start=(tc == 0), stop=(tc == NQ - 1)),
                                              reads=[fu_b, bZH[trig]], writes=[bPS[pi]])
                                    kb.op("act", lambda e: e.copy(out=AK[trig][:].rearrange("p a c -> p (a c)"), in_=PS[pi][:, 0:256]),
                                          reads=[bPS[pi]], writes=[bAK[trig]])
                                    kb.op("dve", lambda e: e.tensor_tensor(out=AK[trig][:, 1, :], in0=AK[trig][:, 1, :], in1=RN[:, o, :], op=ALU.mult),
                                          reads=[bAK[trig], bRN], writes=[bAK[trig]])
                                A_, Kc, B_, Ks = AK[0][:, 0, :], AK[0][:, 1, :], AK[1][:, 0, :], AK[1][:, 1, :]
                                kb.op("dve", lambda e: e.tensor_tensor(out=PT[0][:], in0=A_, in1=Kc, op=ALU.mult), reads=bAK, writes=[bPT[0]])
                                kb.op("pool", lambda e: e.tensor_tensor(out=PT[1][:], in0=B_, in1=Ks, op=ALU.mult), reads=bAK, writes=[bPT[1]])
                                kb.op("dve", lambda e: e.tensor_tensor(out=PT[2][:], in0=B_, in1=Kc, op=ALU.mult), reads=bAK, writes=[bPT[2]])
                                kb.op("pool", lambda e: e.tensor_tensor(out=PT[3][:], in0=A_, in1=Ks, op=ALU.mult), reads=bAK, writes=[bPT[3]])
                                kb.op("dve", lambda e: e.tensor_tensor(out=PQ[:, kq, 0, s, :], in0=PT[0][:], in1=PT[1][:], op=ALU.subtract),
                                      reads=[bPT[0], bPT[1]], writes=[bPQ])
                                kb.op("pool", lambda e: e.tensor_tensor(out=PQ[:, kq, 1, s, :], in0=PT[2][:], in1=PT[3][:], op=ALU.add),
                                      reads=[bPT[2], bPT[3]], writes=[bPQ])
                        if cfg.get("hy_dbg") and cc == 0 and o == 0:
                            dbg_toks.append(kb.dma("sp", dr["dbg_hd"][0:64, 0:L], HD[:, :], reads=[bHY]))
                            dbg_toks.append(kb.dma("sp", dr["dbg_rn"][:, :], RN[:].rearrange("p a c -> p (a c)"), reads=[bRN]))
                            dbg_toks.append(kb.dma("pool", dr["dbg_pq"][:, 0:NQ * 2 * nseq * 128],
                                                   PQ[:].rearrange("p a b c d -> p (a b c d)"), reads=[bPQ]))
                            dbg_toks.append(kb.dma("pool", dr["dbg_zh"][:, 0:NCk * 256],
                                                   ZH[0][:].rearrange("p a b -> p (a b)"), reads=[bZH[0]]))
                        wt = min(512, L)
                        accs = [(s, c0) for s in range(nseq) for c0 in range(0, L, wt)]
                        assert len(accs) == 4
                        if hy_stage < 5:
                            continue
                        for kq in range(NQ):
                            for trig in range(2):
                                if resident:
                                    gu_t, gu_b = GUALL[:, trig, kq], bTAB
                                else:
                                    kg = cnt["g"] % 3
                                    cnt["g"] += 1
                                    kb.dma("sp", GU[kg][:], dr["idft_" + pname][trig, kq], writes=[bGU[kg]])
                                    gu_t, gu_b = GU[kg], bGU[kg]
                                for ai, (s, c0) in enumerate(accs):
                                    if cfg.get("hy_nomm") or ai >= cfg.get("hy_nacc", 4):
                                        continue
                                    ab = cfg.get("hy_accbase", 4)
                                    hv = cfg.get("hy_var", 0)
                                    lh = IDB[:] if hv == 1 else PQ[:, kq, trig, s, :]
                                    rh = UU[0][:, c0:c0 + wt] if hv == 2 else gu_t[:, c0:c0 + wt]
                                    kb.op("pe", lambda e: e.matmul(PS[ab + ai][:, 0:wt], lh, rh,
                                                                    start=(kq == 0 and trig == 0), stop=(kq == NQ - 1 and trig == 1)),
                                          reads=[bPQ, gu_b], writes=[bPS[ab + ai]])
                        fb = PVEC[:, o_fb + (j * 2 + o) * 8 + cc:o_fb + (j * 2 + o) * 8 + cc + 1]
                        if hy_stage < 6:
                            continue
                        for ai, (s, c0) in enumerate(accs):
                            ts = slice(s * L + c0, s * L + c0 + wt)
                            r = ai % 2
                            kb.op("dve", lambda e: e.scalar_tensor_tensor(out=R32[r][:, 0:wt], in0=zin[:, ts], scalar=fb, in1=PS[4 + ai][:, 0:wt],
                                                                           op0=ALU.mult, op1=ALU.add),
                                  reads=[bzin, bPS[4 + ai], bC], writes=[bR32[r]])
                            kb.op("pool", lambda e: e.tensor_tensor(out=ZN[o][:, ts], in0=R32[r][:, 0:wt], in1=UU[o + 1][:, ts], op=ALU.mult),
                                  reads=[bR32[r], bUU[o + 1]], writes=[bZN[o]])
                        zin, bzin = ZN[o], bZN[o]
                    kb.dma("sp", YTD[:, cc, 0:T], ZN[1][:, :], reads=[bZN[1]], writes=[bYTD])
                kb.barrier()
            ps_n[0] = 8

        def outproj(T, w_src, nk, g_idx, scale_cols=None, bias_col=None):
            nt = T // 512
            with contextlib.ExitStack() as ph:
                SRC = sb("SRC", [128, nk, T], BF16, ph)
                WO = sb("WO", [128, nk, D], BF16, ph)
                bSRC, bWO = Buf(), [Buf() for _ in range(nk)]
                GBt = sb("GBt", [128, NCH], F32, ph)
                bGB = Buf()
                if bias_col is not None:
                    kb.op("dve", lambda e: e.tensor_tensor(out=GBt[:], in0=AB[:, g_idx, :], in1=bias_col, op=ALU.mult),
                          reads=[bAB, bC], writes=[bGB])
                kb.dma("sp", SRC[:], YTD[:, 0:nk, 0:T], reads=[bYTD], writes=[bSRC])
                for kc in range(nk):
                    kb.dma("pool", WO[:, kc, :], w_src[kc], writes=[bWO[kc]])
                    if scale_cols is not None:
                        kb.op("dve", lambda e, kc=kc: e.tensor_scalar_mul(out=WO[:, kc, :], in0=WO[:, kc, :], scalar1=scale_cols[:, kc:kc + 1]),
                              reads=[bWO[kc], bC], writes=[bWO[kc]])
                for tt in range(nt):
                    ts = slice(tt * 512, (tt + 1) * 512)
                    for dc in range(NCH):
                        pi = next_ps()
                        for kc in range(nk):
                            kb.op("pe", lambda e, kc=kc, pi=pi, dc=dc, ts=ts: e.matmul(
                                PS[pi][:, :], WO[:, kc, dc * 128:(dc + 1) * 128], SRC[:, kc, ts], start=(kc == 0), stop=(kc == nk - 1)),
                                reads=[bWO[kc], bSRC], writes=[bPS[pi]])
                        kb.op("dve", lambda e, pi=pi, dc=dc, ts=ts: e.scalar_tensor_tensor(
                            out=Xh[0][:, dc, ts], in0=PS[pi][:, :], scalar=AB[:, g_idx, dc:dc + 1], in1=Xh[0][:, dc, ts],
                            op0=ALU.mult, op1=ALU.add),
                            reads=[bPS[pi], bAB, bX[dc][tt]], writes=[bX[dc][tt]])
                        if bias_col is not None:
                            kb.op("dve", lambda e, dc=dc, ts=ts: e.tensor_scalar(
                                out=Xh[0][:, dc, ts], in0=Xh[0][:, dc, ts], scalar1=GBt[:, dc:dc + 1], scalar2=None, op0=ALU.add),
                                reads=[bGB, bX[dc][tt]], writes=[bX[dc][tt]])
                kb.barrier()

        def final_norm(T, pname):
            nt = T // 512
            with contextlib.ExitStack() as ph:
                kb.op("dve", lambda e: e.tensor_copy(out=AB[:, 6, :], in_=pvc("final_g", 0, 8)), reads=[bC], writes=[bAB])
                SQ = [sb(f"FSQ{i}", [128, NCH, 512], BF16, ph) for i in range(2)]
                RS = [sb(f"FRS{i}", [128, 512], F32, ph) for i in range(2)]
                TM = [sb(f"FTM{i}", [128, NCH, 512], F32, ph) for i in range(2)]
                bSQ, bRS, bTM = [Buf(), Buf()], [Buf(), Buf()], [Buf(), Buf()]
                for tt in range(nt):
                    s = tt % 2
                    ts = slice(tt * 512, (tt + 1) * 512)
                    xb = [bX[c][tt] for c in range(NCH)]
                    kb.op("act", lambda e, s=s, ts=ts: e.activation(out=SQ[s][:], in_=Xh[0][:, :, ts], func=AF.Square),
                          reads=xb, writes=[bSQ[s]])
                    pi = next_ps()
                    for c in range(NCH):
                        kb.op("pe", lambda e, c=c, s=s, pi=pi: e.matmul(
                            PS[pi][:, :], ONESB[:, :], SQ[s][:, c, :], start=(c == 0), stop=(c == NCH - 1)),
                            reads=[bSQ[s], bC], writes=[bPS[pi]])
                    kb.op("act", lambda e, s=s, pi=pi: e.activation(
                        out=RS[s][:], in_=PS[pi][:, :], func=AF.Sqrt, bias=EPSC[:, 0:1], scale=1.0 / D),
                        reads=[bPS[pi], bC], writes=[bRS[s]])
                    kb.op("dve", lambda e, s=s: e.reciprocal(out=RS[s][:], in_=RS[s][:]), reads=[bRS[s]], writes=[bRS[s]])
                    kb.op("dve", lambda e, s=s, ts=ts: e.tensor_tensor(
                        out=TM[s][:], in0=Xh[0][:, :, ts], in1=RS[s][:].unsqueeze(1).to_broadcast([128, NCH, 512]),
                        op=ALU.mult), reads=xb + [bRS[s]], writes=[bTM[s]])
                    kb.op("dve", lambda e, s=s: e.tensor_tensor(
                        out=TM[s][:], in0=TM[s][:], in1=AB[:, 6, :].unsqueeze(2).to_broadcast([128, NCH, 512]),
                        op=ALU.mult), reads=[bTM[s], bAB], writes=[bTM[s]])
                    out_toks.append(kb.dma("sp", dr["yT_" + pname][:, :, ts], TM[s][:], reads=[bTM[s]]))
                kb.barrier()

        def load_x(T, src, rd=()):
            nt = T // 512
            for c in range(NCH):
                kb.dma("sp", Xh[0][:, c, 0:T], src[:, c, 0:T], reads=list(rd), writes=[bX[c][tt] for tt in range(nt)])

        def store_x(T):
            nt = T // 512
            for c in range(NCH):
                kb.dma("sp", XS[:, c, 0:T], Xh[0][:, c, 0:T], reads=[bX[c][tt] for tt in range(nt)], writes=[bXS])

        out_toks = []
        do_ffn = cfg.get("ffn", True)
        do_ret = cfg.get("ret", True)
        do_hy = cfg.get("hyena", True)
        passes = cfg.get("passes", ("p", "s"))
        for pname, T, j, L, nseq in (("p", TP, 0, 256, 4), ("s", TS, 1, 2048, 1)):
            if pname not in passes:
                continue
            sample = pname == "s"
            xsrc = dr["xT_" + pname]
            x_in_xs = False
            for l in range(DEPTH):
                l2 = l // 2
                set_ab(l, j)
                has_mixer = (do_ret if l % 2 == 0 else do_hy)
                if has_mixer:
                    with contextlib.ExitStack() as phx:
                        Xh[0] = sb("X", [128, NCH, TS], F32, phx)
                        load_x(T, XS if x_in_xs else xsrc, rd=[bXS] if x_in_xs else [])
                        norm_phase(T, 0, 1)
                    if l % 2 == 0:
                        retention_core(T, L, nseq, l2, sample, pname)
                    else:
                        hyena_core(T, L, nseq, l2, sample, pname)
                with contextlib.ExitStack() as phx:
                    Xh[0] = sb("X", [128, NCH, TS], F32, phx)
                    load_x(T, XS if x_in_xs else xsrc, rd=[bXS] if x_in_xs else [])
                    if has_mixer:
                        if l % 2 == 0:
                            o_ln = pvo["ret_ln_g"][0]
                            outproj(T, dr["ret_wo"][l2], 16, 2, scale_cols=PVEC[:, o_ln + l2 * 16:o_ln + l2 * 16 + 16])
                        else:
                            o_b = pvo["hy_b_out"][0]
                            outproj(T, dr["hy_wo"][l2], 8, 2, bias_col=PVEC[:, o_b + l2 * 8:o_b + l2 * 8 + 8])
                    if do_ffn:
                        if l % 2 == 0:
                            norm_phase(T, 3, 4)
                            ffn_dense(T, l2)
                        else:
                            with contextlib.ExitStack() as ph2:
                                WR = sb("WR", [128, NCH, NE], F32, ph2)
                                GT = sb("GT", [128, TS // 128, NE], F32, ph2)
                                bWR, bGT = Buf(), Buf()
                                kb.dma("sp", WR[:], dr["moe_r"][l2], writes=[bWR])
                                norm_phase(T, 3, 4, router=(WR, GT, bGT, bWR))
                                moe(T, l2, GT, bGT)
                    if l < DEPTH - 1:
                        store_x(T)
                        x_in_xs = True
                        kb.barrier()
                    else:
                        final_norm(T, pname)
        for t in out_toks + state_toks + dbg_toks:
            kb.wait("sp", t[0], t[1])
    return nc


def kernel(**inputs):
    cfg = inputs.pop("_cfg", {})
    inp = {k: np.asarray(v) for k, v in inputs.items()}
    shared = host_prep_shared(inp, cfg.get("ffn", True))
    if not cfg.get("ffn", True):
        shared = {k: v for k, v in shared.items() if not (k.startswith("ffn_") or k.startswith("moe_"))}
    ncores = cfg.get("ncores", 8)
    per_core = [host_prep(inp, i) for i in range(ncores)]
    shapes = {}
    for k, v in {**shared, **per_core[0]}.items():
        shapes[k] = (v.shape, "ExternalInput")
    shapes["yT_p"] = ((128, NCH, TP), "ExternalOutput")
    shapes["yT_s"] = ((128, NCH, TS), "ExternalOutput")
    shapes["nsf"] = ((4, 2, RH, 128, 2, RDV), "ExternalOutput")
    shapes["nsb"] = ((4, 2, RH, 128, 2, RDV), "ExternalOutput")
    if cfg.get("hy_dbg"):
        shapes["dbg_hd"] = ((128, 2048), "ExternalOutput")
        shapes["dbg_rn"] = ((128, 256), "ExternalOutput")
        shapes["dbg_pq"] = ((128, 8192), "ExternalOutput")
        shapes["dbg_zh"] = ((128, 4096), "ExternalOutput")
    nc = build_program(shapes, cfg)
    in_maps = [{**shared, **per_core[i]} for i in range(ncores)]
    res = run_bass_kernel_spmd(nc, in_maps, core_ids=list(range(ncores)))
    yp = np.zeros((32, 256, D), np.float32)
    ys = np.zeros((8, 2048, D), np.float32)
    nsf = np.zeros((32, 2, RH, RDK, RDV), np.float32)
    nsb = np.zeros((32, 2, RH, RDK, RDV), np.float32)
    for i in range(ncores):
        r = res.results[i]
        yp[4 * i:4 * i + 4] = r["yT_p"].transpose(2, 1, 0).reshape(4, 256, D)
        ys[i] = r["yT_s"].transpose(2, 1, 0).reshape(TS, D)
        nsf[4 * i:4 * i + 4] = r["nsf"].transpose(0, 1, 2, 4, 3, 5).reshape(4, 2, RH, RDK, RDV)
        nsb[4 * i:4 * i + 4] = r["nsb"].transpose(0, 1, 2, 4, 3, 5).reshape(4, 2, RH, RDK, RDV)
    return yp, ys, nsf, nsb
```
